# Optimizing a Trainium2 kernel written in Bass

```python
import math
import jax, jax.numpy as jnp
from jax import lax
import numpy as np

D_MODEL = 1024
BATCH = 8
SEQ = 2048
DEPTH = 2
DEC_BATCH = 128
DEC_SEQ = 4
PAST_LEN = 2048
PAGE_SIZE = 128

N_MIXERS = 2
N_ATTN_LAYERS = (DEPTH + 1) // 2
N_RET_LAYERS = DEPTH // 2

ATT_HEADS = 8
ATT_KV_HEADS = 2
ATT_HEAD_DIM = D_MODEL // ATT_HEADS
ATT_GROUP = ATT_HEADS // ATT_KV_HEADS
IDX_HEADS = 4
IDX_DIM = 64
TOPK_MAX = 256
Q_BLOCK = 128
ATT_SPLITS = (ATT_HEADS * ATT_HEAD_DIM, ATT_KV_HEADS * ATT_HEAD_DIM, ATT_KV_HEADS * ATT_HEAD_DIM,
              IDX_HEADS * IDX_DIM, IDX_DIM, IDX_HEADS)
ATT_IN_DIM = sum(ATT_SPLITS)

NUM_BUCKETS = 32
MAX_DISTANCE = 128

RET_HEADS = D_MODEL // 256
RET_KEY_DIM = D_MODEL // RET_HEADS
RET_VAL_DIM = 2 * RET_KEY_DIM
RET_CHUNK = 128
RET_IN_DIM = 2 * RET_HEADS * RET_KEY_DIM + 2 * RET_HEADS * RET_VAL_DIM

D_FF = 4 * D_MODEL

EPS = 1e-6

kernel_name = "dsa_retention_hybrid_step"

F32 = jnp.float32


def _rms(xf):
    return xf * lax.rsqrt(jnp.mean(xf * xf, axis=-1, keepdims=True) + EPS)


def rms_norm(x, g):
    return (_rms(x.astype(F32)) * g.astype(F32)).astype(x.dtype)


def t5_bucket(dist):
    n = jnp.maximum(dist, 0)
    max_exact = NUM_BUCKETS // 2
    nf = jnp.maximum(n, 1).astype(F32)
    large = max_exact + (jnp.log(nf / max_exact) / math.log(MAX_DISTANCE / max_exact)
                         * (NUM_BUCKETS - max_exact)).astype(jnp.int32)
    large = jnp.minimum(large, NUM_BUCKETS - 1)
    return jnp.where(n < max_exact, n, large)


def dsa_project(h, w_in, q_gain, k_gain):
    B, T, _ = h.shape
    z = h @ w_in
    q, k, v, qi, ki, wi = jnp.split(z, np.cumsum(ATT_SPLITS)[:-1].tolist(), axis=-1)
    q = rms_norm(q.reshape(B, T, ATT_HEADS, ATT_HEAD_DIM), q_gain)
    k = rms_norm(k.reshape(B, T, ATT_KV_HEADS, ATT_HEAD_DIM), k_gain)
    v = v.reshape(B, T, ATT_KV_HEADS, ATT_HEAD_DIM)
    qi = qi.reshape(B, T, IDX_HEADS, IDX_DIM)
    return q, k, v, qi, ki, wi


def dsa_select_attend(q, qi, wi, k, v, ki, q_pos, k_pos, topk, rel_bias):
    T = q.shape[0]
    causal = k_pos[None, :] <= q_pos[:, None]
    idx = jnp.einsum('thd,sd->ths', qi.astype(F32), ki.astype(F32))
    score = jnp.einsum('th,ths->ts', wi.astype(F32), jax.nn.relu(idx))
    score = jnp.where(causal, score, -jnp.inf)
    _, sel = lax.top_k(score, topk)
    ks = jnp.take(k, sel, axis=0).astype(F32)
    vs = jnp.take(v, sel, axis=0).astype(F32)
    sel_pos = jnp.take(k_pos, sel, axis=0)
    valid = sel_pos <= q_pos[:, None]
    qg = q.reshape(T, ATT_KV_HEADS, ATT_GROUP, ATT_HEAD_DIM).astype(F32)
    logits = jnp.einsum('tgrd,tkgd->tgrk', qg, ks) * (ATT_HEAD_DIM ** -0.5)
    bias = jnp.take(rel_bias, t5_bucket(q_pos[:, None] - sel_pos), axis=0)
    bias = bias.reshape(T, topk, ATT_KV_HEADS, ATT_GROUP).transpose(0, 2, 3, 1).astype(F32)
    logits = jnp.where(valid[:, None, None, :], logits + bias, -jnp.inf)
    p = jax.nn.softmax(logits, axis=-1)
    o = jnp.einsum('tgrk,tkgd->tgrd', p, vs)
    return o.reshape(T, ATT_HEADS, ATT_HEAD_DIM).astype(q.dtype)


def dsa_prompt(h, w_in, q_gain, k_gain, w_out, rel_bias):
    B, T, _ = h.shape
    q, k, v, qi, ki, wi = dsa_project(h, w_in, q_gain, k_gain)
    topk = min(TOPK_MAX, T // 4)
    nb = T // Q_BLOCK
    pos = jnp.arange(T, dtype=jnp.int32)
    blk = lambda a: a.reshape(B, nb, Q_BLOCK, *a.shape[2:])

    def per_seq(args):
        qb, qib, wib, kk, vv, kki = args

        def per_block(bargs):
            qq, qqi, ww, qp = bargs
            return dsa_select_attend(qq, qqi, ww, kk, vv, kki, qp, pos, topk, rel_bias)

        return lax.map(per_block, (qb, qib, wib, pos.reshape(nb, Q_BLOCK)))

    o = lax.map(per_seq, (blk(q), blk(qi), blk(wi), k, v, ki))
    y = o.reshape(B, T, ATT_HEADS * ATT_HEAD_DIM) @ w_out
    return y, k, v, ki


def dsa_sample(h, cache_k, cache_v, cache_kidx, page_table, w_in, q_gain, k_gain, w_out, rel_bias):
    DB, T, _ = h.shape
    q, k, v, qi, ki, wi = dsa_project(h, w_in, q_gain, k_gain)
    past_len = page_table.shape[1] * cache_k.shape[1]
    past_k = cache_k[page_table].reshape(DB, past_len, ATT_KV_HEADS, ATT_HEAD_DIM).astype(k.dtype)
    past_v = cache_v[page_table].reshape(DB, past_len, ATT_KV_HEADS, ATT_HEAD_DIM).astype(v.dtype)
    past_ki = cache_kidx[page_table].reshape(DB, past_len, IDX_DIM).astype(ki.dtype)
    k_all = jnp.concatenate([past_k, k], axis=1)
    v_all = jnp.concatenate([past_v, v], axis=1)
    ki_all = jnp.concatenate([past_ki, ki], axis=1)
    L = past_len + T
    topk = min(TOPK_MAX, L // 4)
    q_pos = past_len + jnp.arange(T, dtype=jnp.int32)
    k_pos = jnp.arange(L, dtype=jnp.int32)
    o = jax.vmap(lambda a, b, c, d, e, f: dsa_select_attend(a, b, c, d, e, f, q_pos, k_pos, topk, rel_bias))(
        q, qi, wi, k_all, v_all, ki_all)
    y = o.reshape(DB, T, ATT_HEADS * ATT_HEAD_DIM) @ w_out
    return y, k, v, ki


def rotate(x, pos):
    half = x.shape[-1] // 2
    theta = 1.0 / (10000.0 ** jnp.linspace(0.0, 1.0, half, dtype=F32))
    ang = pos.astype(F32)[:, None] * theta[None, :]
    cos = jnp.cos(ang)[None, :, None, :]
    sin = jnp.sin(ang)[None, :, None, :]
    xf = x.astype(F32)
    x1, x2 = xf[..., :half], xf[..., half:]
    return jnp.concatenate([x1 * cos - x2 * sin, x1 * sin + x2 * cos], axis=-1).astype(x.dtype)


def retention_chunked(q, k, v, state0):
    B, T = q.shape[:2]
    C = RET_CHUNK if T % RET_CHUNK == 0 else T
    nc = T // C
    log_g = jnp.log1p(-jnp.exp2(-5.0 - jnp.arange(RET_HEADS, dtype=F32)))
    i = jnp.arange(C, dtype=F32)
    diff = i[:, None] - i[None, :]
    decay_mask = jnp.where(diff >= 0, jnp.exp(log_g[:, None, None] * jnp.maximum(diff, 0.0)), 0.0)
    q_decay = jnp.exp(log_g[None, :] * (i[:, None] + 1.0))
    k_decay = jnp.exp(log_g[None, :] * (C - 1.0 - i[:, None]))
    chunk_decay = jnp.exp(log_g * C)
    to_chunks = lambda a: jnp.moveaxis(a.astype(F32).reshape(B, nc, C, *a.shape[2:]), 1, 0)

    def step(S, inp):
        qc, kc, vc = inp
        att = jnp.einsum('bihd,bjhd->bhij', qc, kc) * decay_mask
        o = (jnp.einsum('bhij,bjhe->bihe', att, vc)
             + jnp.einsum('bihd,bhde->bihe', qc * q_decay[None, :, :, None], S))
        S = (chunk_decay[None, :, None, None] * S
             + jnp.einsum('bjhd,bjhe->bhde', kc * k_decay[None, :, :, None], vc))
        return S, o

    S, o = lax.scan(step, state0.astype(F32), (to_chunks(q), to_chunks(k), to_chunks(v)))
    o = jnp.moveaxis(o, 0, 1).reshape(B, T, RET_HEADS, RET_VAL_DIM)
    return o, S


def retention_mixer(h, state0, pos, w_in, w_out):
    B, T, _ = h.shape
    z = h @ w_in
    qk = RET_HEADS * RET_KEY_DIM
    vd = RET_HEADS * RET_VAL_DIM
    q, k, v, g = jnp.split(z, [qk, 2 * qk, 2 * qk + vd], axis=-1)
    q = rotate(q.reshape(B, T, RET_HEADS, RET_KEY_DIM), pos)
    k = rotate(k.reshape(B, T, RET_HEADS, RET_KEY_DIM), pos) * (RET_KEY_DIM ** -0.5)
    v = v.reshape(B, T, RET_HEADS, RET_VAL_DIM)
    o, S = retention_chunked(q, k, v, state0)
    o = _rms(o) * jax.nn.silu(g.astype(F32)).reshape(B, T, RET_HEADS, RET_VAL_DIM)
    y = o.reshape(B, T, vd).astype(h.dtype) @ w_out
    return y, S


def sqrelu_mlp(h, w1, w2):
    return jnp.square(jax.nn.relu(h @ w1)) @ w2


def setup_inputs(seed: int = 0) -> dict:
    key = jax.random.key(seed)
    ks = jax.random.split(key, 20)
    n_pages = PAST_LEN // PAGE_SIZE
    n_used = DEC_BATCH * n_pages
    n_phys = n_used + n_used // 4
    nrm = lambda kk, shape, s: jax.random.normal(kk, shape, F32) * s
    page_table = jax.random.permutation(ks[0], n_phys)[:n_used].reshape(DEC_BATCH, n_pages).astype(jnp.int32)
    return {
        "x_prompt": nrm(ks[1], (BATCH, SEQ, D_MODEL), 1.0),
        "x_sample": nrm(ks[2], (DEC_BATCH, DEC_SEQ, D_MODEL), 1.0),
        "cache_k": nrm(ks[3], (N_ATTN_LAYERS, n_phys, PAGE_SIZE, ATT_KV_HEADS, ATT_HEAD_DIM), 1.0),
        "cache_v": nrm(ks[4], (N_ATTN_LAYERS, n_phys, PAGE_SIZE, ATT_KV_HEADS, ATT_HEAD_DIM), 1.0),
        "cache_kidx": nrm(ks[5], (N_ATTN_LAYERS, n_phys, PAGE_SIZE, IDX_DIM), 1.0),
        "state_ret": nrm(ks[6], (N_RET_LAYERS, DEC_BATCH, RET_HEADS, RET_KEY_DIM, RET_VAL_DIM), 0.1),
        "page_table": page_table,
        "rel_bias": nrm(ks[7], (NUM_BUCKETS, ATT_HEADS), 0.3),
        "ln_mix": 1.0 + nrm(ks[8], (DEPTH, D_MODEL), 0.01),
        "ln_mlp": 1.0 + nrm(ks[9], (DEPTH, D_MODEL), 0.01),
        "att_w_in": nrm(ks[10], (N_ATTN_LAYERS, D_MODEL, ATT_IN_DIM), D_MODEL ** -0.5),
        "att_q_gain": 1.0 + nrm(ks[11], (N_ATTN_LAYERS, ATT_HEAD_DIM), 0.01),
        "att_k_gain": 1.0 + nrm(ks[12], (N_ATTN_LAYERS, ATT_HEAD_DIM), 0.01),
        "att_w_out": nrm(ks[13], (N_ATTN_LAYERS, ATT_HEADS * ATT_HEAD_DIM, D_MODEL), (ATT_HEADS * ATT_HEAD_DIM) ** -0.5),
        "ret_w_in": nrm(ks[14], (N_RET_LAYERS, D_MODEL, RET_IN_DIM), D_MODEL ** -0.5),
        "ret_w_out": nrm(ks[15], (N_RET_LAYERS, RET_HEADS * RET_VAL_DIM, D_MODEL), (RET_HEADS * RET_VAL_DIM) ** -0.5),
        "mlp_w_in": nrm(ks[16], (DEPTH, D_MODEL, D_FF), D_MODEL ** -0.5),
        "mlp_w_out": nrm(ks[17], (DEPTH, D_FF, D_MODEL), D_FF ** -0.5),
    }


def reference(x_prompt, x_sample, cache_k, cache_v, cache_kidx, state_ret, page_table,
              rel_bias, ln_mix, ln_mlp, att_w_in, att_q_gain, att_k_gain, att_w_out,
              ret_w_in, ret_w_out, mlp_w_in, mlp_w_out):
    yp, ys = x_prompt, x_sample
    Bp, Tp, _ = x_prompt.shape
    Ts = x_sample.shape[1]
    past_len = page_table.shape[1] * cache_k.shape[2]
    pos_p = jnp.arange(Tp, dtype=jnp.int32)
    pos_s = past_len + jnp.arange(Ts, dtype=jnp.int32)
    kp_l, vp_l, kip_l, rp_l = [], [], [], []
    ks_l, vs_l, kis_l, rs_l = [], [], [], []
    for i in range(DEPTH):
        j = i // N_MIXERS
        hp = rms_norm(yp, ln_mix[i])
        hs = rms_norm(ys, ln_mix[i])
        if i % N_MIXERS == 0:
            mp, kp, vp, kip = dsa_prompt(hp, att_w_in[j], att_q_gain[j], att_k_gain[j], att_w_out[j], rel_bias)
            ms, kk, vv, kki = dsa_sample(hs, cache_k[j], cache_v[j], cache_kidx[j], page_table,
                                         att_w_in[j], att_q_gain[j], att_k_gain[j], att_w_out[j], rel_bias)
            kp_l.append(kp); vp_l.append(vp); kip_l.append(kip)
            ks_l.append(kk); vs_l.append(vv); kis_l.append(kki)
        else:
            zero_state = jnp.zeros((Bp, RET_HEADS, RET_KEY_DIM, RET_VAL_DIM), F32)
            mp, Sp = retention_mixer(hp, zero_state, pos_p, ret_w_in[j], ret_w_out[j])
            ms, Ss = retention_mixer(hs, state_ret[j], pos_s, ret_w_in[j], ret_w_out[j])
            rp_l.append(Sp.astype(x_prompt.dtype)); rs_l.append(Ss.astype(state_ret.dtype))
        yp = yp + mp
        ys = ys + ms
        yp = yp + sqrelu_mlp(rms_norm(yp, ln_mlp[i]), mlp_w_in[i], mlp_w_out[i])
        ys = ys + sqrelu_mlp(rms_norm(ys, ln_mlp[i]), mlp_w_in[i], mlp_w_out[i])
    k_prompt = jnp.stack(kp_l)
    v_prompt = jnp.stack(vp_l)
    kidx_prompt = jnp.stack(kip_l)
    ret_prompt = jnp.stack(rp_l)
    k_sample = jnp.stack(ks_l)
    v_sample = jnp.stack(vs_l)
    kidx_sample = jnp.stack(kis_l)
    ret_sample = jnp.stack(rs_l)
    return (yp, ys, k_prompt, v_prompt, kidx_prompt, ret_prompt, k_sample, v_sample, kidx_sample, ret_sample)
```

```python
import math
from contextlib import ExitStack
import numpy as np
import concourse.bass as bass
import concourse.mybir as mybir
from concourse.bass_utils import run_bass_kernel_spmd

F32 = mybir.dt.float32
BF16 = mybir.dt.bfloat16
I32 = mybir.dt.int32
ALU = mybir.AluOpType
AF = mybir.ActivationFunctionType
AX = mybir.AxisListType

NCORES = 8
D = 1024
SEQ = 2048
NS = 64
NSEQ = 16
T = SEQ + NS
NPHYS = 2560
EPS = 1e-6
NEG = -30000.0
BIGNEG = -1.0e30
NBIS = 24
TT = [(0, 512), (512, 512), (1024, 512), (1536, 512), (2048, 64)]
ATT_IN = 1860
GAM = [1.0 - 2.0 ** (-5.0 - h) for h in range(4)]


def _t5_bucket(dist):
    n = np.maximum(dist, 0)
    nf = np.maximum(n, 1).astype(np.float32)
    large = 16 + (np.log(nf / np.float32(16)) / np.float32(math.log(128 / 16)) * np.float32(16)).astype(np.int32)
    large = np.minimum(large, 31)
    return np.where(n < 16, n, large)


def _build_consts():
    c = {}
    p = np.arange(128)
    c["ident"] = np.eye(128, dtype=np.float32)
    t = p[:, None]
    s = p[None, :]
    c["causneg"] = np.where(s <= t, 0.0, BIGNEG).astype(np.float32)
    delta = np.arange(256)[None, :] - p[:, None]
    c["bkt_p"] = np.where(delta >= 0, _t5_bucket(delta), 31).astype(np.float32)
    pos = (128 * (p // 8) + 16 * (p % 8))[:, None, None] + np.arange(16)[None, :, None]
    dl = 2048 + np.arange(4)[None, None, :] - pos
    c["bkt_s"] = _t5_bucket(dl).astype(np.float32).reshape(128, 64)
    bp = np.arange(64) // 4
    tp = np.arange(64) % 4
    valid = (bp[:, None, None] == np.arange(16)[None, :, None]) & (tp[:, None, None] <= np.arange(4)[None, None, :])
    c["newvalid"] = np.where(valid, 0.0, BIGNEG).astype(np.float32).reshape(64, 64)
    c["newvalid"] = np.concatenate([c["newvalid"], np.zeros((64, 64), np.float32)], 0)
    bn = np.clip(np.arange(4)[None, None, :] - tp[:, None, None], 0, 31) * np.ones((1, 16, 1))
    c["bkt_n"] = np.concatenate([bn.reshape(64, 64).astype(np.float32), np.full((64, 64), 31, np.float32)], 0)
    i = np.arange(128, dtype=np.float64)
    dm = np.zeros((128, 4, 128), np.float64)
    qd = np.zeros((128, 4, 128), np.float64)
    kd = np.zeros((128, 4), np.float64)
    dmS = np.zeros((128, 4, 64), np.float64)
    qdS = np.zeros((128, 4, 64), np.float64)
    kdS = np.zeros((128, 4), np.float64)
    for h in range(4):
        lg = np.log1p(-np.exp2(-5.0 - h))
        diff = i[None, :] - i[:, None]
        dm[:, h, :] = np.where(diff >= 0, np.exp(lg * np.maximum(diff, 0)), 0.0)
        qd[:, h, :] = np.exp(lg * (i + 1.0))[None, :]
        kd[:, h] = np.exp(lg * (127.0 - i))
        ii = np.arange(64)
        dS = (ii % 4)[None, :] - (ii % 4)[:, None]
        same = (ii // 4)[None, :] == (ii // 4)[:, None]
        dmS[:64, h, :] = np.where(same & (dS >= 0), np.exp(lg * np.maximum(dS, 0)), 0.0)
        qdS[:, h, :] = np.exp(lg * ((ii % 4) + 1.0))[None, :]
        kdS[:64, h] = np.exp(lg * (3.0 - (ii % 4)))
    c["dm"] = dm.reshape(128, 512).astype(np.float32)
    c["qd"] = qd.reshape(128, 512).astype(np.float32)
    c["kd"] = kd.astype(np.float32)
    c["dmS"] = dmS.reshape(128, 256).astype(np.float32)
    c["qdS"] = qdS.reshape(128, 256).astype(np.float32)
    c["kdS"] = kdS.astype(np.float32)
    bm = np.zeros((128, 16), np.float32)
    bm[np.arange(64), np.arange(64) // 4] = 1.0
    c["bm"] = bm
    bmc = np.zeros((128, 16, 64), np.float32)
    for b in range(16):
        bmc[:, b, 4 * b:4 * b + 4] = 1.0
    c["bmc"] = bmc.reshape(128, 1024)
    c["iota_p"] = np.arange(128, dtype=np.float32).reshape(128, 1)
    c["pofs"] = (16 * (np.arange(128) % 8)).astype(np.float32).reshape(128, 1)
    sel = np.zeros((128, 16), np.float32)
    sel[np.arange(128), np.arange(128) // 8] = 1.0
    c["pagesel"] = sel
    dd = 255 - np.arange(383)
    bk_d = np.where(dd >= 0, _t5_bucket(dd), 31)
    gr = np.zeros((128, 383), np.float32)
    gr[bk_d, np.arange(383)] = 1.0
    c["gr"] = gr
    c["bkt_all"] = np.concatenate([c.pop("bkt_p"), c.pop("bkt_s"), c.pop("bkt_n")], axis=1)
    c["pow2"] = np.tile((2.0 ** -(np.arange(NBIS) + 1.0)).astype(np.float32)[None, :], (128, 1))
    rkeys = ("dm", "qd", "kd", "dmS", "qdS", "kdS", "bm", "bmc", "gr")
    outs = []
    for keys in ([k for k in c if k not in rkeys], list(rkeys)):
        offs = {}
        cols = []
        o = 0
        for k in keys:
            v = c[k]
            offs[k] = (o, v.shape[1])
            cols.append(v)
            o += v.shape[1]
        outs.append((np.ascontiguousarray(np.concatenate(cols, axis=1)), offs))
    return outs


def _build_rot():
    half = 128
    theta = (1.0 / (10000.0 ** np.linspace(0.0, 1.0, half, dtype=np.float32))).astype(np.float32)
    pos = np.concatenate([np.arange(SEQ), np.tile(2048 + np.arange(4), NSEQ)]).astype(np.float32)
    ang = (theta[:, None] * pos[None, :]).astype(np.float32)
    return np.ascontiguousarray(np.stack([np.cos(ang), np.sin(ang)], axis=1).astype(np.float32))


class Buf:
    def __init__(self, t, name, fence):
        self.t = t
        self.name = name
        self.lw = None
        self.rd = dict(fence)
        self.dsem = None
        self.dcnt = 0
        self.excl = False

    def __getitem__(self, k):
        return self.t[k]


class Eng:
    def __init__(self, h, sem, key, is_pe=False):
        self.h = h
        self.sem = sem
        self.key = key
        self.cnt = 0
        self.seen = {}
        self.is_pe = is_pe


class KB:
    def __init__(self, nc):
        self.nc = nc
        self.es = ExitStack()
        self.fence = {}
        self.dma_bufs = []
        self.nsem = 0
        self.uid = 0
        self.dpool = []
        self.dsems = []
        mk = lambda n: self.es.enter_context(nc.semaphore(n))
        self.PE = Eng(nc.tensor, mk("s_pe"), "pe", True)
        self.ACT = Eng(nc.scalar, mk("s_act"), "act")
        self.DVE = Eng(nc.vector, mk("s_dve"), "dve")
        self.POOL = Eng(nc.gpsimd, mk("s_pool"), "pool")
        self.SP = Eng(nc.sync, mk("s_sp"), "sp")

    def sb(self, st, name, shape, dt):
        self.uid += 1
        t = st.enter_context(self.nc.sbuf_tensor("%s_%d" % (name, self.uid), list(shape), dt))
        b = Buf(t, name, self.fence)
        st.callback(self._free, b)
        return b

    def _free(self, b):
        for tk in ([b.lw] if b.lw else []) + list(b.rd.items()):
            if isinstance(tk, tuple) and len(tk) == 2 and isinstance(tk[1], tuple):
                key, (sem, val) = tk
            else:
                key, sem, val = tk
            if self.fence.get(key, (None, 0))[1] < val:
                self.fence[key] = (sem, val)
        if b.dsem is not None:
            self.dpool.extend(b.dsem.values())
            b.dsem = None

    def psum(self, name):
        t = self.es.enter_context(self.nc.psum_tensor(name, [128, 512], F32))
        b = Buf(t, name, {})
        b.excl = True
        return b

    def _deps(self, eng, reads, writes):
        deps = {}

        def add(key, sem, val, war):
            if key == eng.key and eng.is_pe:
                return
            if deps.get(key, (None, 0))[1] < val:
                deps[key] = (sem, val)

        for b in reads:
            if b.lw:
                add(b.lw[0], b.lw[1], b.lw[2], False)
            if b.excl:
                for key, (sem, val) in b.rd.items():
                    if key != eng.key:
                        add(key, sem, val, True)
        for b in writes:
            if b.lw:
                add(b.lw[0], b.lw[1], b.lw[2], False)
            for key, (sem, val) in b.rd.items():
                add(key, sem, val, True)
        for key, (sem, val) in deps.items():
            if eng.seen.get(key, 0) < val:
                eng.h.wait_ge(sem, val)
                eng.seen[key] = val

    def _commit(self, tk, reads, writes):
        key, sem, val = tk
        for b in reads:
            if b.rd.get(key, (None, 0))[1] < val:
                b.rd[key] = (sem, val)
        for b in writes:
            b.lw = tk
            b.rd = {}

    def op(self, eng, fn, reads=(), writes=()):
        self._deps(eng, reads, writes)
        inst = fn()
        eng.cnt += 1
        inst.then_inc(eng.sem, 1)
        self._commit((eng.key, eng.sem, eng.cnt), reads, writes)

    def mm(self, out_buf, out_ap, pairs, reads, start=True, stop=True):
        eng = self.PE
        self._deps(eng, reads, [out_buf])
        n = len(pairs)
        inst = None
        for i, (l, r) in enumerate(pairs):
            inst = self.nc.tensor.matmul(out_ap, lhsT=l, rhs=r, start=(start and i == 0), stop=(stop and i == n - 1))
        eng.cnt += 1
        inst.then_inc(eng.sem, 1)
        self._commit((eng.key, eng.sem, eng.cnt), reads, [out_buf])

    def transposes(self, out_buf, items, reads, ident):
        eng = self.PE
        self._deps(eng, list(reads) + [ident], [out_buf])
        inst = None
        for (o, i) in items:
            inst = self.nc.tensor.transpose(o, i, ident.t[0:i.shape[0], 0:i.shape[0]])
        eng.cnt += 1
        inst.then_inc(eng.sem, 1)
        self._commit((eng.key, eng.sem, eng.cnt), list(reads) + [ident], [out_buf])

    def dma(self, q, out_ap, in_ap, reads=(), writes=(), indirect=None):
        self._deps(q, reads, writes)
        b = (list(writes) + list(reads))[0]
        if b.dsem is None:
            b.dsem = {}
        if q.key not in b.dsem:
            pool = [d for d in self.dpool if d[3] == q.key]
            if pool:
                d0 = pool[-1]
                b.dsem[q.key] = d0
                self.dpool.remove(d0)
                if q.seen.get(d0[1], 0) < d0[2]:
                    q.h.wait_ge(d0[0], d0[2])
                    q.seen[d0[1]] = d0[2]
            else:
                self.nsem += 1
                ds = [self.es.enter_context(self.nc.semaphore("dsem%d" % self.nsem)), "dsem%d" % self.nsem, 0, q.key]
                self.dsems.append(ds)
                b.dsem[q.key] = ds
        ds = b.dsem[q.key]
        ds[2] += 16
        if indirect is not None:
            inst = q.h.indirect_dma_start(out=out_ap, out_offset=None, in_=in_ap, in_offset=indirect)
        else:
            inst = q.h.dma_start(out=out_ap, in_=in_ap)
        inst.then_inc(ds[0], 16)
        self._commit((ds[1], ds[0], ds[2]), reads, writes)

    def finish(self):
        for ds in self.dsems:
            if self.SP.seen.get(ds[1], 0) < ds[2]:
                self.nc.sync.wait_ge(ds[0], ds[2])
                self.SP.seen[ds[1]] = ds[2]
        for e in (self.PE, self.ACT, self.DVE, self.POOL):
            if e.cnt > 0:
                self.nc.sync.wait_ge(e.sem, e.cnt)


def build_program(level=99):
    nc = bass.Bass("TRN2", target_bir_lowering=False)
    (cst_np, CO), (cstr_np, COR) = _build_consts()
    NCST = cst_np.shape[1]
    dr = lambda n, s, dt=F32, kind="ExternalInput": nc.dram_tensor(n, list(s), dt, kind=kind).ap()
    x_p = dr("x_p", [SEQ, D])
    x_s = dr("x_s", [NS, D])
    cache_k = dr("cache_k", [NPHYS * 128, 256])
    cache_v = dr("cache_v", [NPHYS * 128, 256])
    cache_i = dr("cache_i", [NPHYS * 128, 64])
    st_in = dr("st_in", [NSEQ, 4, 256, 512])
    ptab = dr("ptab", [1, NSEQ * 16], I32)
    relb = dr("relb", [1, 256])
    gains = dr("gains", [128, 32])
    qkg = dr("qkg", [128, 2])
    kg_row = dr("kg_row", [1, 128])
    w_att_in = dr("w_att_in", [D, ATT_IN])
    w_att_out = dr("w_att_out", [D, D])
    w_ret_in = dr("w_ret_in", [D, 6144])
    w_ret_out = dr("w_ret_out", [2048, D])
    w_mlp_in = dr("w_mlp_in", [2, D, 4096])
    w_mlp_out = dr("w_mlp_out", [2, 4096, D])
    cst = dr("cst", [128, NCST])
    cstr = dr("cstr", [128, cstr_np.shape[1]])
    rot = dr("rot", [128, 2, T])
    OUT = "ExternalOutput"
    y_p = dr("y_p", [SEQ, D], kind=OUT)
    y_s = dr("y_s", [NS, D], kind=OUT)
    k_p = dr("k_p", [SEQ, 256], kind=OUT)
    v_p = dr("v_p", [SEQ, 256], kind=OUT)
    i_p = dr("i_p", [SEQ, 64], kind=OUT)
    r_p = dr("r_p", [4, 256, 512], kind=OUT)
    k_s = dr("k_s", [NS, 256], kind=OUT)
    v_s = dr("v_s", [NS, 256], kind=OUT)
    i_s = dr("i_s", [NS, 64], kind=OUT)
    r_s = dr("r_s", [NSEQ, 4, 256, 512], kind=OUT)

    DBG = {}
    if DEBUG:
        DBG["I"] = dr("dbgI", [128, 32], I32, kind=OUT)
        DBG["A"] = dr("dbgA", [128, 1024], kind=OUT)
        DBG["L"] = dr("dbgL", [128, 64], kind=OUT)
        DBG["N"] = dr("dbgN", [128, 64], kind=OUT)
        DBG["D"] = dr("dbgD", [128, 32], kind=OUT)
        DBG["W"] = dr("dbgW", [128, 256], kind=OUT)
        DBG["G"] = dr("dbgG", [128, 1024], kind=OUT)
        DBG["B"] = dr("dbgB", [128, 2048], BF16, kind=OUT)
        DBG["T"] = dr("dbgT", [128, 2048], BF16, kind=OUT)
        DBG["M"] = dr("dbgM", [128, 256], kind=OUT)
    kb = KB(nc)
    kb.DBG = DBG
    PE, ACT, DVE, POOL, SP = kb.PE, kb.ACT, kb.DVE, kb.POOL, kb.SP
    V = nc.vector
    S = nc.scalar
    G = nc.gpsimd
    PS = [kb.psum("ps%d" % i) for i in range(8)]
    top = kb.es

    def C(name, rows=128):
        o, w = CO[name]
        return CST.t[0:rows, o:o + w]

    CST = kb.sb(top, "cst", [128, NCST], F32)
    kb.dma(SP, CST.t[:], cst, writes=[CST])
    xT = kb.sb(top, "xT", [128, 8, T], F32)
    identb = kb.sb(top, "identb", [128, 128], BF16)
    onesb = kb.sb(top, "onesb", [128, 128], BF16)
    ones_d = kb.sb(top, "ones_d", [128, 128], BF16)
    ones_h = kb.sb(top, "ones_h", [128, 128], BF16)
    gn = kb.sb(top, "gn", [128, 32], F32)
    qk_g = kb.sb(top, "qk_g", [128, 2], F32)
    kgbc = kb.sb(top, "kgbc", [128, 128], F32)
    kb.dma(SP, gn.t[:], gains, writes=[gn])
    kb.dma(SP, qk_g.t[:], qkg, writes=[qk_g])
    kb.dma(SP, kgbc.t[:], kg_row.partition_broadcast(128), writes=[kgbc])
    kb.op(DVE, lambda: V.tensor_copy(out=identb.t[:], in_=C("ident")), reads=[CST], writes=[identb])
    kb.op(DVE, lambda: V.memset(onesb.t[:], 1.0), writes=[onesb])
    kb.op(DVE, lambda: V.memset(ones_d.t[:], 1.0 / 1024), writes=[ones_d])
    kb.op(DVE, lambda: V.memset(ones_h.t[:], 1.0 / 128), writes=[ones_h])
    kb.op(DVE, lambda: V.tensor_scalar(out=qk_g.t[:, 0:1], in0=qk_g.t[:, 0:1], scalar1=128.0 ** -0.5, scalar2=None,
                                       op0=ALU.mult), reads=[qk_g], writes=[qk_g])

    class IdentF:
        pass
    identf = Buf(None, "identf", {})
    identf.t = CST.t[:, CO["ident"][0]:CO["ident"][0] + 128]
    identf_dep = CST

    rr = [0]

    def evac_engine():
        rr[0] ^= 1
        return ACT if rr[0] else DVE

    def copy(eng, out_ap, in_ap, reads, writes):
        if eng is ACT:
            kb.op(ACT, lambda: S.copy(out=out_ap, in_=in_ap), reads=reads, writes=writes)
        else:
            kb.op(DVE, lambda: V.tensor_copy(out=out_ap, in_=in_ap), reads=reads, writes=writes)

    with ExitStack() as st:
        xs = [kb.sb(st, "xs%d" % i, [128, D], F32) for i in range(4)]
        for c in range(17):
            n = 128 if c < 16 else NS
            src = x_p[c * 128:(c + 1) * 128, :] if c < 16 else x_s[:, :]
            xb = xs[c % 4]
            kb.dma(SP, xb.t[0:n, :], src, writes=[xb])
            for g in range(2):
                pb = PS[(2 * c + g) % 8]
                items = [(pb.t[:, j * 128:j * 128 + n], xb.t[0:n, (4 * g + j) * 128:(4 * g + j + 1) * 128]) for j in range(4)]
                eng = kb.PE
                kb._deps(eng, [xb, CST], [pb])
                inst = None
                for (o, i_) in items:
                    inst = nc.tensor.transpose(o, i_, identf.t[0:n, 0:n])
                eng.cnt += 1
                inst.then_inc(eng.sem, 1)
                kb._commit((eng.key, eng.sem, eng.cnt), [xb, CST], [pb])
                e = evac_engine()
                copy(e, xT.t[:, 4 * g:4 * g + 4, c * 128:c * 128 + n],
                     pb.t[:].rearrange("p (j t) -> p j t", j=4)[:, :, 0:n], [pb], [xT])

    def rmsnorm_tile(hbuf, h_ap, t0, n, gcol, sqb, rsb):
        kb.op(ACT, lambda: S.activation(out=sqb.t[:, :, 0:n], in_=xT.t[:, :, t0:t0 + n], func=AF.Square),
              reads=[xT], writes=[sqb])
        pb = PS[7]
        kb.mm(pb, pb.t[:, 0:n], [(ones_d.t[:], sqb.t[:, kc, 0:n]) for kc in range(8)], [ones_d, sqb])
        kb.op(ACT, lambda: S.activation(out=rsb.t[:, 0:n], in_=pb.t[:, 0:n], func=AF.Sqrt, bias=EPSB.t[:, 0:1], scale=1.0),
              reads=[pb, EPSB], writes=[rsb])
        kb.op(DVE, lambda: V.reciprocal(out=rsb.t[:, 0:n], in_=rsb.t[:, 0:n]), reads=[rsb], writes=[rsb])
        for kc in range(8):
            kb.op(DVE, lambda kc=kc: V.scalar_tensor_tensor(out=h_ap[:, kc, :], in0=xT.t[:, kc, t0:t0 + n],
                                                           scalar=gn.t[:, gcol + kc:gcol + kc + 1], in1=rsb.t[:, 0:n],
                                                           op0=ALU.mult, op1=ALU.mult),
                  reads=[xT, gn, rsb], writes=[hbuf])

    EPSB = kb.sb(top, "epsb", [128, 1], F32)
    kb.op(DVE, lambda: V.memset(EPSB.t[:], EPS), writes=[EPSB])

    def load_w(q, wbuf, w_ap_dst, src_ap):
        kb.dma(q, w_ap_dst, src_ap, writes=[wbuf])

    def add_resid(oc, t0, n, pb):
        kb.op(DVE, lambda: V.tensor_tensor(out=xT.t[:, oc, t0:t0 + n], in0=xT.t[:, oc, t0:t0 + n], in1=pb.t[:, 0:n],
                                           op=ALU.add), reads=[xT, pb], writes=[xT])

    with ExitStack() as L0:
        Win = kb.sb(L0, "Win", [128, 8, ATT_IN + 128], BF16)
        Wo = kb.sb(L0, "Wo", [128, 8, D], BF16)
        wsrc = w_att_in.rearrange("(kc p) n -> p kc n", p=128)
        for (a, b_) in [(1024, 1536), (1536, ATT_IN), (0, 512), (512, 1024)]:
            kb.dma(POOL, Win.t[:, :, a:b_], wsrc[:, :, a:b_], writes=[Win])
        kb.dma(POOL, Win.t[:, :, ATT_IN:ATT_IN + 64], wsrc[:, :, 1792:1856], writes=[Win])
        kb.dma(POOL, Win.t[:, :, ATT_IN + 64:ATT_IN + 128], wsrc[:, :, 1792:1856], writes=[Win])
        kb.dma(POOL, Wo.t[:], w_att_out.rearrange("(kc p) n -> p kc n", p=128), writes=[Wo])

        with ExitStack() as LK:
            kT = kb.sb(LK, "kT", [128, 2, T], BF16)
            Vall = kb.sb(LK, "Vall", [128, 17, 256], BF16)
            kiT = kb.sb(LK, "kiT", [128, T], BF16)
            WIa = kb.sb(LK, "WIa", [128, 17, 4], F32)
            WIs = kb.sb(LK, "WIs", [128, 17, 4], F32)
            biasN = kb.sb(LK, "biasN", [128, 2, 8, 128], BF16)
            biasS = kb.sb(LK, "biasS", [128, 16, 8, 4], F32)
            biasNn = kb.sb(LK, "biasNn", [128, 8, 64], F32)
            with ExitStack() as st:
                rb = kb.sb(st, "rb", [128, 256], F32)
                rbd = kb.sb(st, "rbd", [128, 256], F32)
                oh2 = kb.sb(st, "oh2", [128, 128], F32)
                tmp2 = kb.sb(st, "tmp2", [128, 8, 128], F32)
                bacc2 = kb.sb(st, "bacc2", [128, 8, 128], F32)
                rb32 = kb.sb(st, "rb32", [32, 8], F32)
                r31 = kb.sb(st, "r31", [32, 8], F32)
                rbdb = kb.sb(st, "rbdb", [32, 8], BF16)
                grb = kb.sb(st, "grb", [32, 383], BF16)
                kb.dma(SP, rb.t[:], relb.partition_broadcast(128), writes=[rb])
                kb.dma(SP, rb32.t[:], relb.rearrange("o (k h) -> (o k) h", h=8), writes=[rb32])
                kb.dma(SP, r31.t[:], relb[:, 248:256].partition_broadcast(32), writes=[r31])
                kb.op(DVE, lambda: V.tensor_tensor(out=rbdb.t[:], in0=rb32.t[:], in1=r31.t[:], op=ALU.subtract), reads=[rb32, r31], writes=[rbdb])
                grf = kb.sb(st, "grf", [32, 383], F32)
                kb.dma(SP, grf.t[:], cstr[0:32, COR["gr"][0]:COR["gr"][0] + 383], writes=[grf])
                kb.op(DVE, lambda: V.tensor_copy(out=grb.t[:], in_=grf.t[:]), reads=[grf], writes=[grb])
                for bank in range(4):
                    pbk = PS[bank]
                    items = []
                    for tl in range(64):
                        tp_ = bank * 64 + tl
                        items.append((pbk.t[:, tl * 8:tl * 8 + 8], grb.t[0:32, 255 - tp_:255 - tp_ + 128], rbdb.t[0:32, :]))
                    mm_multi(kb, nc, pbk, items, [grb, rbdb])
                    bb_, th = bank // 2, (bank % 2) * 64
                    copy_any(kb, nc, bank % 2, biasN.t[:, bb_, :, th:th + 64], pbk.t[:, 0:512].rearrange("p (t h) -> p h t", h=8), [pbk], [biasN])
                kb.op(POOL, lambda: G.tensor_tensor(out=rbd.t[:].rearrange("p (k h) -> p k h", h=8),
                                                    in0=rb.t[:].rearrange("p (k h) -> p k h", h=8),
                                                    in1=rb.t[:, 248:256].unsqueeze(1).to_broadcast([128, 32, 8]),
                                                    op=ALU.subtract), reads=[rb], writes=[rbd])
                kb.op(POOL, lambda: G.memset(bacc2.t[:], 0.0), writes=[bacc2])
                bk = C("bkt_all")
                for k in range(31):
                    kb.op(POOL, lambda k=k: G.tensor_single_scalar(out=oh2.t[:], in_=bk[:, 256:384], scalar=float(k), op=ALU.is_equal),
                          reads=[CST], writes=[oh2])
                    kb.op(POOL, lambda k=k: G.tensor_tensor(out=tmp2.t[:], in0=oh2.t[:].unsqueeze(1).to_broadcast([128, 8, 128]),
                                                            in1=rbd.t[:, k * 8:k * 8 + 8].unsqueeze(2).to_broadcast([128, 8, 128]), op=ALU.mult),
                          reads=[oh2, rbd], writes=[tmp2])
                    kb.op(POOL, lambda: G.tensor_tensor(out=bacc2.t[:], in0=bacc2.t[:], in1=tmp2.t[:], op=ALU.add),
                          reads=[bacc2, tmp2], writes=[bacc2])
                kb.op(POOL, lambda: G.tensor_copy(out=biasS.t[:], in_=bacc2.t[:, :, 0:64].rearrange("p h (c t) -> p c h t", t=4)),
                      reads=[bacc2], writes=[biasS])
                kb.op(POOL, lambda: G.tensor_copy(out=biasNn.t[:], in_=bacc2.t[:, :, 64:128]), reads=[bacc2], writes=[biasNn])

            for ti, (t0, n) in enumerate(TT):
                if level < 1:
                    break
                is_s = (ti == 4)
                with ExitStack() as TS:
                    qT = kb.sb(TS, "qT", [128, 8, 512], BF16)
                    qiT = kb.sb(TS, "qiT", [128, 2, 512], BF16)
                    WB = kb.sb(TS, "WB", [128, 16, 16], F32)
                    onT = kb.sb(TS, "onT", [128, 8, 512], BF16)
                    HS = ExitStack()
                    hT = kb.sb(HS, "hT", [128, 8, 512], BF16)
                    with ExitStack() as st:
                        sqb = kb.sb(st, "sqb", [128, 8, 512], BF16)
                        rsb = kb.sb(st, "rsb", [128, 512], F32)
                        rmsnorm_tile(hT, hT.t[:, :, 0:n], t0, n, 0, sqb, rsb)
                    with ExitStack() as st:
                        SQ = [kb.sb(st, "sq", [128, 512], BF16) for _ in range(2)]
                        QR = [kb.sb(st, "qraw", [128, 512], F32) for _ in range(2)]
                        RS = [kb.sb(st, "rs2", [128, 512], F32) for _ in range(2)]

                        def qs1(h):
                            pa = PS[h % 2]
                            sq, qraw = SQ[h % 2], QR[h % 2]
                            kb.mm(pa, pa.t[:, 0:n], [(Win.t[:, kc, h * 128:(h + 1) * 128], hT.t[:, kc, 0:n]) for kc in range(8)],
                                  [Win, hT])
                            kb.op(ACT, lambda: S.activation(out=sq.t[:, 0:n], in_=pa.t[:, 0:n], func=AF.Square),
                                  reads=[pa], writes=[sq])
                            kb.op(DVE, lambda: V.tensor_copy(out=qraw.t[:, 0:n], in_=pa.t[:, 0:n]), reads=[pa], writes=[qraw])

                        def qs2(h):
                            pb2 = PS[2 + h % 2]
                            sq, qraw, rs2 = SQ[h % 2], QR[h % 2], RS[h % 2]
                            kb.mm(pb2, pb2.t[:, 0:n], [(ones_h.t[:], sq.t[:, 0:n])], [ones_h, sq])
                            kb.op(ACT, lambda: S.activation(out=rs2.t[:, 0:n], in_=pb2.t[:, 0:n], func=AF.Sqrt,
                                                            bias=EPSB.t[:, 0:1], scale=1.0), reads=[pb2, EPSB], writes=[rs2])
                            kb.op(DVE, lambda: V.reciprocal(out=rs2.t[:, 0:n], in_=rs2.t[:, 0:n]), reads=[rs2], writes=[rs2])
                            kb.op(DVE, lambda: V.scalar_tensor_tensor(out=qT.t[:, h, 0:n], in0=qraw.t[:, 0:n],
                                                                      scalar=qk_g.t[:, 0:1], in1=rs2.t[:, 0:n],
                                                                      op0=ALU.mult, op1=ALU.mult),
                                  reads=[qraw, qk_g, rs2], writes=[qT])

                        qs1(0)
                        for h in range(8):
                            if h + 1 < 8:
                                qs1(h + 1)
                            qs2(h)
                        for j in range(3):
                            pa = PS[4 + j % 2]
                            c0 = 1536 + j * 128 if j < 2 else ATT_IN
                            kb.mm(pa, pa.t[:, 0:n], [(Win.t[:, kc, c0:c0 + 128], hT.t[:, kc, 0:n]) for kc in range(8)], [Win, hT])
                            if j < 2:
                                copy(evac_engine(), qiT.t[:, j, 0:n], pa.t[:, 0:n], [pa], [qiT])
                            else:
                                copy(evac_engine(), kiT.t[:, t0:t0 + n], pa.t[:, 0:n], [pa], [kiT])
                    if is_s and level >= 6:
                        with ExitStack() as st:
                            Wrep = kb.sb(st, "Wrep", [128, 8, 4, 128], BF16)
                            kb.op(DVE, lambda: V.tensor_copy(out=Wrep.t[:], in_=Win.t[:, :, 1856:1860].unsqueeze(3).to_broadcast([128, 8, 4, 128])),
                                  reads=[Win], writes=[Wrep])
                            pw = PS[6]
                            for h in range(4):
                                xs_ = (h % 2) * 2 + h // 2
                                kb.mm(pw, pw.t[:, xs_ * 64:(xs_ + 1) * 64], [(Wrep.t[:, kc, h, :], hT.t[:, kc, 0:NS]) for kc in range(8)], [Wrep, hT])
                            kb.op(DVE, lambda: V.tensor_copy(out=WB.t[:].rearrange("p b (x t) -> p b x t", x=4), in_=pw.t[:, 0:256].rearrange("p (x b t) -> p b x t", x=4, b=16)), reads=[pw], writes=[WB])
                    with ExitStack() as st:
                        ko = [kb.sb(st, "ko%d" % i, [128, 256], F32) for i in range(2)]
                        vo = [kb.sb(st, "vo%d" % i, [128, 256], F32) for i in range(2)]
                        io = [kb.sb(st, "io%d" % i, [128, 64], F32) for i in range(2)]
                        KBF = [kb.sb(st, "kbf", [128, 256], BF16) for _ in range(2)]
                        SSQ = [kb.sb(st, "ssq", [128, 2], F32) for _ in range(2)]
                        junk = kb.sb(st, "junk", [128, 128], F32)
                        nchunk = 4 if not is_s else 1
                        def ck1(cc):
                            kbf, ssq = KBF[cc % 2], SSQ[cc % 2]
                            cn = 128 if not is_s else NS
                            ci = ti * 4 + cc
                            cs = cc * 128
                            pa = PS[cc % 2]
                            pb2 = PS[2 + cc % 2]
                            kb.mm(pa, pa.t[0:cn, :], [(hT.t[:, kc, cs:cs + cn], Win.t[:, kc, 1024:1536]) for kc in range(8)], [Win, hT])
                            kb.mm(pb2, pb2.t[0:cn, 0:68], [(hT.t[:, kc, cs:cs + cn], Win.t[:, kc, 1792:1860]) for kc in range(8)], [Win, hT])
                            kob, vob, iob = ko[cc % 2], vo[cc % 2], io[cc % 2]
                            for g in range(2):
                                kb.op(ACT, lambda g=g: S.activation(out=junk.t[0:cn, :], in_=pa.t[0:cn, g * 128:(g + 1) * 128],
                                                                     func=AF.Square, accum_out=ssq.t[0:cn, g:g + 1]),
                                      reads=[pa], writes=[junk, ssq])
                            kb.op(ACT, lambda: S.activation(out=ssq.t[0:cn, :], in_=ssq.t[0:cn, :], func=AF.Sqrt,
                                                            bias=EPSB.t[0:cn, 0:1], scale=1.0 / 128), reads=[ssq, EPSB], writes=[ssq])
                            kb.op(DVE, lambda: V.reciprocal(out=ssq.t[0:cn, :], in_=ssq.t[0:cn, :]), reads=[ssq], writes=[ssq])
                            for g in range(2):
                                kb.op(DVE, lambda g=g: V.scalar_tensor_tensor(
                                    out=kob.t[0:cn, g * 128:(g + 1) * 128], in0=pa.t[0:cn, g * 128:(g + 1) * 128],
                                    scalar=ssq.t[0:cn, g:g + 1], in1=kgbc.t[0:cn, :], op0=ALU.mult, op1=ALU.mult),
                                    reads=[pa, ssq, kgbc], writes=[kob])
                            kb.op(ACT, lambda: S.copy(out=vob.t[0:cn, :], in_=pa.t[0:cn, 256:512]), reads=[pa], writes=[vob])
                            kb.op(ACT, lambda: S.copy(out=iob.t[0:cn, :], in_=pb2.t[0:cn, 0:64]), reads=[pb2], writes=[iob])
                            kb.op(ACT, lambda: S.activation(out=WIa.t[0:cn, ci, :], in_=pb2.t[0:cn, 64:68], func=AF.Abs),
                                  reads=[pb2], writes=[WIa])
                            kb.op(DVE, lambda: V.tensor_scalar(out=WIs.t[0:cn, ci, :], in0=pb2.t[0:cn, 64:68], scalar1=0.0,
                                                               scalar2=2.0, op0=ALU.is_ge, op1=ALU.mult), reads=[pb2], writes=[WIs])
                            kb.op(DVE, lambda: V.tensor_scalar(out=WIs.t[0:cn, ci, :], in0=WIs.t[0:cn, ci, :], scalar1=-1.0,
                                                               scalar2=None, op0=ALU.add), reads=[WIs], writes=[WIs])
                            kb.op(DVE, lambda: V.tensor_copy(out=kbf.t[0:cn, :], in_=kob.t[0:cn, :]), reads=[kob], writes=[kbf])
                            kb.op(ACT, lambda: S.copy(out=Vall.t[0:cn, ci, :], in_=vob.t[0:cn, :]), reads=[vob], writes=[Vall])
                            if not is_s:
                                r0 = ci * 128
                                kb.dma(SP, k_p[r0:r0 + 128, :], kob.t[:], reads=[kob])
                                kb.dma(SP, v_p[r0:r0 + 128, :], vob.t[:], reads=[vob])
                                kb.dma(SP, i_p[r0:r0 + 128, :], iob.t[:], reads=[iob])
                            else:
                                kb.dma(SP, k_s[:, :], kob.t[0:NS, :], reads=[kob])
                                kb.dma(SP, v_s[:, :], vob.t[0:NS, :], reads=[vob])
                                kb.dma(SP, i_s[:, :], iob.t[0:NS, :], reads=[iob])

                        def ck2(cc):
                            kbf = KBF[cc % 2]
                            cn = 128 if not is_s else NS
                            cs = cc * 128
                            pt = PS[4 + cc % 2]
                            ptb = pt.t[:].bitcast(BF16)
                            kb.transposes(pt, [(ptb[:, g * 128:g * 128 + cn], kbf.t[0:cn, g * 128:(g + 1) * 128]) for g in range(2)],
                                          [kbf], identb)
                            copy(evac_engine(), kT.t[:, :, t0 + cs:t0 + cs + cn],
                                 ptb[:, 0:256].rearrange("p (g t) -> p g t", g=2)[:, :, 0:cn], [pt], [kT])

                        ck1(0)
                        for cc in range(nchunk):
                            if cc + 1 < nchunk:
                                ck1(cc + 1)
                            ck2(cc)

                    HS.close()
                    if level < 2:
                        continue
                    if not is_s:
                        prompt_attention(nc, kb, PS, C, CST, ti, t0, qT, qiT, kT, Vall, kiT, WIa, WIs, biasN, identb, onesb, onT)
                    elif level < 6:
                        kb.op(DVE, lambda: V.memset(onT.t[:], 0.0), writes=[onT])
                    else:
                        sample_attention(nc, kb, PS, C, CST, qT, qiT, kT, Vall, kiT, WB, identb, onesb, onT,
                                         cache_k, cache_v, cache_i, ptab, relb, Win, biasS, biasNn)
                    for oc in range(8):
                        pb = PS[6 + oc % 2]
                        kb.mm(pb, pb.t[:, 0:n], [(Wo.t[:, kc, oc * 128:(oc + 1) * 128], onT.t[:, kc, 0:n]) for kc in range(8)], [Wo, onT])
                        add_resid(oc, t0, n, pb)

    if level >= 3:
        mlp(nc, kb, PS, xT, gn, EPSB, ones_d, 0, w_mlp_in, w_mlp_out, rmsnorm_tile)
    if level >= 4:
        retention(nc, kb, PS, (cstr, COR, cstr_np.shape[1]), None, xT, gn, identb, w_ret_in, w_ret_out, rot, st_in, r_p, r_s, rmsnorm_tile, level, EPSB)
    if level >= 5:
        mlp(nc, kb, PS, xT, gn, EPSB, ones_d, 1, w_mlp_in, w_mlp_out, rmsnorm_tile)

    with ExitStack() as st:
        ys = [kb.sb(st, "ys%d" % i, [128, D], F32) for i in range(2)]
        for c in range(17):
            n = 128 if c < 16 else NS
            yb = ys[c % 2]
            for g in range(2):
                pb = PS[(2 * c + g) % 8]
                eng = kb.PE
                kb._deps(eng, [xT, CST], [pb])
                inst = None
                for j in range(4):
                    inst = nc.tensor.transpose(pb.t[0:n, j * 128:(j + 1) * 128], xT.t[:, 4 * g + j, c * 128:c * 128 + n], identf.t[:, :])
                eng.cnt += 1
                inst.then_inc(eng.sem, 1)
                kb._commit((eng.key, eng.sem, eng.cnt), [xT, CST], [pb])
                copy(evac_engine(), yb.t[0:n, g * 512:(g + 1) * 512], pb.t[0:n, :], [pb], [yb])
            dst = y_p[c * 128:(c + 1) * 128, :] if c < 16 else y_s[:, :]
            kb.dma(SP, dst, yb.t[0:n, :], reads=[yb])
    kb.finish()
    kb.es.close()
    return nc, (cst_np, cstr_np)


def prompt_attention(nc, kb, PS, C, CST, ti, t0, qT, qiT, kT, Vall, kiT, WIa, WIs, biasN, identb, onesb, onT):
    V = nc.vector
    S = nc.scalar
    PE, ACT, DVE = kb.PE, kb.ACT, kb.DVE
    with ExitStack() as st:
        ACC = [kb.sb(st, "acc%d" % i, [128, 2048], F32) for i in range(2)]
        rt = [kb.sb(st, "rt%d" % i, [128, 512], F32) for i in range(2)]
        MASKB = [kb.sb(st, "maskb%d" % i, [128, 2048], BF16) for i in range(2)]
        MASKT = [kb.sb(st, "maskT%d" % i, [128, 16, 128], BF16) for i in range(2)]
        nearb = [kb.sb(st, "nearb%d" % i, [128, 4, 128], BF16) for i in range(2)]
        pT = [kb.sb(st, "pT%d" % i, [128, 512], BF16) for i in range(2)]
        rden = rt[0]
        LO = kb.sb(st, "b_lo", [128, 2], F32)
        MX = kb.sb(st, "b_mx", [128, 2], F32)
        MID = kb.sb(st, "b_mid", [128, 2], F32)
        CNT = kb.sb(st, "b_cnt", [128, 2], F32)
        GM = kb.sb(st, "b_gm", [128, 2], F32)
        TAU = kb.sb(st, "b_tau", [128, 2], F32)
        WH = kb.sb(st, "b_wh", [128, 2, NBIS], F32)
        HALF = kb.sb(st, "b_half", [128, 2], F32)
        WH2 = kb.sb(st, "b_wh2", [128, NBIS], F32)
        THR = kb.sb(st, "b_thr", [128, 1], F32)
        SA = kb.sb(st, "b_sa", [128, 1], F32)
        GA = kb.sb(st, "b_ga", [128, 1], F32)
        MIDA = [kb.sb(st, "b_mida%d" % i, [128, 1], F32) for i in range(2)]
        ONE = kb.sb(st, "b_one", [128, 2], F32)
        kb.op(DVE, lambda: V.memset(HALF.t[:], 0.5), writes=[HALF])
        kb.op(DVE, lambda: V.memset(ONE.t[:], 1.0), writes=[ONE])
        for pair in range(2):
            blocks = [ti * 4 + 2 * pair, ti * 4 + 2 * pair + 1]
            for k, b in enumerate(blocks):
                acc = ACC[k]
                q0 = (b % 4) * 128
                Sb = 128 * (b + 1)
                nch = (Sb + 511) // 512
                for sc in range(nch):
                    s0 = sc * 512
                    N = min(512, Sb - s0)
                    for h in range(4):
                        par, pr = h % 2, h // 2
                        pa = PS[(sc * 4 + h) % 2]
                        kb.mm(pa, pa.t[:, 0:N], [(qiT.t[par * 64:(par + 1) * 64, pr, q0:q0 + 128],
                                                  kiT.t[par * 64:(par + 1) * 64, s0:s0 + N])], [qiT, kiT])
                        r = rt[h % 2]
                        kb.op(ACT, lambda: S.activation(out=r.t[:, 0:N], in_=pa.t[:, 0:N], func=AF.Relu,
                                                        scale=WIa.t[:, b, h:h + 1]), reads=[pa, WIa], writes=[r])
                        if h == 0:
                            kb.op(DVE, lambda: V.tensor_scalar(out=acc.t[:, s0:s0 + N], in0=r.t[:, 0:N], scalar1=WIs.t[:, b, 0:1],
                                                               scalar2=None, op0=ALU.mult), reads=[r, WIs], writes=[acc])
                        else:
                            kb.op(DVE, lambda h=h: V.scalar_tensor_tensor(out=acc.t[:, s0:s0 + N], in0=r.t[:, 0:N],
                                                                         scalar=WIs.t[:, b, h:h + 1], in1=acc.t[:, s0:s0 + N],
                                                                         op0=ALU.mult, op1=ALU.add), reads=[r, WIs, acc], writes=[acc])
                kb.op(DVE, lambda: V.tensor_tensor(out=acc.t[:, Sb - 128:Sb], in0=acc.t[:, Sb - 128:Sb], in1=C("causneg"), op=ALU.add),
                      reads=[acc, CST], writes=[acc])
            if blocks[0] >= 2:
                for k, b in enumerate(blocks):
                    Sb = 128 * (b + 1)
                    kb.op(DVE, lambda: V.tensor_reduce(out=LO.t[:, k:k + 1], in_=ACC[k].t[:, 0:Sb - 128], axis=AX.X, op=ALU.min),
                          reads=[ACC[k]], writes=[LO])
                    kb.op(DVE, lambda: V.tensor_reduce(out=MX.t[:, k:k + 1], in_=ACC[k].t[:, 0:Sb], axis=AX.X, op=ALU.max),
                          reads=[ACC[k]], writes=[MX])
                kb.op(DVE, lambda: V.scalar_tensor_tensor(out=MX.t[:], in0=MX.t[:], scalar=1.0, in1=LO.t[:], op0=ALU.add, op1=ALU.subtract),
                      reads=[MX, LO], writes=[MX])
                for k in range(2):
                    kb.op(DVE, lambda: V.tensor_scalar(out=WH.t[:, k, :], in0=C("pow2"), scalar1=MX.t[:, k:k + 1], scalar2=None, op0=ALU.mult),
                          reads=[CST, MX], writes=[WH])
                kb.op(DVE, lambda: V.tensor_tensor(out=MID.t[:], in0=LO.t[:], in1=WH.t[:, :, 0], op=ALU.add), reads=[LO, WH], writes=[MID])
                Sb0 = 128 * (blocks[0] + 1)
                Sb1 = 128 * (blocks[1] + 1)
                kb.op(DVE, lambda: V.tensor_scalar(out=WH2.t[:], in0=WH.t[:, 1, :], scalar1=0.5, scalar2=None, op0=ALU.mult), reads=[WH], writes=[WH2])
                kb.op(DVE, lambda: V.memset(THR.t[:], float(Sb1) - 510.5), writes=[THR])
                kb.op(DVE, lambda: V.tensor_copy(out=MIDA[0].t[:], in_=MID.t[:, 1:2]), reads=[MID], writes=[MIDA[0]])
                for it in range(NBIS):
                    last = (it == NBIS - 1)
                    kb.op(DVE, lambda: V.tensor_scalar(out=MASKB[0].t[:, 0:Sb0], in0=ACC[0].t[:, 0:Sb0], scalar1=MID.t[:, 0:1], scalar2=0.0,
                                                       op0=ALU.is_ge, op1=ALU.add, accum_out=CNT.t[:, 0:1]),
                          reads=[ACC[0], MID], writes=[MASKB[0], CNT])
                    kb.op(DVE, lambda: V.scalar_tensor_tensor(out=GM.t[:, 0:1], in0=CNT.t[:, 0:1], scalar=255.5, in1=(ONE if last else HALF).t[:, 0:1],
                                                              op0=ALU.is_ge, op1=ALU.subtract), reads=[CNT, ONE, HALF], writes=[GM])
                    dst = TAU if last else MID
                    kb.op(DVE, lambda: V.scalar_tensor_tensor(out=dst.t[:, 0:1], in0=GM.t[:, 0:1], scalar=WH.t[:, 0, it:it + 1],
                                                              in1=MID.t[:, 0:1], op0=ALU.mult, op1=ALU.add),
                          reads=[GM, WH, MID], writes=[dst])
                    ma, mb_ = MIDA[it % 2], MIDA[(it + 1) % 2]
                    kb.op(ACT, lambda: S.activation(out=MASKB[1].t[:, 0:Sb1], in_=ACC[1].t[:, 0:Sb1], func=AF.Sign, scale=-1.0,
                                                    bias=ma.t[:, 0:1], accum_out=SA.t[:, 0:1]), reads=[ACC[1], ma], writes=[MASKB[1], SA])
                    kb.op(ACT, lambda: S.activation(out=GA.t[:, 0:1], in_=SA.t[:, 0:1], func=AF.Sign, scale=-1.0, bias=THR.t[:, 0:1]),
                          reads=[SA, THR], writes=[GA])
                    kb.op(ACT, lambda: S.activation(out=mb_.t[:, 0:1], in_=GA.t[:, 0:1], func=AF.Identity, scale=WH2.t[:, it:it + 1],
                                                    bias=ma.t[:, 0:1]), reads=[GA, WH2, ma], writes=[mb_])
                kb.op(DVE, lambda: V.tensor_tensor(out=TAU.t[:, 1:2], in0=MIDA[NBIS % 2].t[:, 0:1], in1=WH2.t[:, NBIS - 1:NBIS], op=ALU.subtract),
                      reads=[MIDA[NBIS % 2], WH2], writes=[TAU])
            else:
                kb.op(DVE, lambda: V.memset(TAU.t[:], -1.0e29), writes=[TAU])
            for k, b in enumerate(blocks):
                Sb = 128 * (b + 1)
                maskb, maskT = MASKB[k], MASKT[k]
                kb.op(DVE, lambda: V.tensor_scalar(out=maskb.t[:, 0:Sb], in0=ACC[k].t[:, 0:Sb], scalar1=TAU.t[:, k:k + 1], scalar2=NEG,
                                                   op0=ALU.is_lt, op1=ALU.mult), reads=[ACC[k], TAU], writes=[maskb])
                for j0 in range(0, b + 1, 8):
                    j1 = min(b + 1, j0 + 8)
                    pt = PS[2 + (j0 // 8) % 2]
                    ptb = pt.t[:].bitcast(BF16)
                    kb.transposes(pt, [(ptb[:, (j - j0) * 128:(j - j0 + 1) * 128], maskb.t[:, j * 128:(j + 1) * 128]) for j in range(j0, j1)],
                                  [maskb], identb)
                    src = ptb[:, 0:(j1 - j0) * 128].rearrange("p (j t) -> p j t", t=128)
                    if (j0 // 8) % 2 == 0:
                        kb.op(ACT, lambda: S.copy(out=maskT.t[:, j0:j1, :], in_=src), reads=[pt], writes=[maskT])
                    else:
                        kb.op(DVE, lambda: V.tensor_copy(out=maskT.t[:, j0:j1, :], in_=src), reads=[pt], writes=[maskT])
            for k, b in enumerate(blocks):
                maskT = MASKT[k]
                q0 = (b % 4) * 128
                for g in range(2):
                    po = PS[4 + g]
                    pd = PS[6 + g]
                    def logits(j):
                        pl = PS[j % 2]
                        near = (j >= b - 1)
                        if near:
                            nb_ = nearb[j % 2]
                            kb.op(DVE, lambda: V.tensor_tensor(out=nb_.t[:], in0=biasN.t[:, b - j, 4 * g:4 * g + 4, :],
                                                               in1=maskT.t[:, j, :].unsqueeze(1).to_broadcast([128, 4, 128]), op=ALU.add),
                                  reads=[biasN, maskT], writes=[nb_])
                            rhs2 = nb_.t[:]
                            rd2 = [nb_]
                        else:
                            rhs2 = maskT.t[:, j, :].unsqueeze(1).to_broadcast([128, 4, 128])
                            rd2 = [maskT]
                        kb.mm(pl, pl.t[:].rearrange("p (r t) -> p r t", r=4),
                              [(kT.t[:, g, j * 128:(j + 1) * 128], qT.t[:, 4 * g:4 * g + 4, q0:q0 + 128]), (identb.t[:], rhs2)],
                              [kT, qT, identb] + rd2)

                    logits(0)
                    for j in range(b + 1):
                        if j + 1 <= b:
                            logits(j + 1)
                        pl = PS[j % 2]
                        pp = pT[j % 2]
                        kb.op(ACT, lambda: S.activation(out=pp.t[:], in_=pl.t[:], func=AF.Exp), reads=[pl], writes=[pp])
                        kb.mm(po, po.t[:], [(Vall.t[:, j, g * 128:(g + 1) * 128], pp.t[:])], [Vall, pp], start=(j == 0), stop=(j == b))
                        kb.mm(pd, pd.t[:], [(onesb.t[:], pp.t[:])], [onesb, pp], start=(j == 0), stop=(j == b))
                    kb.op(DVE, lambda: V.reciprocal(out=rden.t[:], in_=pd.t[:]), reads=[pd], writes=[rden])
                    kb.op(DVE, lambda: V.tensor_tensor(out=onT.t[:, 4 * g:4 * g + 4, q0:q0 + 128],
                                                       in0=po.t[:].rearrange("p (r t) -> p r t", r=4),
                                                       in1=rden.t[:].rearrange("p (r t) -> p r t", r=4), op=ALU.mult),
                          reads=[po, rden], writes=[onT])


def mm_multi(kb, nc, out_buf, items, reads):
    eng = kb.PE
    kb._deps(eng, reads, [out_buf])
    inst = None
    for (o, l, r) in items:
        inst = nc.tensor.matmul(o, lhsT=l, rhs=r, start=True, stop=True)
    eng.cnt += 1
    inst.then_inc(eng.sem, 1)
    kb._commit((eng.key, eng.sem, eng.cnt), reads, [out_buf])


def sample_attention(nc, kb, PS, C, CST, qT, qiT, kT, Vall, kiT, WB, identb, onesb, onT, cache_k, cache_v, cache_i, ptab, relb, Win, biasS, biasNn):
    V = nc.vector
    S = nc.scalar
    ACT, DVE, POOL, SP = kb.ACT, kb.DVE, kb.POOL, kb.SP
    IOA = bass.IndirectOffsetOnAxis
    with ExitStack() as st:
        IDX = kb.sb(st, "IDX", [128, 32], I32)
        ISa = kb.sb(st, "ISa", [128, 16, 16, 4], F32)
        ISn = kb.sb(st, "ISn", [128, 16, 4], F32)
        LO = kb.sb(st, "LO", [128, 64], F32)
        with ExitStack() as s2:
            PT = kb.sb(s2, "PT", [128, 256], I32)
            PTf = kb.sb(s2, "PTf", [128, 16, 16], F32)
            PG = kb.sb(s2, "PG", [128, 32], F32)
            kb.dma(SP, PT.t[:], ptab.partition_broadcast(128), writes=[PT])
            kb.op(DVE, lambda: V.tensor_copy(out=PTf.t[:], in_=PT.t[:].rearrange("p (b j) -> p b j", j=16)), reads=[PT], writes=[PTf])
            kb.op(DVE, lambda: V.tensor_tensor(out=PTf.t[:], in0=PTf.t[:], in1=C("pagesel").unsqueeze(1).to_broadcast([128, 16, 16]),
                                               op=ALU.mult), reads=[PTf, CST], writes=[PTf])
            kb.op(DVE, lambda: V.tensor_reduce(out=PG.t[:, 0:16], in_=PTf.t[:], axis=AX.X, op=ALU.add), reads=[PTf], writes=[PG])
            kb.op(DVE, lambda: V.tensor_scalar(out=PG.t[:, 0:16], in0=PG.t[:, 0:16], scalar1=128.0, scalar2=C("pofs")[:, 0:1],
                                               op0=ALU.mult, op1=ALU.add), reads=[PG, CST], writes=[PG])
            kb.op(DVE, lambda: V.tensor_scalar(out=PG.t[:, 16:32], in0=PG.t[:, 0:16], scalar1=8.0, scalar2=None, op0=ALU.add),
                  reads=[PG], writes=[PG])
            kb.op(DVE, lambda: V.tensor_copy(out=IDX.t[:], in_=PG.t[:]), reads=[PG], writes=[IDX])

        with ExitStack() as s2:
            IG = [kb.sb(s2, "IG", [128, 16, 64], F32) for _ in range(2)]
            IB2 = kb.sb(s2, "IB2", [128, 16, 128], BF16)
            ITK = kb.sb(s2, "ITK", [128, 16, 128], BF16)
            tmp = kb.sb(s2, "tmpI", [128, 16, 4, 4], F32)
            for b in range(NSEQ):
                ig = IG[b % 2]
                kb.dma(POOL, ig.t[:].rearrange("p c d -> p (c d)"), cache_i, reads=[IDX], writes=[ig], indirect=IOA(ap=IDX.t[:, b:b + 1], axis=0))
                kb.op(ACT, lambda: S.copy(out=IB2.t[:, :, 0:64], in_=ig.t[:]), reads=[ig], writes=[IB2])
                kb.op(DVE, lambda: V.tensor_copy(out=IB2.t[:, :, 64:128], in_=ig.t[:]), reads=[ig], writes=[IB2])
                for half in range(2):
                    pt = PS[half]
                    ptb = pt.t[:].bitcast(BF16)
                    kb.transposes(pt, [(ptb[:, j * 128:(j + 1) * 128], IB2.t[:, half * 8 + j, :]) for j in range(8)], [IB2], identb)
                    copy_any(kb, nc, half, ITK.t[:, half * 8:half * 8 + 8, :], ptb[:, :].rearrange("p (j t) -> p j t", t=128), [pt], [ITK])
                pIs = [PS[2], PS[7]]
                wbb = WB.t[:, b, :]
                for par in range(2):
                    pI = pIs[par]
                    items = []
                    for c in range(16):
                        items.append((pI.t[:, c * 8:c * 8 + 8], ITK.t[par * 64:(par + 1) * 64, c, :],
                                      qiT.t[par * 64:(par + 1) * 64, :, 4 * b:4 * b + 4]))
                    items.append((pI.t[0:NS, 128:136], kiT.t[par * 64:(par + 1) * 64, SEQ:SEQ + NS],
                                  qiT.t[par * 64:(par + 1) * 64, :, 4 * b:4 * b + 4]))
                    mm_multi(kb, nc, pI, items, [ITK, qiT, kiT])
                for par in range(2):
                    pI = pIs[par]
                    kb.op(DVE, lambda: V.scalar_tensor_tensor(out=tmp.t[:, :, 2 * par:2 * par + 2, :].rearrange("p c h t -> p c (h t)"),
                                                              in0=pI.t[:, 0:128].rearrange("p (c x) -> p c x", c=16), scalar=0.0,
                                                              in1=wbb[:, 8 * par:8 * par + 8].unsqueeze(1).to_broadcast([128, 16, 8]),
                                                              op0=ALU.max, op1=ALU.mult), reads=[pI, WB], writes=[tmp])
                kb.op(DVE, lambda: V.tensor_reduce(out=ISa.t[:, b, :, :], in_=tmp.t[:].rearrange("p c h t -> p c t h"), axis=AX.X, op=ALU.add),
                      reads=[tmp], writes=[ISa])
                if kb.DBG and b == 14:
                    kb.dma(SP, kb.DBG["G"], ig.t[:].rearrange("p c d -> p (c d)"), reads=[ig])
                    kb.dma(SP, kb.DBG["B"], IB2.t[:].rearrange("p c d -> p (c d)"), reads=[IB2])
                    kb.dma(SP, kb.DBG["T"], ITK.t[:].rearrange("p c d -> p (c d)"), reads=[ITK])
                    kb.dma(SP, kb.DBG["M"], tmp.t[:].rearrange("p c h t -> p (c h t)"), reads=[tmp])
                for par in range(2):
                    pI = pIs[par]
                    kb.op(DVE, lambda: V.scalar_tensor_tensor(out=tmp.t[0:NS, 0, 2 * par:2 * par + 2, :].rearrange("p h t -> p (h t)"),
                                                              in0=pI.t[0:NS, 128:136], scalar=0.0, in1=wbb[0:NS, 8 * par:8 * par + 8],
                                                              op0=ALU.max, op1=ALU.mult), reads=[pI, WB], writes=[tmp])
                kb.op(DVE, lambda: V.tensor_reduce(out=ISn.t[0:NS, b, :], in_=tmp.t[0:NS, 0, :, :].rearrange("p h t -> p t h"), axis=AX.X, op=ALU.add),
                      reads=[tmp], writes=[ISn])
            kb.op(DVE, lambda: V.tensor_tensor(out=ISn.t[0:NS, :, :], in0=ISn.t[0:NS, :, :],
                                               in1=C("newvalid")[0:NS, :].rearrange("p (b t) -> p b t", t=4), op=ALU.add),
                  reads=[ISn, CST], writes=[ISn])

        with ExitStack() as s2:
            Wd = kb.sb(s2, "Wd", [128, 64], F32)
            WH = kb.sb(s2, "WH", [128, 64], F32)
            MID = kb.sb(s2, "MID", [128, 64], F32)
            GE = kb.sb(s2, "GE", [128, 64], F32)
            CMP = kb.sb(s2, "CMP", [128, 16, 16, 4], BF16)
            CNP = kb.sb(s2, "CNP", [128, 64], F32)
            CMN = kb.sb(s2, "CMN", [128, 64], F32)
            MX = kb.sb(s2, "MX", [128, 128], F32)
            DG = kb.sb(s2, "DG", [128, 128], F32)
            onesf = kb.sb(s2, "onesf", [128, 128], F32)
            kb.op(DVE, lambda: V.memset(onesf.t[:], 1.0), writes=[onesf])
            kb.op(DVE, lambda: V.tensor_reduce(out=MX.t[:, 0:64].rearrange("p (b t) -> p b t", t=4), in_=ISa.t[:].rearrange("p b c t -> p b t c"),
                                               axis=AX.X, op=ALU.max), reads=[ISa], writes=[MX])
            kb.op(DVE, lambda: V.tensor_reduce(out=MX.t[:, 64:128].rearrange("p (b t) -> p b t", t=4), in_=ISa.t[:].rearrange("p b c t -> p b t c"),
                                               axis=AX.X, op=ALU.min), reads=[ISa], writes=[MX])
            kb.op(DVE, lambda: V.tensor_tensor(out=MX.t[0:NS, 0:64], in0=MX.t[0:NS, 0:64], in1=ISn.t[0:NS, :, :].rearrange("p b t -> p (b t)"),
                                               op=ALU.max), reads=[MX, ISn], writes=[MX])
            pm = PS[3]
            eng = kb.PE
            kb._deps(eng, [MX, CST], [pm])
            ins_ = nc.tensor.transpose(pm.t[:, 0:128], MX.t[:, :], CST.t[:, 0:128])
            eng.cnt += 1
            ins_.then_inc(eng.sem, 1)
            kb._commit((eng.key, eng.sem, eng.cnt), [MX, CST], [pm])
            kb.op(DVE, lambda: V.tensor_reduce(out=GE.t[0:64, 0:1], in_=pm.t[0:64, 0:128], axis=AX.X, op=ALU.max), reads=[pm], writes=[GE])
            kb.op(DVE, lambda: V.tensor_reduce(out=GE.t[64:128, 0:1], in_=pm.t[64:128, 0:128], axis=AX.X, op=ALU.min), reads=[pm], writes=[GE])
            kb.op(DVE, lambda: V.tensor_scalar(out=DG.t[:], in0=CST.t[:, 0:128], scalar1=GE.t[:, 0:1], scalar2=None, op0=ALU.mult),
                  reads=[CST, GE], writes=[DG])
            pq = PS[4]
            kb.mm(pq, pq.t[:, 0:128], [(onesf.t[:], DG.t[:])], [onesf, DG])
            kb.op(DVE, lambda: V.tensor_copy(out=LO.t[:], in_=pq.t[:, 64:128]), reads=[pq], writes=[LO])
            kb.op(DVE, lambda: V.scalar_tensor_tensor(out=Wd.t[:], in0=pq.t[:, 0:64], scalar=1.0, in1=LO.t[:], op0=ALU.add, op1=ALU.subtract),
                  reads=[pq, LO], writes=[Wd])
            pc = PS[5]
            for it in range(NBIS):
                kb.op(DVE, lambda: V.tensor_scalar(out=WH.t[:], in0=Wd.t[:], scalar1=0.5, scalar2=None, op0=ALU.mult), reads=[Wd], writes=[WH])
                kb.op(DVE, lambda: V.tensor_tensor(out=MID.t[:], in0=LO.t[:], in1=WH.t[:], op=ALU.add), reads=[LO, WH], writes=[MID])
                kb.op(DVE, lambda: V.tensor_tensor(out=CMP.t[:], in0=ISa.t[:],
                                                   in1=MID.t[:].rearrange("p (b t) -> p b t", t=4).unsqueeze(2).to_broadcast([128, 16, 16, 4]),
                                                   op=ALU.is_ge), reads=[ISa, MID], writes=[CMP])
                kb.op(DVE, lambda: V.tensor_reduce(out=CNP.t[:].rearrange("p (b t) -> p b t", t=4), in_=CMP.t[:].rearrange("p b c t -> p b t c"),
                                                   axis=AX.X, op=ALU.add), reads=[CMP], writes=[CNP])
                kb.op(DVE, lambda: V.tensor_tensor(out=CMN.t[0:NS, :], in0=ISn.t[0:NS, :, :].rearrange("p b t -> p (b t)"), in1=MID.t[0:NS, :],
                                                   op=ALU.is_ge), reads=[ISn, MID], writes=[CMN])
                kb.mm(pc, pc.t[:, 0:64], [(onesf.t[:], CNP.t[:]), (onesf.t[0:NS, :], CMN.t[0:NS, :])], [onesf, CNP, CMN])
                kb.op(DVE, lambda: V.tensor_scalar(out=GE.t[:, 0:64], in0=pc.t[:, 0:64], scalar1=255.5, scalar2=None, op0=ALU.is_ge), reads=[pc], writes=[GE])
                kb.op(DVE, lambda: V.tensor_tensor(out=GE.t[:, 0:64], in0=GE.t[:, 0:64], in1=WH.t[:], op=ALU.mult), reads=[GE, WH], writes=[GE])
                kb.op(DVE, lambda: V.tensor_tensor(out=LO.t[:], in0=LO.t[:], in1=GE.t[:, 0:64], op=ALU.add), reads=[LO, GE], writes=[LO])
                kb.op(DVE, lambda: V.tensor_copy(out=Wd.t[:], in_=WH.t[:]), reads=[WH], writes=[Wd])

        with ExitStack() as s2:
            MB = kb.sb(s2, "MB", [128, 16, 16, 4], F32)
            MBN = kb.sb(s2, "MBN", [128, 16, 4], F32)
            kb.op(DVE, lambda: V.tensor_tensor(out=MB.t[:], in0=ISa.t[:],
                                               in1=LO.t[:].rearrange("p (b t) -> p b t", t=4).unsqueeze(2).to_broadcast([128, 16, 16, 4]),
                                               op=ALU.is_lt), reads=[ISa, LO], writes=[MB])
            kb.op(DVE, lambda: V.tensor_scalar(out=MB.t[:], in0=MB.t[:], scalar1=NEG, scalar2=None, op0=ALU.mult), reads=[MB], writes=[MB])
            kb.op(DVE, lambda: V.tensor_tensor(out=MBN.t[0:NS], in0=ISn.t[0:NS], in1=LO.t[0:NS, :].rearrange("p (b t) -> p b t", t=4), op=ALU.is_lt),
                  reads=[ISn, LO], writes=[MBN])
            kb.op(DVE, lambda: V.tensor_scalar(out=MBN.t[0:NS], in0=MBN.t[0:NS], scalar1=NEG, scalar2=None, op0=ALU.mult), reads=[MBN], writes=[MBN])
            KGS = [kb.sb(s2, "KG", [128, 8, 256], F32) for _ in range(2)]
            kgi = [0]
            wfl = Win.t[:].rearrange("p a b -> p (a b)")
            wf = {}
            if Win.lw:
                wf[Win.lw[0]] = (Win.lw[1], Win.lw[2])
            for k_, v_ in Win.rd.items():
                if wf.get(k_, (None, 0))[1] < v_[1]:
                    wf[k_] = v_
            KB_ = Buf(wfl[:, 0:4096].rearrange("p (c d) -> p c d", d=256), "KBb", wf)
            VB_ = Buf(wfl[:, 4096:8192].rearrange("p (c d) -> p c d", d=256), "VBb", wf)
            KT = Buf(wfl[:, 8192:12288].rearrange("p (c g t) -> p c g t", g=2, t=128), "KTs", wf)
            LG = kb.sb(s2, "LG", [128, 16, 8, 4], F32)
            MBI = kb.sb(s2, "MBI", [128, 16, 8, 4], F32)
            LGN = kb.sb(s2, "LGN", [128, 8, 4], F32)
            PT_ = kb.sb(s2, "PTs", [128, 16, 8, 4], BF16)
            PTN = kb.sb(s2, "PTN", [128, 8, 4], BF16)
            DEN = kb.sb(s2, "DEN", [128, 8, 4], F32)
            for b in range(NSEQ):
                for (src, dstb, e) in ((cache_k, KB_, 0), (cache_v, VB_, 1)):
                    for half in range(2):
                        KG = KGS[kgi[0] % 2]
                        kgi[0] += 1
                        kb.dma(POOL, KG.t[:].rearrange("p c d -> p (c d)"), src, reads=[IDX], writes=[KG], indirect=IOA(ap=IDX.t[:, half * 16 + b:half * 16 + b + 1], axis=0))
                        copy_any(kb, nc, (half + e) % 2, dstb.t[:, half * 8:half * 8 + 8, :], KG.t[:], [KG], [dstb])
                for q4 in range(4):
                    pt = PS[q4 % 2]
                    ptb = pt.t[:].bitcast(BF16)
                    kb.transposes(pt, [(ptb[:, (2 * j + g) * 128:(2 * j + g + 1) * 128], KB_.t[:, q4 * 4 + j, g * 128:(g + 1) * 128])
                                       for j in range(4) for g in range(2)], [KB_], identb)
                    copy_any(kb, nc, q4 % 2, KT.t[:, q4 * 4:q4 * 4 + 4, :, :], ptb[:, :].rearrange("p (j g t) -> p j g t", j=4, g=2), [pt], [KT])
                pL = PS[2]
                pLv = pL.t[:].rearrange("p (c h t) -> p c h t", c=16, h=8)
                items = []
                for c in range(16):
                    for g in range(2):
                        items.append((pL.t[:, (c * 8 + 4 * g) * 4:(c * 8 + 4 * g) * 4 + 16], KT.t[:, c, g, :], qT.t[:, 4 * g:4 * g + 4, 4 * b:4 * b + 4]))
                mm_multi(kb, nc, pL, items, [KT, qT])
                pN = PS[3]
                pNv = pN.t[0:NS, 0:32].rearrange("p (h t) -> p h t", h=8)
                mm_multi(kb, nc, pN, [(pN.t[0:NS, g * 16:(g + 1) * 16], kT.t[:, g, SEQ:SEQ + NS], qT.t[:, 4 * g:4 * g + 4, 4 * b:4 * b + 4]) for g in range(2)],
                         [kT, qT])
                kb.op(DVE, lambda: V.tensor_tensor(out=MBI.t[:], in0=biasS.t[:], in1=MB.t[:, b, :, :].unsqueeze(2).to_broadcast([128, 16, 8, 4]),
                                                   op=ALU.add), reads=[biasS, MB], writes=[MBI])
                kb.op(DVE, lambda: V.tensor_tensor(out=LG.t[:], in0=pLv, in1=MBI.t[:], op=ALU.add), reads=[pL, MBI], writes=[LG])
                kb.op(ACT, lambda: S.activation(out=PT_.t[:], in_=LG.t[:], func=AF.Exp), reads=[LG], writes=[PT_])
                kb.op(DVE, lambda: V.tensor_tensor(out=LGN.t[0:NS], in0=biasNn.t[0:NS, :, 4 * b:4 * b + 4],
                                                   in1=MBN.t[0:NS, b, :].unsqueeze(1).to_broadcast([NS, 8, 4]), op=ALU.add),
                      reads=[biasNn, MBN], writes=[LGN])
                kb.op(DVE, lambda: V.tensor_tensor(out=LGN.t[0:NS], in0=pNv, in1=LGN.t[0:NS], op=ALU.add), reads=[pN, LGN], writes=[LGN])
                kb.op(ACT, lambda: S.activation(out=PTN.t[0:NS], in_=LGN.t[0:NS], func=AF.Exp), reads=[LGN], writes=[PTN])
                pD = PS[4]
                kb.mm(pD, pD.t[:], [(onesb.t[:], PT_.t[:].rearrange("p c h t -> p (c h t)"))], [onesb, PT_])
                pDn = PS[5]
                kb.mm(pDn, pDn.t[:, 0:32], [(onesb.t[0:NS, :], PTN.t[0:NS].rearrange("p h t -> p (h t)"))], [onesb, PTN])
                kb.op(DVE, lambda: V.tensor_reduce(out=DEN.t[:], in_=pD.t[:].rearrange("p (c h t) -> p h t c", c=16, h=8), axis=AX.X, op=ALU.add),
                      reads=[pD], writes=[DEN])
                kb.op(DVE, lambda: V.tensor_tensor(out=DEN.t[:], in0=DEN.t[:], in1=pDn.t[:, 0:32].rearrange("p (h t) -> p h t", h=8), op=ALU.add),
                      reads=[DEN, pDn], writes=[DEN])
                kb.op(DVE, lambda: V.reciprocal(out=DEN.t[:], in_=DEN.t[:]), reads=[DEN], writes=[DEN])
                pO = PS[6]
                pOv = pO.t[:, 0:32].rearrange("p (h t) -> p h t", h=8)
                for g in range(2):
                    prs = [(VB_.t[:, c, g * 128:(g + 1) * 128], PT_.t[:, c, 4 * g:4 * g + 4, :]) for c in range(16)]
                    prs.append((Vall.t[0:NS, 16, g * 128:(g + 1) * 128], PTN.t[0:NS, 4 * g:4 * g + 4, :]))
                    kb.mm(pO, pO.t[:, g * 16:(g + 1) * 16], prs, [VB_, PT_, Vall, PTN])
                kb.op(DVE, lambda: V.tensor_tensor(out=onT.t[:, :, 4 * b:4 * b + 4], in0=pOv, in1=DEN.t[:], op=ALU.mult),
                      reads=[pO, DEN], writes=[onT])
            if kb.DBG:
                kb.dma(SP, kb.DBG["I"], IDX.t[:], reads=[IDX])
                kb.dma(SP, kb.DBG["A"], ISa.t[:].rearrange("p b c t -> p (b c t)"), reads=[ISa])
                kb.dma(SP, kb.DBG["L"], LO.t[:], reads=[LO])
                kb.dma(SP, kb.DBG["N"][0:NS, :], ISn.t[0:NS].rearrange("p b t -> p (b t)"), reads=[ISn])
                kb.dma(SP, kb.DBG["D"], DEN.t[:].rearrange("p h t -> p (h t)"), reads=[DEN])
                kb.dma(SP, kb.DBG["W"], WB.t[:].rearrange("p b x -> p (b x)"), reads=[WB])


def copy_any(kb, nc, which, out_ap, in_ap, reads, writes):
    if which == 0:
        kb.op(kb.ACT, lambda: nc.scalar.copy(out=out_ap, in_=in_ap), reads=reads, writes=writes)
    else:
        kb.op(kb.DVE, lambda: nc.vector.tensor_copy(out=out_ap, in_=in_ap), reads=reads, writes=writes)


def mlp(nc, kb, PS, xT, gn, EPSB, ones_d, l, w_in, w_out, rmsnorm_tile):
    V = nc.vector
    S = nc.scalar
    ACT, DVE, POOL = kb.ACT, kb.DVE, kb.POOL
    with ExitStack() as st:
        hT = kb.sb(st, "hTm", [128, 8, T], BF16)
        with ExitStack() as s2:
            sqb = kb.sb(s2, "sqbm", [128, 8, 512], BF16)
            rsb = kb.sb(s2, "rsbm", [128, 512], F32)
            for (t0, n) in TT:
                rmsnorm_tile(hT, hT.t[:, :, t0:t0 + n], t0, n, 8 + 16 * l, sqb, rsb)
        W1 = [kb.sb(st, "W1", [128, 8, 512], BF16) for _ in range(3)]
        W2 = [kb.sb(st, "W2", [128, 4, 1024], BF16) for _ in range(3)]
        U = [kb.sb(st, "U", [128, 4, T], BF16) for _ in range(2)]
        R = [kb.sb(st, "R", [128, 512], BF16) for _ in range(2)]
        w1src = w_in[l].rearrange("(kc p) n -> p kc n", p=128)
        cnt = [0, 0]

        def load(fg):
            kb.dma(POOL, W1[fg % 3].t[:], w1src[:, :, fg * 512:(fg + 1) * 512], writes=[W1[fg % 3]])
            kb.dma(POOL, W2[fg % 3].t[:], w_out[l][fg * 512:(fg + 1) * 512, :].rearrange("(c p) n -> p c n", p=128),
                   writes=[W2[fg % 3]])

        def phase1(fg):
            w1, ub = W1[fg % 3], U[fg % 2]
            for ffc in range(4):
                for (t0, n) in TT:
                    pb = PS[cnt[0] % 4]
                    rb = R[cnt[0] % 2]
                    cnt[0] += 1
                    kb.mm(pb, pb.t[:, 0:n], [(w1.t[:, kc, ffc * 128:(ffc + 1) * 128], hT.t[:, kc, t0:t0 + n]) for kc in range(8)],
                          [w1, hT])
                    kb.op(ACT, lambda: S.activation(out=rb.t[:, 0:n], in_=pb.t[:, 0:n], func=AF.Relu), reads=[pb], writes=[rb])
                    kb.op(DVE, lambda: V.tensor_tensor(out=ub.t[:, ffc, t0:t0 + n], in0=rb.t[:, 0:n], in1=rb.t[:, 0:n], op=ALU.mult),
                          reads=[rb], writes=[ub])

        def phase2(fg):
            w2, ub = W2[fg % 3], U[fg % 2]
            for oc in range(8):
                for (t0, n) in TT:
                    pb = PS[4 + cnt[1] % 4]
                    cnt[1] += 1
                    kb.mm(pb, pb.t[:, 0:n], [(w2.t[:, ffc, oc * 128:(oc + 1) * 128], ub.t[:, ffc, t0:t0 + n]) for ffc in range(4)],
                          [w2, ub])
                    kb.op(DVE, lambda: V.tensor_tensor(out=xT.t[:, oc, t0:t0 + n], in0=xT.t[:, oc, t0:t0 + n], in1=pb.t[:, 0:n],
                                                       op=ALU.add), reads=[xT, pb], writes=[xT])

        load(0)
        load(1)
        phase1(0)
        for fg in range(8):
            if fg + 2 < 8:
                load(fg + 2)
            if fg + 1 < 8:
                phase1(fg + 1)
            phase2(fg)


def retention(nc, kb, PS, C, CST, xT, gn, identb, w_in, w_out, rot, st_in, r_p, r_s, rmsnorm_tile, level, EPSB_):
    V = nc.vector
    S = nc.scalar
    ACT, DVE, POOL, SP = kb.ACT, kb.DVE, kb.POOL, kb.SP
    with ExitStack() as st:
        cstr_ap, COR, ncr = C
        CST = kb.sb(st, "cstr", [128, ncr], F32)
        kb.dma(SP, CST.t[:], cstr_ap, writes=[CST])

        def C(name):
            o, w = COR[name]
            return CST.t[:, o:o + w]
        hT = kb.sb(st, "hTr", [128, 8, T], BF16)
        with ExitStack() as s2:
            sqb = kb.sb(s2, "sqbr", [128, 8, 512], BF16)
            rsb = kb.sb(s2, "rsbr", [128, 512], F32)
            for (t0, n) in TT:
                rmsnorm_tile(hT, hT.t[:, :, t0:t0 + n], t0, n, 16, sqb, rsb)
        Sf = kb.sb(st, "Sf", [128, 2, 512], F32)
        Sb = kb.sb(st, "Sb", [128, 2, 512], BF16)
        Wqk = [kb.sb(st, "Wqk", [128, 8, 512], BF16) for _ in range(1)]
        Wv = kb.sb(st, "Wv", [128, 8, 512], BF16)
        Wg = kb.sb(st, "Wg", [128, 8, 512], BF16)
        Wo = kb.sb(st, "Wor", [128, 4, 1024], BF16)
        RT = [kb.sb(st, "rt", [128, 2, 512], F32) for _ in range(2)]
        QK = [kb.sb(st, "qk", [128, 4, 512], BF16) for _ in range(2)]
        OGT = [kb.sb(st, "ogT", [128, 4, 512], BF16) for _ in range(2)]
        t1 = kb.sb(st, "t1", [128, 512], F32)
        t2 = kb.sb(st, "t2", [128, 512], F32)
        VB = [kb.sb(st, "vb", [128, 512], BF16) for _ in range(2)]
        GT = [kb.sb(st, "gt", [128, 512], BF16) for _ in range(2)]
        ATM = [kb.sb(st, "atm", [128, 128], BF16) for _ in range(2)]
        QD = [kb.sb(st, "qd", [128, 2, 128], BF16) for _ in range(2)]
        OG = [kb.sb(st, "og", [128, 512], BF16) for _ in range(2)]
        KD = [kb.sb(st, "kd", [128, 256], BF16) for _ in range(2)]
        SS = kb.sb(st, "ss", [128, 2], F32)
        junk = kb.sb(st, "junkr", [128, 512], BF16)
        SST = [kb.sb(st, "sst", [128, 2, 512], F32) for _ in range(2)]
        S0B = [kb.sb(st, "s0b", [128, 2, 512], BF16) for _ in range(2)]
        QP = [kb.sb(st, "qp", [128, 2, 64], BF16) for _ in range(2)]
        KDB = [kb.sb(st, "kdb", [128, 256], BF16) for _ in range(2)]
        wsrc = w_in.rearrange("(kc p) n -> p kc n", p=128)
        gidx = [0]
        for h in range(4):
            wqk = Wqk[0]
            kb.dma(POOL, wqk.t[:, :, 0:256], wsrc[:, :, h * 256:(h + 1) * 256], writes=[wqk])
            kb.dma(POOL, wqk.t[:, :, 256:512], wsrc[:, :, 1024 + h * 256:1024 + (h + 1) * 256], writes=[wqk])
            kb.dma(POOL, Wv.t[:], wsrc[:, :, 2048 + h * 512:2048 + (h + 1) * 512], writes=[Wv])
            kb.dma(POOL, Wg.t[:], wsrc[:, :, 4096 + h * 512:4096 + (h + 1) * 512], writes=[Wg])
            kb.dma(POOL, Wo.t[:], w_out[h * 512:(h + 1) * 512, :].rearrange("(c p) n -> p c n", p=128), writes=[Wo])
            dmh = C("dm")[:, h * 128:(h + 1) * 128]
            qdh = C("qd")[:, h * 128:(h + 1) * 128]
            cdec = float(np.exp(np.log1p(-np.exp2(-5.0 - h)) * 128.0))
            cdec4 = float(np.exp(np.log1p(-np.exp2(-5.0 - h)) * 4.0))

            def gn_gate(po, rows, gt, og):
                kb.op(ACT, lambda: S.activation(out=junk.t[0:rows, :], in_=po.t[0:rows, :], func=AF.Square,
                                                accum_out=SS.t[0:rows, 0:1]), reads=[po], writes=[junk, SS])
                kb.op(ACT, lambda: S.activation(out=SS.t[0:rows, 0:1], in_=SS.t[0:rows, 0:1], func=AF.Sqrt,
                                                bias=EPSB_.t[0:rows, 0:1], scale=1.0 / 512), reads=[SS, EPSB_], writes=[SS])
                kb.op(DVE, lambda: V.reciprocal(out=SS.t[0:rows, 0:1], in_=SS.t[0:rows, 0:1]), reads=[SS], writes=[SS])
                kb.op(DVE, lambda: V.scalar_tensor_tensor(out=og.t[0:rows, :], in0=po.t[0:rows, :], scalar=SS.t[0:rows, 0:1],
                                                          in1=gt.t[0:rows, :], op0=ALU.mult, op1=ALU.mult),
                      reads=[po, SS, gt], writes=[og])

            def qkproj(ti):
                t0, n = TT[ti]
                rtile = RT[ti % 2]
                kb.dma(SP, rtile.t[:, :, 0:n], rot[:, :, t0:t0 + n], writes=[rtile])
                qkt = QK[ti % 2]
                cos = rtile.t[:, 0, 0:n]
                sin = rtile.t[:, 1, 0:n]
                for which in range(2):
                    sc = 1.0 if which == 0 else 1.0 / 16
                    pa, pb = PS[0], PS[1]
                    kb.mm(pa, pa.t[:, 0:n], [(wqk.t[:, kc, which * 256:which * 256 + 128], hT.t[:, kc, t0:t0 + n]) for kc in range(8)], [wqk, hT])
                    kb.mm(pb, pb.t[:, 0:n], [(wqk.t[:, kc, which * 256 + 128:which * 256 + 256], hT.t[:, kc, t0:t0 + n]) for kc in range(8)], [wqk, hT])
                    for part in range(2):
                        ca, cb = (cos, sin) if part == 0 else (sin, cos)
                        kb.op(DVE, lambda: V.scalar_tensor_tensor(out=t1.t[:, 0:n], in0=pa.t[:, 0:n], scalar=sc, in1=ca, op0=ALU.mult, op1=ALU.mult),
                              reads=[pa, rtile], writes=[t1])
                        kb.op(DVE, lambda: V.scalar_tensor_tensor(out=t2.t[:, 0:n], in0=pb.t[:, 0:n], scalar=sc, in1=cb, op0=ALU.mult, op1=ALU.mult),
                              reads=[pb, rtile], writes=[t2])
                        kb.op(DVE, lambda: V.tensor_tensor(out=qkt.t[:, 2 * which + part, 0:n], in0=t1.t[:, 0:n], in1=t2.t[:, 0:n],
                                                           op=(ALU.subtract if part == 0 else ALU.add)), reads=[t1, t2], writes=[qkt])

            qkproj(0)
            for ti, (t0, n) in enumerate(TT):
                is_s = (ti == 4)
                qkt = QK[ti % 2]
                if ti + 1 < len(TT):
                    qkproj(ti + 1)
                ogt = OGT[ti % 2]
                if not is_s:
                    def stageA(cc):
                        gi = ti * 4 + cc
                        first = (ti == 0 and cc == 0)
                        cs = cc * 128
                        tok0 = t0 + cs
                        vb, gt, atm, qd, kd = VB[gi % 2], GT[gi % 2], ATM[gi % 2], QD[gi % 2], KD[gi % 2]
                        pv, pg = PS[0], PS[1]
                        kb.mm(pv, pv.t[:], [(hT.t[:, kc, tok0:tok0 + 128], Wv.t[:, kc, :]) for kc in range(8)], [hT, Wv])
                        kb.mm(pg, pg.t[:], [(hT.t[:, kc, tok0:tok0 + 128], Wg.t[:, kc, :]) for kc in range(8)], [hT, Wg])
                        kb.op(ACT, lambda: S.copy(out=vb.t[:], in_=pv.t[:]), reads=[pv], writes=[vb])
                        kb.op(ACT, lambda: S.activation(out=gt.t[:], in_=pg.t[:], func=AF.Silu), reads=[pg], writes=[gt])
                        pa = PS[2]
                        kb.mm(pa, pa.t[:, 0:128], [(qkt.t[:, 2 + hf, cs:cs + 128], qkt.t[:, hf, cs:cs + 128]) for hf in range(2)], [qkt])
                        kb.op(DVE, lambda: V.tensor_tensor(out=atm.t[:], in0=pa.t[:, 0:128], in1=dmh, op=ALU.mult), reads=[pa, CST], writes=[atm])
                        if not first:
                            kb.op(DVE, lambda: V.tensor_tensor(out=qd.t[:], in0=qkt.t[:, 0:2, cs:cs + 128],
                                                               in1=qdh.unsqueeze(1).to_broadcast([128, 2, 128]), op=ALU.mult),
                                  reads=[qkt, CST], writes=[qd])
                        pab = pa.t[:].bitcast(BF16)
                        kb.transposes(pa, [(pab[:, 256 + hf * 128:256 + (hf + 1) * 128], qkt.t[:, 2 + hf, cs:cs + 128]) for hf in range(2)], [qkt], identb)
                        kb.op(DVE, lambda: V.tensor_scalar(out=kd.t[:], in0=pab[:, 256:512], scalar1=C("kd")[:, h:h + 1], scalar2=None,
                                                           op0=ALU.mult), reads=[pa, CST], writes=[kd])

                    def stageB(cc):
                        gi = ti * 4 + cc
                        first = (ti == 0 and cc == 0)
                        cs = cc * 128
                        vb, gt, atm, qd, og, kd = VB[gi % 2], GT[gi % 2], ATM[gi % 2], QD[gi % 2], OG[gi % 2], KD[gi % 2]
                        po = PS[3]
                        pairs = [(atm.t[:], vb.t[:])]
                        rds = [atm, vb]
                        if not first:
                            pairs += [(qd.t[:, hf, :], Sb.t[:, hf, :]) for hf in range(2)]
                            rds += [qd, Sb]
                        kb.mm(po, po.t[:], pairs, rds)
                        for hf in range(2):
                            ps_ = PS[5 + hf]
                            kb.mm(ps_, ps_.t[:], [(kd.t[:, hf * 128:(hf + 1) * 128], vb.t[:])], [kd, vb])
                            if first:
                                kb.op(DVE, lambda: V.tensor_copy(out=Sf.t[:, hf, :], in_=ps_.t[:]), reads=[ps_], writes=[Sf])
                            else:
                                kb.op(DVE, lambda: V.scalar_tensor_tensor(out=Sf.t[:, hf, :], in0=Sf.t[:, hf, :], scalar=cdec, in1=ps_.t[:],
                                                                          op0=ALU.mult, op1=ALU.add), reads=[Sf, ps_], writes=[Sf])
                        kb.op(ACT, lambda: S.copy(out=Sb.t[:], in_=Sf.t[:]), reads=[Sf], writes=[Sb])
                        gn_gate(po, 128, gt, og)
                        pt = PS[4]
                        ptb = pt.t[:].bitcast(BF16)
                        kb.transposes(pt, [(ptb[:, ec * 128:(ec + 1) * 128], og.t[:, ec * 128:(ec + 1) * 128]) for ec in range(4)], [og], identb)
                        kb.op(ACT, lambda: S.copy(out=ogt.t[:, :, cs:cs + 128], in_=ptb[:, 0:512].rearrange("p (e t) -> p e t", e=4)),
                              reads=[pt], writes=[ogt])

                    stageA(0)
                    for cc in range(4):
                        if cc + 1 < 4:
                            stageA(cc + 1)
                        stageB(cc)
                    if ti == 3:
                        kb.dma(SP, r_p[h].rearrange("(hf p) e -> p hf e", p=128), Sf.t[:], reads=[Sf])
                else:
                    vb, gt, atm, og, kd = VB[0], GT[0], ATM[0], OG[0], KD[0]
                    pv, pg = PS[0], PS[1]
                    kb.mm(pv, pv.t[0:NS, :], [(hT.t[:, kc, t0:t0 + NS], Wv.t[:, kc, :]) for kc in range(8)], [hT, Wv])
                    kb.mm(pg, pg.t[0:NS, :], [(hT.t[:, kc, t0:t0 + NS], Wg.t[:, kc, :]) for kc in range(8)], [hT, Wg])
                    kb.op(ACT, lambda: S.copy(out=vb.t[0:NS, :], in_=pv.t[0:NS, :]), reads=[pv], writes=[vb])
                    kb.op(ACT, lambda: S.activation(out=gt.t[0:NS, :], in_=pg.t[0:NS, :], func=AF.Silu), reads=[pg], writes=[gt])
                    pa = PS[2]
                    kb.mm(pa, pa.t[0:NS, 0:NS], [(qkt.t[:, 2 + hf, 0:NS], qkt.t[:, hf, 0:NS]) for hf in range(2)], [qkt])
                    kb.op(DVE, lambda: V.tensor_tensor(out=atm.t[0:NS, 0:NS], in0=pa.t[0:NS, 0:NS], in1=C("dmS")[0:NS, h * 64:(h + 1) * 64],
                                                       op=ALU.mult), reads=[pa, CST], writes=[atm])
                    qds = QD[0]
                    kb.op(DVE, lambda: V.tensor_tensor(out=qds.t[:, :, 0:NS], in0=qkt.t[:, 0:2, 0:NS],
                                                       in1=C("qdS")[:, h * 64:(h + 1) * 64].unsqueeze(1).to_broadcast([128, 2, NS]), op=ALU.mult),
                          reads=[qkt, CST], writes=[qds])
                    pab = pa.t[:].bitcast(BF16)
                    kb.transposes(pa, [(pab[0:NS, 256 + hf * 128:256 + (hf + 1) * 128], qkt.t[:, 2 + hf, 0:NS]) for hf in range(2)], [qkt], identb)
                    kb.op(DVE, lambda: V.tensor_scalar(out=kd.t[0:NS, :], in0=pab[0:NS, 256:512], scalar1=C("kdS")[0:NS, h:h + 1], scalar2=None,
                                                       op0=ALU.mult), reads=[pa, CST], writes=[kd])
                    po = PS[3]
                    kb.mm(po, po.t[0:NS, :], [(atm.t[0:NS, 0:NS], vb.t[0:NS, :])], [atm, vb], start=True, stop=False)
                    al_src = [RT[1], QK[1], OGT[1]]
                    al = []
                    for o_ in al_src:
                        ap_ = o_.t[:] if o_ is RT[1] else o_.t[:].rearrange("p a b -> p (a b)").bitcast(F32).rearrange("p (h e) -> p h e", h=2)
                        fz = dict(o_.rd)
                        if o_.lw and fz.get(o_.lw[0], (None, 0))[1] < o_.lw[2]:
                            fz[o_.lw[0]] = (o_.lw[1], o_.lw[2])
                        al.append(Buf(ap_, o_.name + "_al", fz))
                    stbufs = SST + al
                    def st_load(bb):
                        kb.dma(SP, stbufs[bb % 5].t[:], st_in[bb, h].rearrange("(hf p) e -> p hf e", p=128), writes=[stbufs[bb % 5]])
                    for bb in range(4):
                        st_load(bb)
                    for b in range(NSEQ):
                        stb, s0b, qp, kdb = stbufs[b % 5], S0B[b % 2], QP[b % 2], KDB[b % 2]
                        kb.op(ACT, lambda: S.copy(out=s0b.t[:], in_=stb.t[:]), reads=[stb], writes=[s0b])
                        kb.op(DVE, lambda: V.tensor_tensor(out=qp.t[:], in0=qds.t[:, :, 0:NS],
                                                           in1=C("bmc")[:, b * 64:(b + 1) * 64].unsqueeze(1).to_broadcast([128, 2, NS]), op=ALU.mult),
                              reads=[qds, CST], writes=[qp])
                        kb.mm(po, po.t[0:NS, :], [(qp.t[:, hf, :], s0b.t[:, hf, :]) for hf in range(2)], [qp, s0b], start=False, stop=(b == NSEQ - 1))
                        kb.op(DVE, lambda: V.tensor_scalar(out=kdb.t[0:NS, :], in0=kd.t[0:NS, :], scalar1=C("bm")[0:NS, b:b + 1], scalar2=None,
                                                           op0=ALU.mult), reads=[kd, CST], writes=[kdb])
                        for hf in range(2):
                            ps_ = PS[5 + hf]
                            kb.mm(ps_, ps_.t[:], [(kdb.t[0:NS, hf * 128:(hf + 1) * 128], vb.t[0:NS, :])], [kdb, vb])
                            kb.op(DVE, lambda: V.scalar_tensor_tensor(out=stb.t[:, hf, :], in0=stb.t[:, hf, :], scalar=cdec4, in1=ps_.t[:],
                                                                      op0=ALU.mult, op1=ALU.add), reads=[stb, ps_], writes=[stb])
                        kb.dma(SP, r_s[b, h].rearrange("(hf p) e -> p hf e", p=128), stb.t[:], reads=[stb])
                        if b + 4 < NSEQ:
                            st_load(b + 4)
                    for o_, a_ in zip(al_src, al):
                        for tk in ([a_.lw] if a_.lw else []):
                            if o_.rd.get(tk[0], (None, 0))[1] < tk[2]:
                                o_.rd[tk[0]] = (tk[1], tk[2])
                        for k_, v_ in a_.rd.items():
                            if o_.rd.get(k_, (None, 0))[1] < v_[1]:
                                o_.rd[k_] = v_
                        if a_.dsem:
                            kb.dpool.extend(a_.dsem.values())
                    gn_gate(po, NS, gt, og)
                    pt = PS[4]
                    ptb = pt.t[:].bitcast(BF16)
                    kb.transposes(pt, [(ptb[:, ec * 128:ec * 128 + NS], og.t[0:NS, ec * 128:(ec + 1) * 128]) for ec in range(4)], [og], identb)
                    kb.op(ACT, lambda: S.copy(out=ogt.t[:, :, 0:NS], in_=ptb[:, 0:512].rearrange("p (e t) -> p e t", e=4)[:, :, 0:NS]),
                          reads=[pt], writes=[ogt])
                for oc in range(8):
                    pb = PS[7]
                    kb.mm(pb, pb.t[:, 0:n], [(Wo.t[:, ec, oc * 128:(oc + 1) * 128], ogt.t[:, ec, 0:n]) for ec in range(4)], [Wo, ogt])
                    kb.op(DVE, lambda: V.tensor_tensor(out=xT.t[:, oc, t0:t0 + n], in0=xT.t[:, oc, t0:t0 + n], in1=pb.t[:, 0:n], op=ALU.add),
                          reads=[xT, pb], writes=[xT])


_CACHE = {}
LEVEL = 6
DEBUG = False
DBG_OUT = {}


def kernel(x_prompt, x_sample, cache_k, cache_v, cache_kidx, state_ret, page_table, rel_bias, ln_mix, ln_mlp,
           att_w_in, att_q_gain, att_k_gain, att_w_out, ret_w_in, ret_w_out, mlp_w_in, mlp_w_out):
    if "nc" not in _CACHE:
        _CACHE["nc"] = build_program(LEVEL)
    nc, (cst_np, cstr_np) = _CACHE["nc"]
    rot = _build_rot()
    f = lambda a: np.ascontiguousarray(np.asarray(a, dtype=np.float32))
    gl = np.concatenate([np.asarray(v, np.float32).reshape(8, 128).T for v in (ln_mix[0], ln_mlp[0], ln_mix[1], ln_mlp[1])], axis=1)
    shared = {
        "cache_k": f(cache_k).reshape(NPHYS * 128, 256), "cache_v": f(cache_v).reshape(NPHYS * 128, 256),
        "cache_i": f(cache_kidx).reshape(NPHYS * 128, 64),
        "relb": f(rel_bias).reshape(1, 256), "gains": np.ascontiguousarray(gl),
        "qkg": np.ascontiguousarray(np.stack([f(att_q_gain)[0], f(att_k_gain)[0]], axis=1)),
        "kg_row": f(att_k_gain).reshape(1, 128),
        "w_att_in": f(att_w_in)[0], "w_att_out": f(att_w_out)[0], "w_ret_in": f(ret_w_in)[0], "w_ret_out": f(ret_w_out)[0],
        "w_mlp_in": f(mlp_w_in), "w_mlp_out": f(mlp_w_out), "cst": cst_np, "cstr": cstr_np, "rot": rot,
    }
    xp = f(x_prompt)
    xs = f(x_sample)
    stt = f(state_ret)
    pt = np.ascontiguousarray(np.asarray(page_table, dtype=np.int32))
    in_maps = []
    for i in range(NCORES):
        m = dict(shared)
        m["x_p"] = xp[i]
        m["x_s"] = xs[16 * i:16 * i + 16].reshape(NS, D)
        m["st_in"] = stt[0, 16 * i:16 * i + 16]
        m["ptab"] = pt[16 * i:16 * i + 16].reshape(1, 256)
        in_maps.append(m)
    res = run_bass_kernel_spmd(nc, in_maps, core_ids=list(range(NCORES)))
    R = res.results
    if DEBUG:
        for k_ in ('dbgI', 'dbgA', 'dbgL', 'dbgN', 'dbgD', 'dbgW', 'dbgG', 'dbgB', 'dbgT', 'dbgM'):
            DBG_OUT[k_] = np.asarray(R[0][k_])
    cat = lambda k: np.stack([np.asarray(r[k]) for r in R])
    y_p = cat("y_p").astype(np.float32)
    y_s = cat("y_s").reshape(128, 4, D).astype(np.float32)
    k_p = cat("k_p").reshape(1, 8, SEQ, 2, 128).astype(np.float32)
    v_p = cat("v_p").reshape(1, 8, SEQ, 2, 128).astype(np.float32)
    i_p = cat("i_p").reshape(1, 8, SEQ, 64).astype(np.float32)
    r_p = cat("r_p").reshape(1, 8, 4, 256, 512).astype(np.float32)
    k_s = cat("k_s").reshape(1, 128, 4, 2, 128).astype(np.float32)
    v_s = cat("v_s").reshape(1, 128, 4, 2, 128).astype(np.float32)
    i_s = cat("i_s").reshape(1, 128, 4, 64).astype(np.float32)
    r_s = cat("r_s").reshape(1, 128, 4, 256, 512).astype(np.float32)
    return (y_p, y_s, k_p, v_p, i_p, r_p, k_s, v_s, i_s, r_s)
```

```python
import math
from contextlib import ExitStack
import numpy as np
import concourse.bass as bass
import concourse.mybir as mybir
from concourse.bass_utils import run_bass_kernel_spmd

F32 = mybir.dt.float32
BF16 = mybir.dt.bfloat16
I32 = mybir.dt.int32
ALU = mybir.AluOpType
AF = mybir.ActivationFunctionType
AX = mybir.AxisListType

NCORES = 8
D = 1024
SEQ = 2048
NS = 64
NSEQ = 16
T = SEQ + NS
NPHYS = 2560
EPS = 1e-6
NEG = -30000.0
BIGNEG = -1.0e30
NBIS = 24
TT = [(0, 512), (512, 512), (1024, 512), (1536, 512), (2048, 64)]
ATT_IN = 1860
GAM = [1.0 - 2.0 ** (-5.0 - h) for h in range(4)]


def _t5_bucket(dist):
    n = np.maximum(dist, 0)
    nf = np.maximum(n, 1).astype(np.float32)
    large = 16 + (np.log(nf / np.float32(16)) / np.float32(math.log(128 / 16)) * np.float32(16)).astype(np.int32)
    large = np.minimum(large, 31)
    return np.where(n < 16, n, large)


def _build_consts():
    c = {}
    p = np.arange(128)
    c["ident"] = np.eye(128, dtype=np.float32)
    t = p[:, None]
    s = p[None, :]
    c["causneg"] = np.where(s <= t, 0.0, BIGNEG).astype(np.float32)
    delta = np.arange(256)[None, :] - p[:, None]
    c["bkt_p"] = np.where(delta >= 0, _t5_bucket(delta), 31).astype(np.float32)
    pos = (128 * (p // 8) + 16 * (p % 8))[:, None, None] + np.arange(16)[None, :, None]
    dl = 2048 + np.arange(4)[None, None, :] - pos
    c["bkt_s"] = _t5_bucket(dl).astype(np.float32).reshape(128, 64)
    bp = np.arange(64) // 4
    tp = np.arange(64) % 4
    valid = (bp[:, None, None] == np.arange(16)[None, :, None]) & (tp[:, None, None] <= np.arange(4)[None, None, :])
    c["newvalid"] = np.where(valid, 0.0, BIGNEG).astype(np.float32).reshape(64, 64)
    c["newvalid"] = np.concatenate([c["newvalid"], np.zeros((64, 64), np.float32)], 0)
    bn = np.clip(np.arange(4)[None, None, :] - tp[:, None, None], 0, 31) * np.ones((1, 16, 1))
    c["bkt_n"] = np.concatenate([bn.reshape(64, 64).astype(np.float32), np.full((64, 64), 31, np.float32)], 0)
    i = np.arange(128, dtype=np.float64)
    dm = np.zeros((128, 4, 128), np.float64)
    qd = np.zeros((128, 4, 128), np.float64)
    kd = np.zeros((128, 4), np.float64)
    dmS = np.zeros((128, 4, 64), np.float64)
    qdS = np.zeros((128, 4, 64), np.float64)
    kdS = np.zeros((128, 4), np.float64)
    for h in range(4):
        lg = np.log1p(-np.exp2(-5.0 - h))
        diff = i[None, :] - i[:, None]
        dm[:, h, :] = np.where(diff >= 0, np.exp(lg * np.maximum(diff, 0)), 0.0)
        qd[:, h, :] = np.exp(lg * (i + 1.0))[None, :]
        kd[:, h] = np.exp(lg * (127.0 - i))
        ii = np.arange(64)
        dS = (ii % 4)[None, :] - (ii % 4)[:, None]
        same = (ii // 4)[None, :] == (ii // 4)[:, None]
        dmS[:64, h, :] = np.where(same & (dS >= 0), np.exp(lg * np.maximum(dS, 0)), 0.0)
        qdS[:, h, :] = np.exp(lg * ((ii % 4) + 1.0))[None, :]
        kdS[:64, h] = np.exp(lg * (3.0 - (ii % 4)))
    c["dm"] = dm.reshape(128, 512).astype(np.float32)
    c["qd"] = qd.reshape(128, 512).astype(np.float32)
    c["kd"] = kd.astype(np.float32)
    c["dmS"] = dmS.reshape(128, 256).astype(np.float32)
    c["qdS"] = qdS.reshape(128, 256).astype(np.float32)
    c["kdS"] = kdS.astype(np.float32)
    bm = np.zeros((128, 16), np.float32)
    bm[np.arange(64), np.arange(64) // 4] = 1.0
    c["bm"] = bm
    bmc = np.zeros((128, 16, 64), np.float32)
    for b in range(16):
        bmc[:, b, 4 * b:4 * b + 4] = 1.0
    c["bmc"] = bmc.reshape(128, 1024)
    c["iota_p"] = np.arange(128, dtype=np.float32).reshape(128, 1)
    c["pofs"] = (16 * (np.arange(128) % 8)).astype(np.float32).reshape(128, 1)
    sel = np.zeros((128, 16), np.float32)
    sel[np.arange(128), np.arange(128) // 8] = 1.0
    c["pagesel"] = sel
    dd = 255 - np.arange(383)
    bk_d = np.where(dd >= 0, _t5_bucket(dd), 31)
    gr = np.zeros((128, 383), np.float32)
    gr[bk_d, np.arange(383)] = 1.0
    c["gr"] = gr
    c["bkt_all"] = np.concatenate([c.pop("bkt_p"), c.pop("bkt_s"), c.pop("bkt_n")], axis=1)
    c["pow2"] = np.tile((2.0 ** -(np.arange(NBIS) + 1.0)).astype(np.float32)[None, :], (128, 1))
    rkeys = ("dm", "qd", "kd", "dmS", "qdS", "kdS", "bm", "bmc", "gr")
    outs = []
    for keys in ([k for k in c if k not in rkeys], list(rkeys)):
        offs = {}
        cols = []
        o = 0
        for k in keys:
            v = c[k]
            offs[k] = (o, v.shape[1])
            cols.append(v)
            o += v.shape[1]
        outs.append((np.ascontiguousarray(np.concatenate(cols, axis=1)), offs))
    return outs


def _build_rot():
    half = 128
    theta = (1.0 / (10000.0 ** np.linspace(0.0, 1.0, half, dtype=np.float32))).astype(np.float32)
    pos = np.concatenate([np.arange(SEQ), np.tile(2048 + np.arange(4), NSEQ)]).astype(np.float32)
    ang = (theta[:, None] * pos[None, :]).astype(np.float32)
    return np.ascontiguousarray(np.stack([np.cos(ang), np.sin(ang)], axis=1).astype(np.float32))


class Buf:
    def __init__(self, t, name, fence):
        self.t = t
        self.name = name
        self.lw = None
        self.rd = dict(fence)
        self.dsem = None
        self.dcnt = 0
        self.excl = False

    def __getitem__(self, k):
        return self.t[k]


class Eng:
    def __init__(self, h, sem, key, is_pe=False):
        self.h = h
        self.sem = sem
        self.key = key
        self.cnt = 0
        self.seen = {}
        self.is_pe = is_pe


class KB:
    def __init__(self, nc):
        self.nc = nc
        self.es = ExitStack()
        self.fence = {}
        self.dma_bufs = []
        self.nsem = 0
        self.uid = 0
        self.dpool = []
        self.dsems = []
        mk = lambda n: self.es.enter_context(nc.semaphore(n))
        self.PE = Eng(nc.tensor, mk("s_pe"), "pe", True)
        self.ACT = Eng(nc.scalar, mk("s_act"), "act")
        self.DVE = Eng(nc.vector, mk("s_dve"), "dve")
        self.POOL = Eng(nc.gpsimd, mk("s_pool"), "pool")
        self.SP = Eng(nc.sync, mk("s_sp"), "sp")

    def sb(self, st, name, shape, dt):
        self.uid += 1
        t = st.enter_context(self.nc.sbuf_tensor("%s_%d" % (name, self.uid), list(shape), dt))
        b = Buf(t, name, self.fence)
        st.callback(self._free, b)
        return b

    def _free(self, b):
        for tk in ([b.lw] if b.lw else []) + list(b.rd.items()):
            if isinstance(tk, tuple) and len(tk) == 2 and isinstance(tk[1], tuple):
                key, (sem, val) = tk
            else:
                key, sem, val = tk
            if self.fence.get(key, (None, 0))[1] < val:
                self.fence[key] = (sem, val)
        if b.dsem is not None:
            self.dpool.extend(b.dsem.values())
            b.dsem = None

    def psum(self, name):
        t = self.es.enter_context(self.nc.psum_tensor(name, [128, 512], F32))
        b = Buf(t, name, {})
        b.excl = True
        return b

    def _deps(self, eng, reads, writes):
        deps = {}

        def add(key, sem, val, war):
            if key == eng.key and eng.is_pe:
                return
            if deps.get(key, (None, 0))[1] < val:
                deps[key] = (sem, val)

        for b in reads:
            if b.lw:
                add(b.lw[0], b.lw[1], b.lw[2], False)
            if b.excl:
                for key, (sem, val) in b.rd.items():
                    if key != eng.key:
                        add(key, sem, val, True)
        for b in writes:
            if b.lw:
                add(b.lw[0], b.lw[1], b.lw[2], False)
            for key, (sem, val) in b.rd.items():
                add(key, sem, val, True)
        for key, (sem, val) in deps.items():
            if eng.seen.get(key, 0) < val:
                eng.h.wait_ge(sem, val)
                eng.seen[key] = val

    def _commit(self, tk, reads, writes):
        key, sem, val = tk
        for b in reads:
            if b.rd.get(key, (None, 0))[1] < val:
                b.rd[key] = (sem, val)
        for b in writes:
            b.lw = tk
            b.rd = {}

    def op(self, eng, fn, reads=(), writes=()):
        self._deps(eng, reads, writes)
        inst = fn()
        eng.cnt += 1
        inst.then_inc(eng.sem, 1)
        self._commit((eng.key, eng.sem, eng.cnt), reads, writes)

    def mm(self, out_buf, out_ap, pairs, reads, start=True, stop=True):
        eng = self.PE
        self._deps(eng, reads, [out_buf])
        n = len(pairs)
        inst = None
        for i, (l, r) in enumerate(pairs):
            inst = self.nc.tensor.matmul(out_ap, lhsT=l, rhs=r, start=(start and i == 0), stop=(stop and i == n - 1))
        eng.cnt += 1
        inst.then_inc(eng.sem, 1)
        self._commit((eng.key, eng.sem, eng.cnt), reads, [out_buf])

    def transposes(self, out_buf, items, reads, ident):
        eng = self.PE
        self._deps(eng, list(reads) + [ident], [out_buf])
        inst = None
        for (o, i) in items:
            inst = self.nc.tensor.transpose(o, i, ident.t[0:i.shape[0], 0:i.shape[0]])
        eng.cnt += 1
        inst.then_inc(eng.sem, 1)
        self._commit((eng.key, eng.sem, eng.cnt), list(reads) + [ident], [out_buf])

    def dma(self, q, out_ap, in_ap, reads=(), writes=(), indirect=None):
        self._deps(q, reads, writes)
        b = (list(writes) + list(reads))[0]
        if b.dsem is None:
            b.dsem = {}
        if q.key not in b.dsem:
            pool = [d for d in self.dpool if d[3] == q.key]
            if pool:
                d0 = pool[-1]
                b.dsem[q.key] = d0
                self.dpool.remove(d0)
                if q.seen.get(d0[1], 0) < d0[2]:
                    q.h.wait_ge(d0[0], d0[2])
                    q.seen[d0[1]] = d0[2]
            else:
                self.nsem += 1
                ds = [self.es.enter_context(self.nc.semaphore("dsem%d" % self.nsem)), "dsem%d" % self.nsem, 0, q.key]
                self.dsems.append(ds)
                b.dsem[q.key] = ds
        ds = b.dsem[q.key]
        ds[2] += 16
        if indirect is not None:
            inst = q.h.indirect_dma_start(out=out_ap, out_offset=None, in_=in_ap, in_offset=indirect)
        else:
            inst = q.h.dma_start(out=out_ap, in_=in_ap)
        inst.then_inc(ds[0], 16)
        self._commit((ds[1], ds[0], ds[2]), reads, writes)

    def finish(self):
        for ds in self.dsems:
            if self.SP.seen.get(ds[1], 0) < ds[2]:
                self.nc.sync.wait_ge(ds[0], ds[2])
                self.SP.seen[ds[1]] = ds[2]
        for e in (self.PE, self.ACT, self.DVE, self.POOL):
            if e.cnt > 0:
                self.nc.sync.wait_ge(e.sem, e.cnt)


def build_program(level=99):
    nc = bass.Bass("TRN2", target_bir_lowering=False)
    (cst_np, CO), (cstr_np, COR) = _build_consts()
    NCST = cst_np.shape[1]
    dr = lambda n, s, dt=F32, kind="ExternalInput": nc.dram_tensor(n, list(s), dt, kind=kind).ap()
    x_p = dr("x_p", [SEQ, D])
    x_s = dr("x_s", [NS, D])
    cache_k = dr("cache_k", [NPHYS * 128, 256])
    cache_v = dr("cache_v", [NPHYS * 128, 256])
    cache_i = dr("cache_i", [NPHYS * 128, 64])
    st_in = dr("st_in", [NSEQ, 4, 256, 512])
    ptab = dr("ptab", [1, NSEQ * 16], I32)
    relb = dr("relb", [1, 256])
    gains = dr("gains", [128, 32])
    qkg = dr("qkg", [128, 2])
    kg_row = dr("kg_row", [1, 128])
    w_att_in = dr("w_att_in", [D, ATT_IN])
    w_att_out = dr("w_att_out", [D, D])
    w_ret_in = dr("w_ret_in", [D, 6144])
    w_ret_out = dr("w_ret_out", [2048, D])
    w_mlp_in = dr("w_mlp_in", [2, D, 4096])
    w_mlp_out = dr("w_mlp_out", [2, 4096, D])
    cst = dr("cst", [128, NCST])
    cstr = dr("cstr", [128, cstr_np.shape[1]])
    rot = dr("rot", [128, 2, T])
    OUT = "ExternalOutput"
    y_p = dr("y_p", [SEQ, D], kind=OUT)
    y_s = dr("y_s", [NS, D], kind=OUT)
    k_p = dr("k_p", [SEQ, 256], kind=OUT)
    v_p = dr("v_p", [SEQ, 256], kind=OUT)
    i_p = dr("i_p", [SEQ, 64], kind=OUT)
    r_p = dr("r_p", [4, 256, 512], kind=OUT)
    k_s = dr("k_s", [NS, 256], kind=OUT)
    v_s = dr("v_s", [NS, 256], kind=OUT)
    i_s = dr("i_s", [NS, 64], kind=OUT)
    r_s = dr("r_s", [NSEQ, 4, 256, 512], kind=OUT)

    DBG = {}
    if DEBUG:
        DBG["I"] = dr("dbgI", [128, 32], I32, kind=OUT)
        DBG["A"] = dr("dbgA", [128, 1024], kind=OUT)
        DBG["L"] = dr("dbgL", [128, 64], kind=OUT)
        DBG["N"] = dr("dbgN", [128, 64], kind=OUT)
        DBG["D"] = dr("dbgD", [128, 32], kind=OUT)
        DBG["W"] = dr("dbgW", [128, 256], kind=OUT)
        DBG["G"] = dr("dbgG", [128, 1024], kind=OUT)
        DBG["B"] = dr("dbgB", [128, 2048], BF16, kind=OUT)
        DBG["T"] = dr("dbgT", [128, 2048], BF16, kind=OUT)
        DBG["M"] = dr("dbgM", [128, 256], kind=OUT)
    kb = KB(nc)
    kb.DBG = DBG
    PE, ACT, DVE, POOL, SP = kb.PE, kb.ACT, kb.DVE, kb.POOL, kb.SP
    V = nc.vector
    S = nc.scalar
    G = nc.gpsimd
    PS = [kb.psum("ps%d" % i) for i in range(8)]
    top = kb.es

    def C(name, rows=128):
        o, w = CO[name]
        return CST.t[0:rows, o:o + w]

    CST = kb.sb(top, "cst", [128, NCST], F32)
    kb.dma(SP, CST.t[:], cst, writes=[CST])
    xT = kb.sb(top, "xT", [128, 8, T], F32)
    identb = kb.sb(top, "identb", [128, 128], BF16)
    onesb = kb.sb(top, "onesb", [128, 128], BF16)
    ones_d = kb.sb(top, "ones_d", [128, 128], BF16)
    ones_h = kb.sb(top, "ones_h", [128, 128], BF16)
    gn = kb.sb(top, "gn", [128, 32], F32)
    qk_g = kb.sb(top, "qk_g", [128, 2], F32)
    kgbc = kb.sb(top, "kgbc", [128, 128], F32)
    kb.dma(SP, gn.t[:], gains, writes=[gn])
    kb.dma(SP, qk_g.t[:], qkg, writes=[qk_g])
    kb.dma(SP, kgbc.t[:], kg_row.partition_broadcast(128), writes=[kgbc])
    kb.op(DVE, lambda: V.tensor_copy(out=identb.t[:], in_=C("ident")), reads=[CST], writes=[identb])
    kb.op(DVE, lambda: V.memset(onesb.t[:], 1.0), writes=[onesb])
    kb.op(DVE, lambda: V.memset(ones_d.t[:], 1.0 / 1024), writes=[ones_d])
    kb.op(DVE, lambda: V.memset(ones_h.t[:], 1.0 / 128), writes=[ones_h])
    kb.op(DVE, lambda: V.tensor_scalar(out=qk_g.t[:, 0:1], in0=qk_g.t[:, 0:1], scalar1=128.0 ** -0.5, scalar2=None,
                                       op0=ALU.mult), reads=[qk_g], writes=[qk_g])

    class IdentF:
        pass
    identf = Buf(None, "identf", {})
    identf.t = CST.t[:, CO["ident"][0]:CO["ident"][0] + 128]
    identf_dep = CST

    rr = [0]

    def evac_engine():
        rr[0] ^= 1
        return ACT if rr[0] else DVE

    def copy(eng, out_ap, in_ap, reads, writes):
        if eng is ACT:
            kb.op(ACT, lambda: S.copy(out=out_ap, in_=in_ap), reads=reads, writes=writes)
        else:
            kb.op(DVE, lambda: V.tensor_copy(out=out_ap, in_=in_ap), reads=reads, writes=writes)

    with ExitStack() as st:
        xs = [kb.sb(st, "xs%d" % i, [128, D], F32) for i in range(2)]
        for c in range(17):
            n = 128 if c < 16 else NS
            src = x_p[c * 128:(c + 1) * 128, :] if c < 16 else x_s[:, :]
            xb = xs[c % 2]
            kb.dma(SP, xb.t[0:n, :], src, writes=[xb])
            for g in range(2):
                pb = PS[(2 * c + g) % 8]
                items = [(pb.t[:, j * 128:j * 128 + n], xb.t[0:n, (4 * g + j) * 128:(4 * g + j + 1) * 128]) for j in range(4)]
                eng = kb.PE
                kb._deps(eng, [xb, CST], [pb])
                inst = None
                for (o, i_) in items:
                    inst = nc.tensor.transpose(o, i_, identf.t[0:n, 0:n])
                eng.cnt += 1
                inst.then_inc(eng.sem, 1)
                kb._commit((eng.key, eng.sem, eng.cnt), [xb, CST], [pb])
                e = evac_engine()
                copy(e, xT.t[:, 4 * g:4 * g + 4, c * 128:c * 128 + n],
                     pb.t[:].rearrange("p (j t) -> p j t", j=4)[:, :, 0:n], [pb], [xT])

    def rmsnorm_tile(hbuf, h_ap, t0, n, gcol, sqb, rsb):
        kb.op(ACT, lambda: S.activation(out=sqb.t[:, :, 0:n], in_=xT.t[:, :, t0:t0 + n], func=AF.Square),
              reads=[xT], writes=[sqb])
        pb = PS[7]
        kb.mm(pb, pb.t[:, 0:n], [(ones_d.t[:], sqb.t[:, kc, 0:n]) for kc in range(8)], [ones_d, sqb])
        kb.op(ACT, lambda: S.activation(out=rsb.t[:, 0:n], in_=pb.t[:, 0:n], func=AF.Sqrt, bias=EPSB.t[:, 0:1], scale=1.0),
              reads=[pb, EPSB], writes=[rsb])
        kb.op(DVE, lambda: V.reciprocal(out=rsb.t[:, 0:n], in_=rsb.t[:, 0:n]), reads=[rsb], writes=[rsb])
        for kc in range(8):
            kb.op(DVE, lambda kc=kc: V.scalar_tensor_tensor(out=h_ap[:, kc, :], in0=xT.t[:, kc, t0:t0 + n],
                                                           scalar=gn.t[:, gcol + kc:gcol + kc + 1], in1=rsb.t[:, 0:n],
                                                           op0=ALU.mult, op1=ALU.mult),
                  reads=[xT, gn, rsb], writes=[hbuf])

    EPSB = kb.sb(top, "epsb", [128, 1], F32)
    kb.op(DVE, lambda: V.memset(EPSB.t[:], EPS), writes=[EPSB])

    def load_w(q, wbuf, w_ap_dst, src_ap):
        kb.dma(q, w_ap_dst, src_ap, writes=[wbuf])

    def add_resid(oc, t0, n, pb):
        kb.op(DVE, lambda: V.tensor_tensor(out=xT.t[:, oc, t0:t0 + n], in0=xT.t[:, oc, t0:t0 + n], in1=pb.t[:, 0:n],
                                           op=ALU.add), reads=[xT, pb], writes=[xT])

    with ExitStack() as L0:
        Win = kb.sb(L0, "Win", [128, 8, ATT_IN + 128], BF16)
        Wo = kb.sb(L0, "Wo", [128, 8, D], BF16)
        wsrc = w_att_in.rearrange("(kc p) n -> p kc n", p=128)
        for (a, b_) in [(1024, 1536), (1536, ATT_IN), (0, 512), (512, 1024)]:
            kb.dma(POOL, Win.t[:, :, a:b_], wsrc[:, :, a:b_], writes=[Win])
        kb.dma(POOL, Win.t[:, :, ATT_IN:ATT_IN + 64], wsrc[:, :, 1792:1856], writes=[Win])
        kb.dma(POOL, Win.t[:, :, ATT_IN + 64:ATT_IN + 128], wsrc[:, :, 1792:1856], writes=[Win])
        kb.dma(POOL, Wo.t[:], w_att_out.rearrange("(kc p) n -> p kc n", p=128), writes=[Wo])

        with ExitStack() as LK:
            kT = kb.sb(LK, "kT", [128, 2, T], BF16)
            Vall = kb.sb(LK, "Vall", [128, 17, 256], BF16)
            kiT = kb.sb(LK, "kiT", [128, T], BF16)
            WIa = kb.sb(LK, "WIa", [128, 17, 4], F32)
            WIs = kb.sb(LK, "WIs", [128, 17, 4], F32)
            biasN = kb.sb(LK, "biasN", [128, 2, 8, 128], BF16)
            biasS = kb.sb(LK, "biasS", [128, 16, 8, 4], F32)
            biasNn = kb.sb(LK, "biasNn", [128, 8, 64], F32)
            with ExitStack() as st:
                rb = kb.sb(st, "rb", [128, 256], F32)
                rbd = kb.sb(st, "rbd", [128, 256], F32)
                oh2 = kb.sb(st, "oh2", [128, 128], F32)
                tmp2 = kb.sb(st, "tmp2", [128, 8, 128], F32)
                bacc2 = kb.sb(st, "bacc2", [128, 8, 128], F32)
                rb32 = kb.sb(st, "rb32", [32, 8], F32)
                r31 = kb.sb(st, "r31", [32, 8], F32)
                rbdb = kb.sb(st, "rbdb", [32, 8], BF16)
                grb = kb.sb(st, "grb", [32, 383], BF16)
                kb.dma(SP, rb.t[:], relb.partition_broadcast(128), writes=[rb])
                kb.dma(SP, rb32.t[:], relb.rearrange("o (k h) -> (o k) h", h=8), writes=[rb32])
                kb.dma(SP, r31.t[:], relb[:, 248:256].partition_broadcast(32), writes=[r31])
                kb.op(DVE, lambda: V.tensor_tensor(out=rbdb.t[:], in0=rb32.t[:], in1=r31.t[:], op=ALU.subtract), reads=[rb32, r31], writes=[rbdb])
                grf = kb.sb(st, "grf", [32, 383], F32)
                kb.dma(SP, grf.t[:], cstr[0:32, COR["gr"][0]:COR["gr"][0] + 383], writes=[grf])
                kb.op(DVE, lambda: V.tensor_copy(out=grb.t[:], in_=grf.t[:]), reads=[grf], writes=[grb])
                for bank in range(4):
                    pbk = PS[bank]
                    items = []
                    for tl in range(64):
                        tp_ = bank * 64 + tl
                        items.append((pbk.t[:, tl * 8:tl * 8 + 8], grb.t[0:32, 255 - tp_:255 - tp_ + 128], rbdb.t[0:32, :]))
                    mm_multi(kb, nc, pbk, items, [grb, rbdb])
                    bb_, th = bank // 2, (bank % 2) * 64
                    copy_any(kb, nc, bank % 2, biasN.t[:, bb_, :, th:th + 64], pbk.t[:, 0:512].rearrange("p (t h) -> p h t", h=8), [pbk], [biasN])
                kb.op(POOL, lambda: G.tensor_tensor(out=rbd.t[:].rearrange("p (k h) -> p k h", h=8),
                                                    in0=rb.t[:].rearrange("p (k h) -> p k h", h=8),
                                                    in1=rb.t[:, 248:256].unsqueeze(1).to_broadcast([128, 32, 8]),
                                                    op=ALU.subtract), reads=[rb], writes=[rbd])
                kb.op(POOL, lambda: G.memset(bacc2.t[:], 0.0), writes=[bacc2])
                bk = C("bkt_all")
                for k in range(31):
                    kb.op(POOL, lambda k=k: G.tensor_single_scalar(out=oh2.t[:], in_=bk[:, 256:384], scalar=float(k), op=ALU.is_equal),
                          reads=[CST], writes=[oh2])
                    kb.op(POOL, lambda k=k: G.tensor_tensor(out=tmp2.t[:], in0=oh2.t[:].unsqueeze(1).to_broadcast([128, 8, 128]),
                                                            in1=rbd.t[:, k * 8:k * 8 + 8].unsqueeze(2).to_broadcast([128, 8, 128]), op=ALU.mult),
                          reads=[oh2, rbd], writes=[tmp2])
                    kb.op(POOL, lambda: G.tensor_tensor(out=bacc2.t[:], in0=bacc2.t[:], in1=tmp2.t[:], op=ALU.add),
                          reads=[bacc2, tmp2], writes=[bacc2])
                kb.op(POOL, lambda: G.tensor_copy(out=biasS.t[:], in_=bacc2.t[:, :, 0:64].rearrange("p h (c t) -> p c h t", t=4)),
                      reads=[bacc2], writes=[biasS])
                kb.op(POOL, lambda: G.tensor_copy(out=biasNn.t[:], in_=bacc2.t[:, :, 64:128]), reads=[bacc2], writes=[biasNn])

            for ti, (t0, n) in enumerate(TT):
                if level < 1:
                    break
                is_s = (ti == 4)
                with ExitStack() as TS:
                    qT = kb.sb(TS, "qT", [128, 8, 512], BF16)
                    qiT = kb.sb(TS, "qiT", [128, 2, 512], BF16)
                    WB = kb.sb(TS, "WB", [128, 16, 16], F32)
                    onT = kb.sb(TS, "onT", [128, 8, 512], BF16)
                    HS = ExitStack()
                    hT = kb.sb(HS, "hT", [128, 8, 512], BF16)
                    with ExitStack() as st:
                        sqb = kb.sb(st, "sqb", [128, 8, 512], BF16)
                        rsb = kb.sb(st, "rsb", [128, 512], F32)
                        rmsnorm_tile(hT, hT.t[:, :, 0:n], t0, n, 0, sqb, rsb)
                    with ExitStack() as st:
                        SQ = [kb.sb(st, "sq", [128, 512], BF16) for _ in range(2)]
                        QR = [kb.sb(st, "qraw", [128, 512], F32) for _ in range(2)]
                        RS = [kb.sb(st, "rs2", [128, 512], F32) for _ in range(2)]

                        def qs1(h):
                            pa = PS[h % 2]
                            sq, qraw = SQ[h % 2], QR[h % 2]
                            kb.mm(pa, pa.t[:, 0:n], [(Win.t[:, kc, h * 128:(h + 1) * 128], hT.t[:, kc, 0:n]) for kc in range(8)],
                                  [Win, hT])
                            kb.op(ACT, lambda: S.activation(out=sq.t[:, 0:n], in_=pa.t[:, 0:n], func=AF.Square),
                                  reads=[pa], writes=[sq])
                            kb.op(DVE, lambda: V.tensor_copy(out=qraw.t[:, 0:n], in_=pa.t[:, 0:n]), reads=[pa], writes=[qraw])

                        def qs2(h):
                            pb2 = PS[2 + h % 2]
                            sq, qraw, rs2 = SQ[h % 2], QR[h % 2], RS[h % 2]
                            kb.mm(pb2, pb2.t[:, 0:n], [(ones_h.t[:], sq.t[:, 0:n])], [ones_h, sq])
                            kb.op(ACT, lambda: S.activation(out=rs2.t[:, 0:n], in_=pb2.t[:, 0:n], func=AF.Sqrt,
                                                            bias=EPSB.t[:, 0:1], scale=1.0), reads=[pb2, EPSB], writes=[rs2])
                            kb.op(DVE, lambda: V.reciprocal(out=rs2.t[:, 0:n], in_=rs2.t[:, 0:n]), reads=[rs2], writes=[rs2])
                            kb.op(DVE, lambda: V.scalar_tensor_tensor(out=qT.t[:, h, 0:n], in0=qraw.t[:, 0:n],
                                                                      scalar=qk_g.t[:, 0:1], in1=rs2.t[:, 0:n],
                                                                      op0=ALU.mult, op1=ALU.mult),
                                  reads=[qraw, qk_g, rs2], writes=[qT])

                        qs1(0)
                        for h in range(8):
                            if h + 1 < 8:
                                qs1(h + 1)
                            qs2(h)
                        for j in range(3):
                            pa = PS[4 + j % 2]
                            c0 = 1536 + j * 128 if j < 2 else ATT_IN
                            kb.mm(pa, pa.t[:, 0:n], [(Win.t[:, kc, c0:c0 + 128], hT.t[:, kc, 0:n]) for kc in range(8)], [Win, hT])
                            if j < 2:
                                copy(evac_engine(), qiT.t[:, j, 0:n], pa.t[:, 0:n], [pa], [qiT])
                            else:
                                copy(evac_engine(), kiT.t[:, t0:t0 + n], pa.t[:, 0:n], [pa], [kiT])
                    if is_s and level >= 6:
                        with ExitStack() as st:
                            Wrep = kb.sb(st, "Wrep", [128, 8, 4, 128], BF16)
                            kb.op(DVE, lambda: V.tensor_copy(out=Wrep.t[:], in_=Win.t[:, :, 1856:1860].unsqueeze(3).to_broadcast([128, 8, 4, 128])),
                                  reads=[Win], writes=[Wrep])
                            pw = PS[6]
                            for h in range(4):
                                xs_ = (h % 2) * 2 + h // 2
                                kb.mm(pw, pw.t[:, xs_ * 64:(xs_ + 1) * 64], [(Wrep.t[:, kc, h, :], hT.t[:, kc, 0:NS]) for kc in range(8)], [Wrep, hT])
                            kb.op(DVE, lambda: V.tensor_copy(out=WB.t[:].rearrange("p b (x t) -> p b x t", x=4), in_=pw.t[:, 0:256].rearrange("p (x b t) -> p b x t", x=4, b=16)), reads=[pw], writes=[WB])
                    with ExitStack() as st:
                        ko = [kb.sb(st, "ko%d" % i, [128, 256], F32) for i in range(2)]
                        vo = [kb.sb(st, "vo%d" % i, [128, 256], F32) for i in range(2)]
                        io = [kb.sb(st, "io%d" % i, [128, 64], F32) for i in range(2)]
                        KBF = [kb.sb(st, "kbf", [128, 256], BF16) for _ in range(2)]
                        SSQ = [kb.sb(st, "ssq", [128, 2], F32) for _ in range(2)]
                        junk = kb.sb(st, "junk", [128, 128], F32)
                        nchunk = 4 if not is_s else 1
                        def ck1(cc):
                            kbf, ssq = KBF[cc % 2], SSQ[cc % 2]
                            cn = 128 if not is_s else NS
                            ci = ti * 4 + cc
                            cs = cc * 128
                            pa = PS[cc % 2]
                            pb2 = PS[2 + cc % 2]
                            kb.mm(pa, pa.t[0:cn, :], [(hT.t[:, kc, cs:cs + cn], Win.t[:, kc, 1024:1536]) for kc in range(8)], [Win, hT])
                            kb.mm(pb2, pb2.t[0:cn, 0:68], [(hT.t[:, kc, cs:cs + cn], Win.t[:, kc, 1792:1860]) for kc in range(8)], [Win, hT])
                            kob, vob, iob = ko[cc % 2], vo[cc % 2], io[cc % 2]
                            for g in range(2):
                                kb.op(ACT, lambda g=g: S.activation(out=junk.t[0:cn, :], in_=pa.t[0:cn, g * 128:(g + 1) * 128],
                                                                     func=AF.Square, accum_out=ssq.t[0:cn, g:g + 1]),
                                      reads=[pa], writes=[junk, ssq])
                            kb.op(ACT, lambda: S.activation(out=ssq.t[0:cn, :], in_=ssq.t[0:cn, :], func=AF.Sqrt,
                                                            bias=EPSB.t[0:cn, 0:1], scale=1.0 / 128), reads=[ssq, EPSB], writes=[ssq])
                            kb.op(DVE, lambda: V.reciprocal(out=ssq.t[0:cn, :], in_=ssq.t[0:cn, :]), reads=[ssq], writes=[ssq])
                            for g in range(2):
                                kb.op(DVE, lambda g=g: V.scalar_tensor_tensor(
                                    out=kob.t[0:cn, g * 128:(g + 1) * 128], in0=pa.t[0:cn, g * 128:(g + 1) * 128],
                                    scalar=ssq.t[0:cn, g:g + 1], in1=kgbc.t[0:cn, :], op0=ALU.mult, op1=ALU.mult),
                                    reads=[pa, ssq, kgbc], writes=[kob])
                            kb.op(ACT, lambda: S.copy(out=vob.t[0:cn, :], in_=pa.t[0:cn, 256:512]), reads=[pa], writes=[vob])
                            kb.op(ACT, lambda: S.copy(out=iob.t[0:cn, :], in_=pb2.t[0:cn, 0:64]), reads=[pb2], writes=[iob])
                            kb.op(ACT, lambda: S.activation(out=WIa.t[0:cn, ci, :], in_=pb2.t[0:cn, 64:68], func=AF.Abs),
                                  reads=[pb2], writes=[WIa])
                            kb.op(DVE, lambda: V.tensor_scalar(out=WIs.t[0:cn, ci, :], in0=pb2.t[0:cn, 64:68], scalar1=0.0,
                                                               scalar2=2.0, op0=ALU.is_ge, op1=ALU.mult), reads=[pb2], writes=[WIs])
                            kb.op(DVE, lambda: V.tensor_scalar(out=WIs.t[0:cn, ci, :], in0=WIs.t[0:cn, ci, :], scalar1=-1.0,
                                                               scalar2=None, op0=ALU.add), reads=[WIs], writes=[WIs])
                            kb.op(DVE, lambda: V.tensor_copy(out=kbf.t[0:cn, :], in_=kob.t[0:cn, :]), reads=[kob], writes=[kbf])
                            kb.op(ACT, lambda: S.copy(out=Vall.t[0:cn, ci, :], in_=vob.t[0:cn, :]), reads=[vob], writes=[Vall])
                            if not is_s:
                                r0 = ci * 128
                                kb.dma(SP, k_p[r0:r0 + 128, :], kob.t[:], reads=[kob])
                                kb.dma(SP, v_p[r0:r0 + 128, :], vob.t[:], reads=[vob])
                                kb.dma(SP, i_p[r0:r0 + 128, :], iob.t[:], reads=[iob])
                            else:
                                kb.dma(SP, k_s[:, :], kob.t[0:NS, :], reads=[kob])
                                kb.dma(SP, v_s[:, :], vob.t[0:NS, :], reads=[vob])
                                kb.dma(SP, i_s[:, :], iob.t[0:NS, :], reads=[iob])

                        def ck2(cc):
                            kbf = KBF[cc % 2]
                            cn = 128 if not is_s else NS
                            cs = cc * 128
                            pt = PS[4 + cc % 2]
                            ptb = pt.t[:].bitcast(BF16)
                            kb.transposes(pt, [(ptb[:, g * 128:g * 128 + cn], kbf.t[0:cn, g * 128:(g + 1) * 128]) for g in range(2)],
                                          [kbf], identb)
                            copy(evac_engine(), kT.t[:, :, t0 + cs:t0 + cs + cn],
                                 ptb[:, 0:256].rearrange("p (g t) -> p g t", g=2)[:, :, 0:cn], [pt], [kT])

                        ck1(0)
                        for cc in range(nchunk):
                            if cc + 1 < nchunk:
                                ck1(cc + 1)
                            ck2(cc)

                    HS.close()
                    if level < 2:
                        continue
                    if not is_s:
                        prompt_attention(nc, kb, PS, C, CST, ti, t0, qT, qiT, kT, Vall, kiT, WIa, WIs, biasN, identb, onesb, onT)
                    elif level < 6:
                        kb.op(DVE, lambda: V.memset(onT.t[:], 0.0), writes=[onT])
                    else:
                        sample_attention(nc, kb, PS, C, CST, qT, qiT, kT, Vall, kiT, WB, identb, onesb, onT,
                                         cache_k, cache_v, cache_i, ptab, relb, Win, biasS, biasNn)
                    for oc in range(8):
                        pb = PS[6 + oc % 2]
                        kb.mm(pb, pb.t[:, 0:n], [(Wo.t[:, kc, oc * 128:(oc + 1) * 128], onT.t[:, kc, 0:n]) for kc in range(8)], [Wo, onT])
                        add_resid(oc, t0, n, pb)

    if level >= 3:
        mlp(nc, kb, PS, xT, gn, EPSB, ones_d, 0, w_mlp_in, w_mlp_out, rmsnorm_tile)
    if level >= 4:
        retention(nc, kb, PS, (cstr, COR, cstr_np.shape[1]), None, xT, gn, identb, w_ret_in, w_ret_out, rot, st_in, r_p, r_s, rmsnorm_tile, level, EPSB)
    if level >= 5:
        mlp(nc, kb, PS, xT, gn, EPSB, ones_d, 1, w_mlp_in, w_mlp_out, rmsnorm_tile)

    with ExitStack() as st:
        ys = [kb.sb(st, "ys%d" % i, [128, D], F32) for i in range(2)]
        for c in range(17):
            n = 128 if c < 16 else NS
            yb = ys[c % 2]
            for g in range(2):
                pb = PS[(2 * c + g) % 8]
                eng = kb.PE
                kb._deps(eng, [xT, CST], [pb])
                inst = None
                for j in range(4):
                    inst = nc.tensor.transpose(pb.t[0:n, j * 128:(j + 1) * 128], xT.t[:, 4 * g + j, c * 128:c * 128 + n], identf.t[:, :])
                eng.cnt += 1
                inst.then_inc(eng.sem, 1)
                kb._commit((eng.key, eng.sem, eng.cnt), [xT, CST], [pb])
                copy(evac_engine(), yb.t[0:n, g * 512:(g + 1) * 512], pb.t[0:n, :], [pb], [yb])
            dst = y_p[c * 128:(c + 1) * 128, :] if c < 16 else y_s[:, :]
            kb.dma(SP, dst, yb.t[0:n, :], reads=[yb])
    kb.finish()
    kb.es.close()
    return nc, (cst_np, cstr_np)


def prompt_attention(nc, kb, PS, C, CST, ti, t0, qT, qiT, kT, Vall, kiT, WIa, WIs, biasN, identb, onesb, onT):
    V = nc.vector
    S = nc.scalar
    PE, ACT, DVE = kb.PE, kb.ACT, kb.DVE
    with ExitStack() as st:
        ACC = [kb.sb(st, "acc%d" % i, [128, 2048], F32) for i in range(2)]
        rt = [kb.sb(st, "rt%d" % i, [128, 512], F32) for i in range(2)]
        MASKB = [kb.sb(st, "maskb%d" % i, [128, 2048], BF16) for i in range(2)]
        MASKT = [kb.sb(st, "maskT%d" % i, [128, 16, 128], BF16) for i in range(2)]
        nearb = [kb.sb(st, "nearb%d" % i, [128, 4, 128], BF16) for i in range(2)]
        pT = [kb.sb(st, "pT%d" % i, [128, 512], BF16) for i in range(2)]
        rden = rt[0]
        LO = kb.sb(st, "b_lo", [128, 2], F32)
        MX = kb.sb(st, "b_mx", [128, 2], F32)
        MID = kb.sb(st, "b_mid", [128, 2], F32)
        CNT = kb.sb(st, "b_cnt", [128, 2], F32)
        GM = kb.sb(st, "b_gm", [128, 2], F32)
        TAU = kb.sb(st, "b_tau", [128, 2], F32)
        WH = kb.sb(st, "b_wh", [128, 2, NBIS], F32)
        HALF = kb.sb(st, "b_half", [128, 2], F32)
        WH2 = kb.sb(st, "b_wh2", [128, NBIS], F32)
        THR = kb.sb(st, "b_thr", [128, 1], F32)
        SA = kb.sb(st, "b_sa", [128, 1], F32)
        GA = kb.sb(st, "b_ga", [128, 1], F32)
        MIDA = [kb.sb(st, "b_mida%d" % i, [128, 1], F32) for i in range(2)]
        ONE = kb.sb(st, "b_one", [128, 2], F32)
        kb.op(DVE, lambda: V.memset(HALF.t[:], 0.5), writes=[HALF])
        kb.op(DVE, lambda: V.memset(ONE.t[:], 1.0), writes=[ONE])
        for pair in range(2):
            blocks = [ti * 4 + 2 * pair, ti * 4 + 2 * pair + 1]
            for k, b in enumerate(blocks):
                acc = ACC[k]
                q0 = (b % 4) * 128
                Sb = 128 * (b + 1)
                nch = (Sb + 511) // 512
                for sc in range(nch):
                    s0 = sc * 512
                    N = min(512, Sb - s0)
                    for h in range(4):
                        par, pr = h % 2, h // 2
                        pa = PS[(sc * 4 + h) % 2]
                        kb.mm(pa, pa.t[:, 0:N], [(qiT.t[par * 64:(par + 1) * 64, pr, q0:q0 + 128],
                                                  kiT.t[par * 64:(par + 1) * 64, s0:s0 + N])], [qiT, kiT])
                        r = rt[h % 2]
                        kb.op(ACT, lambda: S.activation(out=r.t[:, 0:N], in_=pa.t[:, 0:N], func=AF.Relu,
                                                        scale=WIa.t[:, b, h:h + 1]), reads=[pa, WIa], writes=[r])
                        if h == 0:
                            kb.op(DVE, lambda: V.tensor_scalar(out=acc.t[:, s0:s0 + N], in0=r.t[:, 0:N], scalar1=WIs.t[:, b, 0:1],
                                                               scalar2=None, op0=ALU.mult), reads=[r, WIs], writes=[acc])
                        else:
                            kb.op(DVE, lambda h=h: V.scalar_tensor_tensor(out=acc.t[:, s0:s0 + N], in0=r.t[:, 0:N],
                                                                         scalar=WIs.t[:, b, h:h + 1], in1=acc.t[:, s0:s0 + N],
                                                                         op0=ALU.mult, op1=ALU.add), reads=[r, WIs, acc], writes=[acc])
                kb.op(DVE, lambda: V.tensor_tensor(out=acc.t[:, Sb - 128:Sb], in0=acc.t[:, Sb - 128:Sb], in1=C("causneg"), op=ALU.add),
                      reads=[acc, CST], writes=[acc])
            if blocks[0] >= 2:
                for k, b in enumerate(blocks):
                    Sb = 128 * (b + 1)
                    kb.op(DVE, lambda: V.tensor_reduce(out=LO.t[:, k:k + 1], in_=ACC[k].t[:, 0:Sb - 128], axis=AX.X, op=ALU.min),
                          reads=[ACC[k]], writes=[LO])
                    kb.op(DVE, lambda: V.tensor_reduce(out=MX.t[:, k:k + 1], in_=ACC[k].t[:, 0:Sb], axis=AX.X, op=ALU.max),
                          reads=[ACC[k]], writes=[MX])
                kb.op(DVE, lambda: V.scalar_tensor_tensor(out=MX.t[:], in0=MX.t[:], scalar=1.0, in1=LO.t[:], op0=ALU.add, op1=ALU.subtract),
                      reads=[MX, LO], writes=[MX])
                for k in range(2):
                    kb.op(DVE, lambda: V.tensor_scalar(out=WH.t[:, k, :], in0=C("pow2"), scalar1=MX.t[:, k:k + 1], scalar2=None, op0=ALU.mult),
                          reads=[CST, MX], writes=[WH])
                kb.op(DVE, lambda: V.tensor_tensor(out=MID.t[:], in0=LO.t[:], in1=WH.t[:, :, 0], op=ALU.add), reads=[LO, WH], writes=[MID])
                Sb0 = 128 * (blocks[0] + 1)
                Sb1 = 128 * (blocks[1] + 1)
                kb.op(DVE, lambda: V.tensor_scalar(out=WH2.t[:], in0=WH.t[:, 1, :], scalar1=0.5, scalar2=None, op0=ALU.mult), reads=[WH], writes=[WH2])
                kb.op(DVE, lambda: V.memset(THR.t[:], float(Sb1) - 510.5), writes=[THR])
                kb.op(DVE, lambda: V.tensor_copy(out=MIDA[0].t[:], in_=MID.t[:, 1:2]), reads=[MID], writes=[MIDA[0]])
                for it in range(NBIS):
                    last = (it == NBIS - 1)
                    kb.op(DVE, lambda: V.tensor_scalar(out=MASKB[0].t[:, 0:Sb0], in0=ACC[0].t[:, 0:Sb0], scalar1=MID.t[:, 0:1], scalar2=0.0,
                                                       op0=ALU.is_ge, op1=ALU.add, accum_out=CNT.t[:, 0:1]),
                          reads=[ACC[0], MID], writes=[MASKB[0], CNT])
                    kb.op(DVE, lambda: V.scalar_tensor_tensor(out=GM.t[:, 0:1], in0=CNT.t[:, 0:1], scalar=255.5, in1=(ONE if last else HALF).t[:, 0:1],
                                                              op0=ALU.is_ge, op1=ALU.subtract), reads=[CNT, ONE, HALF], writes=[GM])
                    dst = TAU if last else MID
                    kb.op(DVE, lambda: V.scalar_tensor_tensor(out=dst.t[:, 0:1], in0=GM.t[:, 0:1], scalar=WH.t[:, 0, it:it + 1],
                                                              in1=MID.t[:, 0:1], op0=ALU.mult, op1=ALU.add),
                          reads=[GM, WH, MID], writes=[dst])
                    ma, mb_ = MIDA[it % 2], MIDA[(it + 1) % 2]
                    kb.op(ACT, lambda: S.activation(out=MASKB[1].t[:, 0:Sb1], in_=ACC[1].t[:, 0:Sb1], func=AF.Sign, scale=-1.0,
                                                    bias=ma.t[:, 0:1], accum_out=SA.t[:, 0:1]), reads=[ACC[1], ma], writes=[MASKB[1], SA])
                    kb.op(ACT, lambda: S.activation(out=GA.t[:, 0:1], in_=SA.t[:, 0:1], func=AF.Sign, scale=-1.0, bias=THR.t[:, 0:1]),
                          reads=[SA, THR], writes=[GA])
                    kb.op(ACT, lambda: S.activation(out=mb_.t[:, 0:1], in_=GA.t[:, 0:1], func=AF.Identity, scale=WH2.t[:, it:it + 1],
                                                    bias=ma.t[:, 0:1]), reads=[GA, WH2, ma], writes=[mb_])
                kb.op(DVE, lambda: V.tensor_tensor(out=TAU.t[:, 1:2], in0=MIDA[NBIS % 2].t[:, 0:1], in1=WH2.t[:, NBIS - 1:NBIS], op=ALU.subtract),
                      reads=[MIDA[NBIS % 2], WH2], writes=[TAU])
            else:
                kb.op(DVE, lambda: V.memset(TAU.t[:], -1.0e29), writes=[TAU])
            for k, b in enumerate(blocks):
                Sb = 128 * (b + 1)
                maskb, maskT = MASKB[k], MASKT[k]
                kb.op(DVE, lambda: V.tensor_scalar(out=maskb.t[:, 0:Sb], in0=ACC[k].t[:, 0:Sb], scalar1=TAU.t[:, k:k + 1], scalar2=NEG,
                                                   op0=ALU.is_lt, op1=ALU.mult), reads=[ACC[k], TAU], writes=[maskb])
                for j0 in range(0, b + 1, 8):
                    j1 = min(b + 1, j0 + 8)
                    pt = PS[2 + (j0 // 8) % 2]
                    ptb = pt.t[:].bitcast(BF16)
                    kb.transposes(pt, [(ptb[:, (j - j0) * 128:(j - j0 + 1) * 128], maskb.t[:, j * 128:(j + 1) * 128]) for j in range(j0, j1)],
                                  [maskb], identb)
                    src = ptb[:, 0:(j1 - j0) * 128].rearrange("p (j t) -> p j t", t=128)
                    if (j0 // 8) % 2 == 0:
                        kb.op(ACT, lambda: S.copy(out=maskT.t[:, j0:j1, :], in_=src), reads=[pt], writes=[maskT])
                    else:
                        kb.op(DVE, lambda: V.tensor_copy(out=maskT.t[:, j0:j1, :], in_=src), reads=[pt], writes=[maskT])
            for k, b in enumerate(blocks):
                maskT = MASKT[k]
                q0 = (b % 4) * 128
                for g in range(2):
                    po = PS[4 + g]
                    pd = PS[6 + g]
                    def logits(j):
                        pl = PS[j % 2]
                        near = (j >= b - 1)
                        if near:
                            nb_ = nearb[j % 2]
                            kb.op(DVE, lambda: V.tensor_tensor(out=nb_.t[:], in0=biasN.t[:, b - j, 4 * g:4 * g + 4, :],
                                                               in1=maskT.t[:, j, :].unsqueeze(1).to_broadcast([128, 4, 128]), op=ALU.add),
                                  reads=[biasN, maskT], writes=[nb_])
                            rhs2 = nb_.t[:]
                            rd2 = [nb_]
                        else:
                            rhs2 = maskT.t[:, j, :].unsqueeze(1).to_broadcast([128, 4, 128])
                            rd2 = [maskT]
                        kb.mm(pl, pl.t[:].rearrange("p (r t) -> p r t", r=4),
                              [(kT.t[:, g, j * 128:(j + 1) * 128], qT.t[:, 4 * g:4 * g + 4, q0:q0 + 128]), (identb.t[:], rhs2)],
                              [kT, qT, identb] + rd2)

                    logits(0)
                    for j in range(b + 1):
                        if j + 1 <= b:
                            logits(j + 1)
                        pl = PS[j % 2]
                        pp = pT[j % 2]
                        kb.op(ACT, lambda: S.activation(out=pp.t[:], in_=pl.t[:], func=AF.Exp), reads=[pl], writes=[pp])
                        kb.mm(po, po.t[:], [(Vall.t[:, j, g * 128:(g + 1) * 128], pp.t[:])], [Vall, pp], start=(j == 0), stop=(j == b))
                        kb.mm(pd, pd.t[:], [(onesb.t[:], pp.t[:])], [onesb, pp], start=(j == 0), stop=(j == b))
                    kb.op(DVE, lambda: V.reciprocal(out=rden.t[:], in_=pd.t[:]), reads=[pd], writes=[rden])
                    kb.op(DVE, lambda: V.tensor_tensor(out=onT.t[:, 4 * g:4 * g + 4, q0:q0 + 128],
                                                       in0=po.t[:].rearrange("p (r t) -> p r t", r=4),
                                                       in1=rden.t[:].rearrange("p (r t) -> p r t", r=4), op=ALU.mult),
                          reads=[po, rden], writes=[onT])


def mm_multi(kb, nc, out_buf, items, reads):
    eng = kb.PE
    kb._deps(eng, reads, [out_buf])
    inst = None
    for (o, l, r) in items:
        inst = nc.tensor.matmul(o, lhsT=l, rhs=r, start=True, stop=True)
    eng.cnt += 1
    inst.then_inc(eng.sem, 1)
    kb._commit((eng.key, eng.sem, eng.cnt), reads, [out_buf])


def sample_attention(nc, kb, PS, C, CST, qT, qiT, kT, Vall, kiT, WB, identb, onesb, onT, cache_k, cache_v, cache_i, ptab, relb, Win, biasS, biasNn):
    V = nc.vector
    S = nc.scalar
    ACT, DVE, POOL, SP = kb.ACT, kb.DVE, kb.POOL, kb.SP
    IOA = bass.IndirectOffsetOnAxis
    with ExitStack() as st:
        IDX = kb.sb(st, "IDX", [128, 32], I32)
        ISa = kb.sb(st, "ISa", [128, 16, 16, 4], F32)
        ISn = kb.sb(st, "ISn", [128, 16, 4], F32)
        LO = kb.sb(st, "LO", [128, 64], F32)
        with ExitStack() as s2:
            PT = kb.sb(s2, "PT", [128, 256], I32)
            PTf = kb.sb(s2, "PTf", [128, 16, 16], F32)
            PG = kb.sb(s2, "PG", [128, 32], F32)
            kb.dma(SP, PT.t[:], ptab.partition_broadcast(128), writes=[PT])
            kb.op(DVE, lambda: V.tensor_copy(out=PTf.t[:], in_=PT.t[:].rearrange("p (b j) -> p b j", j=16)), reads=[PT], writes=[PTf])
            kb.op(DVE, lambda: V.tensor_tensor(out=PTf.t[:], in0=PTf.t[:], in1=C("pagesel").unsqueeze(1).to_broadcast([128, 16, 16]),
                                               op=ALU.mult), reads=[PTf, CST], writes=[PTf])
            kb.op(DVE, lambda: V.tensor_reduce(out=PG.t[:, 0:16], in_=PTf.t[:], axis=AX.X, op=ALU.add), reads=[PTf], writes=[PG])
            kb.op(DVE, lambda: V.tensor_scalar(out=PG.t[:, 0:16], in0=PG.t[:, 0:16], scalar1=128.0, scalar2=C("pofs")[:, 0:1],
                                               op0=ALU.mult, op1=ALU.add), reads=[PG, CST], writes=[PG])
            kb.op(DVE, lambda: V.tensor_scalar(out=PG.t[:, 16:32], in0=PG.t[:, 0:16], scalar1=8.0, scalar2=None, op0=ALU.add),
                  reads=[PG], writes=[PG])
            kb.op(DVE, lambda: V.tensor_copy(out=IDX.t[:], in_=PG.t[:]), reads=[PG], writes=[IDX])

        with ExitStack() as s2:
            IG = [kb.sb(s2, "IG", [128, 16, 64], F32) for _ in range(2)]
            IB2 = kb.sb(s2, "IB2", [128, 16, 128], BF16)
            ITK = kb.sb(s2, "ITK", [128, 16, 128], BF16)
            tmp = kb.sb(s2, "tmpI", [128, 16, 4, 4], F32)
            for b in range(NSEQ):
                ig = IG[b % 2]
                kb.dma(POOL, ig.t[:].rearrange("p c d -> p (c d)"), cache_i, reads=[IDX], writes=[ig], indirect=IOA(ap=IDX.t[:, b:b + 1], axis=0))
                kb.op(ACT, lambda: S.copy(out=IB2.t[:, :, 0:64], in_=ig.t[:]), reads=[ig], writes=[IB2])
                kb.op(DVE, lambda: V.tensor_copy(out=IB2.t[:, :, 64:128], in_=ig.t[:]), reads=[ig], writes=[IB2])
                for half in range(2):
                    pt = PS[half]
                    ptb = pt.t[:].bitcast(BF16)
                    kb.transposes(pt, [(ptb[:, j * 128:(j + 1) * 128], IB2.t[:, half * 8 + j, :]) for j in range(8)], [IB2], identb)
                    copy_any(kb, nc, half, ITK.t[:, half * 8:half * 8 + 8, :], ptb[:, :].rearrange("p (j t) -> p j t", t=128), [pt], [ITK])
                pIs = [PS[2], PS[7]]
                wbb = WB.t[:, b, :]
                for par in range(2):
                    pI = pIs[par]
                    items = []
                    for c in range(16):
                        items.append((pI.t[:, c * 8:c * 8 + 8], ITK.t[par * 64:(par + 1) * 64, c, :],
                                      qiT.t[par * 64:(par + 1) * 64, :, 4 * b:4 * b + 4]))
                    items.append((pI.t[0:NS, 128:136], kiT.t[par * 64:(par + 1) * 64, SEQ:SEQ + NS],
                                  qiT.t[par * 64:(par + 1) * 64, :, 4 * b:4 * b + 4]))
                    mm_multi(kb, nc, pI, items, [ITK, qiT, kiT])
                for par in range(2):
                    pI = pIs[par]
                    kb.op(DVE, lambda: V.scalar_tensor_tensor(out=tmp.t[:, :, 2 * par:2 * par + 2, :].rearrange("p c h t -> p c (h t)"),
                                                              in0=pI.t[:, 0:128].rearrange("p (c x) -> p c x", c=16), scalar=0.0,
                                                              in1=wbb[:, 8 * par:8 * par + 8].unsqueeze(1).to_broadcast([128, 16, 8]),
                                                              op0=ALU.max, op1=ALU.mult), reads=[pI, WB], writes=[tmp])
                kb.op(DVE, lambda: V.tensor_reduce(out=ISa.t[:, b, :, :], in_=tmp.t[:].rearrange("p c h t -> p c t h"), axis=AX.X, op=ALU.add),
                      reads=[tmp], writes=[ISa])
                if kb.DBG and b == 14:
                    kb.dma(SP, kb.DBG["G"], ig.t[:].rearrange("p c d -> p (c d)"), reads=[ig])
                    kb.dma(SP, kb.DBG["B"], IB2.t[:].rearrange("p c d -> p (c d)"), reads=[IB2])
                    kb.dma(SP, kb.DBG["T"], ITK.t[:].rearrange("p c d -> p (c d)"), reads=[ITK])
                    kb.dma(SP, kb.DBG["M"], tmp.t[:].rearrange("p c h t -> p (c h t)"), reads=[tmp])
                for par in range(2):
                    pI = pIs[par]
                    kb.op(DVE, lambda: V.scalar_tensor_tensor(out=tmp.t[0:NS, 0, 2 * par:2 * par + 2, :].rearrange("p h t -> p (h t)"),
                                                              in0=pI.t[0:NS, 128:136], scalar=0.0, in1=wbb[0:NS, 8 * par:8 * par + 8],
                                                              op0=ALU.max, op1=ALU.mult), reads=[pI, WB], writes=[tmp])
                kb.op(DVE, lambda: V.tensor_reduce(out=ISn.t[0:NS, b, :], in_=tmp.t[0:NS, 0, :, :].rearrange("p h t -> p t h"), axis=AX.X, op=ALU.add),
                      reads=[tmp], writes=[ISn])
            kb.op(DVE, lambda: V.tensor_tensor(out=ISn.t[0:NS, :, :], in0=ISn.t[0:NS, :, :],
                                               in1=C("newvalid")[0:NS, :].rearrange("p (b t) -> p b t", t=4), op=ALU.add),
                  reads=[ISn, CST], writes=[ISn])

        with ExitStack() as s2:
            Wd = kb.sb(s2, "Wd", [128, 64], F32)
            WH = kb.sb(s2, "WH", [128, 64], F32)
            MID = kb.sb(s2, "MID", [128, 64], F32)
            GE = kb.sb(s2, "GE", [128, 64], F32)
            CMP = kb.sb(s2, "CMP", [128, 16, 16, 4], BF16)
            CNP = kb.sb(s2, "CNP", [128, 64], F32)
            CMN = kb.sb(s2, "CMN", [128, 64], F32)
            MX = kb.sb(s2, "MX", [128, 128], F32)
            DG = kb.sb(s2, "DG", [128, 128], F32)
            onesf = kb.sb(s2, "onesf", [128, 128], F32)
            kb.op(DVE, lambda: V.memset(onesf.t[:], 1.0), writes=[onesf])
            kb.op(DVE, lambda: V.tensor_reduce(out=MX.t[:, 0:64].rearrange("p (b t) -> p b t", t=4), in_=ISa.t[:].rearrange("p b c t -> p b t c"),
                                               axis=AX.X, op=ALU.max), reads=[ISa], writes=[MX])
            kb.op(DVE, lambda: V.tensor_reduce(out=MX.t[:, 64:128].rearrange("p (b t) -> p b t", t=4), in_=ISa.t[:].rearrange("p b c t -> p b t c"),
                                               axis=AX.X, op=ALU.min), reads=[ISa], writes=[MX])
            kb.op(DVE, lambda: V.tensor_tensor(out=MX.t[0:NS, 0:64], in0=MX.t[0:NS, 0:64], in1=ISn.t[0:NS, :, :].rearrange("p b t -> p (b t)"),
                                               op=ALU.max), reads=[MX, ISn], writes=[MX])
            pm = PS[3]
            eng = kb.PE
            kb._deps(eng, [MX, CST], [pm])
            ins_ = nc.tensor.transpose(pm.t[:, 0:128], MX.t[:, :], CST.t[:, 0:128])
            eng.cnt += 1
            ins_.then_inc(eng.sem, 1)
            kb._commit((eng.key, eng.sem, eng.cnt), [MX, CST], [pm])
            kb.op(DVE, lambda: V.tensor_reduce(out=GE.t[0:64, 0:1], in_=pm.t[0:64, 0:128], axis=AX.X, op=ALU.max), reads=[pm], writes=[GE])
            kb.op(DVE, lambda: V.tensor_reduce(out=GE.t[64:128, 0:1], in_=pm.t[64:128, 0:128], axis=AX.X, op=ALU.min), reads=[pm], writes=[GE])
            kb.op(DVE, lambda: V.tensor_scalar(out=DG.t[:], in0=CST.t[:, 0:128], scalar1=GE.t[:, 0:1], scalar2=None, op0=ALU.mult),
                  reads=[CST, GE], writes=[DG])
            pq = PS[4]
            kb.mm(pq, pq.t[:, 0:128], [(onesf.t[:], DG.t[:])], [onesf, DG])
            kb.op(DVE, lambda: V.tensor_copy(out=LO.t[:], in_=pq.t[:, 64:128]), reads=[pq], writes=[LO])
            kb.op(DVE, lambda: V.scalar_tensor_tensor(out=Wd.t[:], in0=pq.t[:, 0:64], scalar=1.0, in1=LO.t[:], op0=ALU.add, op1=ALU.subtract),
                  reads=[pq, LO], writes=[Wd])
            pc = PS[5]
            for it in range(NBIS):
                kb.op(DVE, lambda: V.tensor_scalar(out=WH.t[:], in0=Wd.t[:], scalar1=0.5, scalar2=None, op0=ALU.mult), reads=[Wd], writes=[WH])
                kb.op(DVE, lambda: V.tensor_tensor(out=MID.t[:], in0=LO.t[:], in1=WH.t[:], op=ALU.add), reads=[LO, WH], writes=[MID])
                kb.op(DVE, lambda: V.tensor_tensor(out=CMP.t[:], in0=ISa.t[:],
                                                   in1=MID.t[:].rearrange("p (b t) -> p b t", t=4).unsqueeze(2).to_broadcast([128, 16, 16, 4]),
                                                   op=ALU.is_ge), reads=[ISa, MID], writes=[CMP])
                kb.op(DVE, lambda: V.tensor_reduce(out=CNP.t[:].rearrange("p (b t) -> p b t", t=4), in_=CMP.t[:].rearrange("p b c t -> p b t c"),
                                                   axis=AX.X, op=ALU.add), reads=[CMP], writes=[CNP])
                kb.op(DVE, lambda: V.tensor_tensor(out=CMN.t[0:NS, :], in0=ISn.t[0:NS, :, :].rearrange("p b t -> p (b t)"), in1=MID.t[0:NS, :],
                                                   op=ALU.is_ge), reads=[ISn, MID], writes=[CMN])
                kb.mm(pc, pc.t[:, 0:64], [(onesf.t[:], CNP.t[:]), (onesf.t[0:NS, :], CMN.t[0:NS, :])], [onesf, CNP, CMN])
                kb.op(DVE, lambda: V.tensor_scalar(out=GE.t[:, 0:64], in0=pc.t[:, 0:64], scalar1=255.5, scalar2=None, op0=ALU.is_ge), reads=[pc], writes=[GE])
                kb.op(DVE, lambda: V.tensor_tensor(out=GE.t[:, 0:64], in0=GE.t[:, 0:64], in1=WH.t[:], op=ALU.mult), reads=[GE, WH], writes=[GE])
                kb.op(DVE, lambda: V.tensor_tensor(out=LO.t[:], in0=LO.t[:], in1=GE.t[:, 0:64], op=ALU.add), reads=[LO, GE], writes=[LO])
                kb.op(DVE, lambda: V.tensor_copy(out=Wd.t[:], in_=WH.t[:]), reads=[WH], writes=[Wd])

        with ExitStack() as s2:
            MB = kb.sb(s2, "MB", [128, 16, 16, 4], F32)
            MBN = kb.sb(s2, "MBN", [128, 16, 4], F32)
            kb.op(DVE, lambda: V.tensor_tensor(out=MB.t[:], in0=ISa.t[:],
                                               in1=LO.t[:].rearrange("p (b t) -> p b t", t=4).unsqueeze(2).to_broadcast([128, 16, 16, 4]),
                                               op=ALU.is_lt), reads=[ISa, LO], writes=[MB])
            kb.op(DVE, lambda: V.tensor_scalar(out=MB.t[:], in0=MB.t[:], scalar1=NEG, scalar2=None, op0=ALU.mult), reads=[MB], writes=[MB])
            kb.op(DVE, lambda: V.tensor_tensor(out=MBN.t[0:NS], in0=ISn.t[0:NS], in1=LO.t[0:NS, :].rearrange("p (b t) -> p b t", t=4), op=ALU.is_lt),
                  reads=[ISn, LO], writes=[MBN])
            kb.op(DVE, lambda: V.tensor_scalar(out=MBN.t[0:NS], in0=MBN.t[0:NS], scalar1=NEG, scalar2=None, op0=ALU.mult), reads=[MBN], writes=[MBN])
            KGS = [kb.sb(s2, "KG", [128, 8, 256], F32) for _ in range(2)]
            kgi = [0]
            wfl = Win.t[:].rearrange("p a b -> p (a b)")
            wf = {}
            if Win.lw:
                wf[Win.lw[0]] = (Win.lw[1], Win.lw[2])
            for k_, v_ in Win.rd.items():
                if wf.get(k_, (None, 0))[1] < v_[1]:
                    wf[k_] = v_
            KB_ = Buf(wfl[:, 0:4096].rearrange("p (c d) -> p c d", d=256), "KBb", wf)
            VB_ = Buf(wfl[:, 4096:8192].rearrange("p (c d) -> p c d", d=256), "VBb", wf)
            KT = Buf(wfl[:, 8192:12288].rearrange("p (c g t) -> p c g t", g=2, t=128), "KTs", wf)
            LG = kb.sb(s2, "LG", [128, 16, 8, 4], F32)
            MBI = kb.sb(s2, "MBI", [128, 16, 8, 4], F32)
            LGN = kb.sb(s2, "LGN", [128, 8, 4], F32)
            PT_ = kb.sb(s2, "PTs", [128, 16, 8, 4], BF16)
            PTN = kb.sb(s2, "PTN", [128, 8, 4], BF16)
            DEN = kb.sb(s2, "DEN", [128, 8, 4], F32)
            for b in range(NSEQ):
                for (src, dstb, e) in ((cache_k, KB_, 0), (cache_v, VB_, 1)):
                    for half in range(2):
                        KG = KGS[kgi[0] % 2]
                        kgi[0] += 1
                        kb.dma(POOL, KG.t[:].rearrange("p c d -> p (c d)"), src, reads=[IDX], writes=[KG], indirect=IOA(ap=IDX.t[:, half * 16 + b:half * 16 + b + 1], axis=0))
                        copy_any(kb, nc, (half + e) % 2, dstb.t[:, half * 8:half * 8 + 8, :], KG.t[:], [KG], [dstb])
                for q4 in range(4):
                    pt = PS[q4 % 2]
                    ptb = pt.t[:].bitcast(BF16)
                    kb.transposes(pt, [(ptb[:, (2 * j + g) * 128:(2 * j + g + 1) * 128], KB_.t[:, q4 * 4 + j, g * 128:(g + 1) * 128])
                                       for j in range(4) for g in range(2)], [KB_], identb)
                    copy_any(kb, nc, q4 % 2, KT.t[:, q4 * 4:q4 * 4 + 4, :, :], ptb[:, :].rearrange("p (j g t) -> p j g t", j=4, g=2), [pt], [KT])
                pL = PS[2]
                pLv = pL.t[:].rearrange("p (c h t) -> p c h t", c=16, h=8)
                items = []
                for c in range(16):
                    for g in range(2):
                        items.append((pL.t[:, (c * 8 + 4 * g) * 4:(c * 8 + 4 * g) * 4 + 16], KT.t[:, c, g, :], qT.t[:, 4 * g:4 * g + 4, 4 * b:4 * b + 4]))
                mm_multi(kb, nc, pL, items, [KT, qT])
                pN = PS[3]
                pNv = pN.t[0:NS, 0:32].rearrange("p (h t) -> p h t", h=8)
                mm_multi(kb, nc, pN, [(pN.t[0:NS, g * 16:(g + 1) * 16], kT.t[:, g, SEQ:SEQ + NS], qT.t[:, 4 * g:4 * g + 4, 4 * b:4 * b + 4]) for g in range(2)],
                         [kT, qT])
                kb.op(DVE, lambda: V.tensor_tensor(out=MBI.t[:], in0=biasS.t[:], in1=MB.t[:, b, :, :].unsqueeze(2).to_broadcast([128, 16, 8, 4]),
                                                   op=ALU.add), reads=[biasS, MB], writes=[MBI])
                kb.op(DVE, lambda: V.tensor_tensor(out=LG.t[:], in0=pLv, in1=MBI.t[:], op=ALU.add), reads=[pL, MBI], writes=[LG])
                kb.op(ACT, lambda: S.activation(out=PT_.t[:], in_=LG.t[:], func=AF.Exp), reads=[LG], writes=[PT_])
                kb.op(DVE, lambda: V.tensor_tensor(out=LGN.t[0:NS], in0=biasNn.t[0:NS, :, 4 * b:4 * b + 4],
                                                   in1=MBN.t[0:NS, b, :].unsqueeze(1).to_broadcast([NS, 8, 4]), op=ALU.add),
                      reads=[biasNn, MBN], writes=[LGN])
                kb.op(DVE, lambda: V.tensor_tensor(out=LGN.t[0:NS], in0=pNv, in1=LGN.t[0:NS], op=ALU.add), reads=[pN, LGN], writes=[LGN])
                kb.op(ACT, lambda: S.activation(out=PTN.t[0:NS], in_=LGN.t[0:NS], func=AF.Exp), reads=[LGN], writes=[PTN])
                pD = PS[4]
                kb.mm(pD, pD.t[:], [(onesb.t[:], PT_.t[:].rearrange("p c h t -> p (c h t)"))], [onesb, PT_])
                pDn = PS[5]
                kb.mm(pDn, pDn.t[:, 0:32], [(onesb.t[0:NS, :], PTN.t[0:NS].rearrange("p h t -> p (h t)"))], [onesb, PTN])
                kb.op(DVE, lambda: V.tensor_reduce(out=DEN.t[:], in_=pD.t[:].rearrange("p (c h t) -> p h t c", c=16, h=8), axis=AX.X, op=ALU.add),
                      reads=[pD], writes=[DEN])
                kb.op(DVE, lambda: V.tensor_tensor(out=DEN.t[:], in0=DEN.t[:], in1=pDn.t[:, 0:32].rearrange("p (h t) -> p h t", h=8), op=ALU.add),
                      reads=[DEN, pDn], writes=[DEN])
                kb.op(DVE, lambda: V.reciprocal(out=DEN.t[:], in_=DEN.t[:]), reads=[DEN], writes=[DEN])
                pO = PS[6]
                pOv = pO.t[:, 0:32].rearrange("p (h t) -> p h t", h=8)
                for g in range(2):
                    prs = [(VB_.t[:, c, g * 128:(g + 1) * 128], PT_.t[:, c, 4 * g:4 * g + 4, :]) for c in range(16)]
                    prs.append((Vall.t[0:NS, 16, g * 128:(g + 1) * 128], PTN.t[0:NS, 4 * g:4 * g + 4, :]))
                    kb.mm(pO, pO.t[:, g * 16:(g + 1) * 16], prs, [VB_, PT_, Vall, PTN])
                kb.op(DVE, lambda: V.tensor_tensor(out=onT.t[:, :, 4 * b:4 * b + 4], in0=pOv, in1=DEN.t[:], op=ALU.mult),
                      reads=[pO, DEN], writes=[onT])
            if kb.DBG:
                kb.dma(SP, kb.DBG["I"], IDX.t[:], reads=[IDX])
                kb.dma(SP, kb.DBG["A"], ISa.t[:].rearrange("p b c t -> p (b c t)"), reads=[ISa])
                kb.dma(SP, kb.DBG["L"], LO.t[:], reads=[LO])
                kb.dma(SP, kb.DBG["N"][0:NS, :], ISn.t[0:NS].rearrange("p b t -> p (b t)"), reads=[ISn])
                kb.dma(SP, kb.DBG["D"], DEN.t[:].rearrange("p h t -> p (h t)"), reads=[DEN])
                kb.dma(SP, kb.DBG["W"], WB.t[:].rearrange("p b x -> p (b x)"), reads=[WB])


def copy_any(kb, nc, which, out_ap, in_ap, reads, writes):
    if which == 0:
        kb.op(kb.ACT, lambda: nc.scalar.copy(out=out_ap, in_=in_ap), reads=reads, writes=writes)
    else:
        kb.op(kb.DVE, lambda: nc.vector.tensor_copy(out=out_ap, in_=in_ap), reads=reads, writes=writes)


def mlp(nc, kb, PS, xT, gn, EPSB, ones_d, l, w_in, w_out, rmsnorm_tile):
    V = nc.vector
    S = nc.scalar
    ACT, DVE, POOL = kb.ACT, kb.DVE, kb.POOL
    with ExitStack() as st:
        hT = kb.sb(st, "hTm", [128, 8, T], BF16)
        with ExitStack() as s2:
            sqb = kb.sb(s2, "sqbm", [128, 8, 512], BF16)
            rsb = kb.sb(s2, "rsbm", [128, 512], F32)
            for (t0, n) in TT:
                rmsnorm_tile(hT, hT.t[:, :, t0:t0 + n], t0, n, 8 + 16 * l, sqb, rsb)
        W1 = [kb.sb(st, "W1", [128, 8, 512], BF16) for _ in range(3)]
        W2 = [kb.sb(st, "W2", [128, 4, 1024], BF16) for _ in range(3)]
        U = [kb.sb(st, "U", [128, 4, T], BF16) for _ in range(2)]
        R = [kb.sb(st, "R", [128, 512], BF16) for _ in range(2)]
        w1src = w_in[l].rearrange("(kc p) n -> p kc n", p=128)
        cnt = [0, 0]

        def load(fg):
            kb.dma(POOL, W1[fg % 3].t[:], w1src[:, :, fg * 512:(fg + 1) * 512], writes=[W1[fg % 3]])
            kb.dma(POOL, W2[fg % 3].t[:], w_out[l][fg * 512:(fg + 1) * 512, :].rearrange("(c p) n -> p c n", p=128),
                   writes=[W2[fg % 3]])

        def phase1(fg):
            w1, ub = W1[fg % 3], U[fg % 2]
            for ffc in range(4):
                for (t0, n) in TT:
                    pb = PS[cnt[0] % 4]
                    rb = R[cnt[0] % 2]
                    cnt[0] += 1
                    kb.mm(pb, pb.t[:, 0:n], [(w1.t[:, kc, ffc * 128:(ffc + 1) * 128], hT.t[:, kc, t0:t0 + n]) for kc in range(8)],
                          [w1, hT])
                    kb.op(ACT, lambda: S.activation(out=rb.t[:, 0:n], in_=pb.t[:, 0:n], func=AF.Relu), reads=[pb], writes=[rb])
                    kb.op(DVE, lambda: V.tensor_tensor(out=ub.t[:, ffc, t0:t0 + n], in0=rb.t[:, 0:n], in1=rb.t[:, 0:n], op=ALU.mult),
                          reads=[rb], writes=[ub])

        def phase2(fg):
            w2, ub = W2[fg % 3], U[fg % 2]
            for oc in range(8):
                for (t0, n) in TT:
                    pb = PS[4 + cnt[1] % 4]
                    cnt[1] += 1
                    kb.mm(pb, pb.t[:, 0:n], [(w2.t[:, ffc, oc * 128:(oc + 1) * 128], ub.t[:, ffc, t0:t0 + n]) for ffc in range(4)],
                          [w2, ub])
                    kb.op(DVE, lambda: V.tensor_tensor(out=xT.t[:, oc, t0:t0 + n], in0=xT.t[:, oc, t0:t0 + n], in1=pb.t[:, 0:n],
                                                       op=ALU.add), reads=[xT, pb], writes=[xT])

        load(0)
        load(1)
        phase1(0)
        for fg in range(8):
            if fg + 2 < 8:
                load(fg + 2)
            if fg + 1 < 8:
                phase1(fg + 1)
            phase2(fg)


def retention(nc, kb, PS, C, CST, xT, gn, identb, w_in, w_out, rot, st_in, r_p, r_s, rmsnorm_tile, level, EPSB_):
    V = nc.vector
    S = nc.scalar
    ACT, DVE, POOL, SP = kb.ACT, kb.DVE, kb.POOL, kb.SP
    with ExitStack() as st:
        cstr_ap, COR, ncr = C
        CST = kb.sb(st, "cstr", [128, ncr], F32)
        kb.dma(SP, CST.t[:], cstr_ap, writes=[CST])

        def C(name):
            o, w = COR[name]
            return CST.t[:, o:o + w]
        hT = kb.sb(st, "hTr", [128, 8, T], BF16)
        with ExitStack() as s2:
            sqb = kb.sb(s2, "sqbr", [128, 8, 512], BF16)
            rsb = kb.sb(s2, "rsbr", [128, 512], F32)
            for (t0, n) in TT:
                rmsnorm_tile(hT, hT.t[:, :, t0:t0 + n], t0, n, 16, sqb, rsb)
        Sf = kb.sb(st, "Sf", [128, 2, 512], F32)
        Sb = kb.sb(st, "Sb", [128, 2, 512], BF16)
        Wqk = [kb.sb(st, "Wqk", [128, 8, 512], BF16) for _ in range(1)]
        Wv = kb.sb(st, "Wv", [128, 8, 512], BF16)
        Wg = kb.sb(st, "Wg", [128, 8, 512], BF16)
        Wo = kb.sb(st, "Wor", [128, 4, 1024], BF16)
        RT = [kb.sb(st, "rt", [128, 2, 512], F32) for _ in range(2)]
        QK = [kb.sb(st, "qk", [128, 4, 512], BF16) for _ in range(2)]
        OGT = [kb.sb(st, "ogT", [128, 4, 512], BF16) for _ in range(2)]
        t1 = kb.sb(st, "t1", [128, 512], F32)
        t2 = kb.sb(st, "t2", [128, 512], F32)
        VB = [kb.sb(st, "vb", [128, 512], BF16) for _ in range(2)]
        GT = [kb.sb(st, "gt", [128, 512], BF16) for _ in range(2)]
        ATM = [kb.sb(st, "atm", [128, 128], BF16) for _ in range(2)]
        QD = [kb.sb(st, "qd", [128, 2, 128], BF16) for _ in range(2)]
        OG = [kb.sb(st, "og", [128, 512], BF16) for _ in range(2)]
        KD = [kb.sb(st, "kd", [128, 256], BF16) for _ in range(2)]
        SS = kb.sb(st, "ss", [128, 2], F32)
        junk = kb.sb(st, "junkr", [128, 512], BF16)
        SST = [kb.sb(st, "sst", [128, 2, 512], F32) for _ in range(2)]
        S0B = [kb.sb(st, "s0b", [128, 2, 512], BF16) for _ in range(2)]
        QP = [kb.sb(st, "qp", [128, 2, 64], BF16) for _ in range(2)]
        KDB = [kb.sb(st, "kdb", [128, 256], BF16) for _ in range(2)]
        wsrc = w_in.rearrange("(kc p) n -> p kc n", p=128)
        gidx = [0]
        for h in range(4):
            wqk = Wqk[0]
            kb.dma(POOL, wqk.t[:, :, 0:256], wsrc[:, :, h * 256:(h + 1) * 256], writes=[wqk])
            kb.dma(POOL, wqk.t[:, :, 256:512], wsrc[:, :, 1024 + h * 256:1024 + (h + 1) * 256], writes=[wqk])
            kb.dma(POOL, Wv.t[:], wsrc[:, :, 2048 + h * 512:2048 + (h + 1) * 512], writes=[Wv])
            kb.dma(POOL, Wg.t[:], wsrc[:, :, 4096 + h * 512:4096 + (h + 1) * 512], writes=[Wg])
            kb.dma(POOL, Wo.t[:], w_out[h * 512:(h + 1) * 512, :].rearrange("(c p) n -> p c n", p=128), writes=[Wo])
            dmh = C("dm")[:, h * 128:(h + 1) * 128]
            qdh = C("qd")[:, h * 128:(h + 1) * 128]
            cdec = float(np.exp(np.log1p(-np.exp2(-5.0 - h)) * 128.0))
            cdec4 = float(np.exp(np.log1p(-np.exp2(-5.0 - h)) * 4.0))

            def gn_gate(po, rows, gt, og):
                kb.op(ACT, lambda: S.activation(out=junk.t[0:rows, :], in_=po.t[0:rows, :], func=AF.Square,
                                                accum_out=SS.t[0:rows, 0:1]), reads=[po], writes=[junk, SS])
                kb.op(ACT, lambda: S.activation(out=SS.t[0:rows, 0:1], in_=SS.t[0:rows, 0:1], func=AF.Sqrt,
                                                bias=EPSB_.t[0:rows, 0:1], scale=1.0 / 512), reads=[SS, EPSB_], writes=[SS])
                kb.op(DVE, lambda: V.reciprocal(out=SS.t[0:rows, 0:1], in_=SS.t[0:rows, 0:1]), reads=[SS], writes=[SS])
                kb.op(DVE, lambda: V.scalar_tensor_tensor(out=og.t[0:rows, :], in0=po.t[0:rows, :], scalar=SS.t[0:rows, 0:1],
                                                          in1=gt.t[0:rows, :], op0=ALU.mult, op1=ALU.mult),
                      reads=[po, SS, gt], writes=[og])

            def qkproj(ti):
                t0, n = TT[ti]
                rtile = RT[ti % 2]
                kb.dma(SP, rtile.t[:, :, 0:n], rot[:, :, t0:t0 + n], writes=[rtile])
                qkt = QK[ti % 2]
                cos = rtile.t[:, 0, 0:n]
                sin = rtile.t[:, 1, 0:n]
                for which in range(2):
                    sc = 1.0 if which == 0 else 1.0 / 16
                    pa, pb = PS[0], PS[1]
                    kb.mm(pa, pa.t[:, 0:n], [(wqk.t[:, kc, which * 256:which * 256 + 128], hT.t[:, kc, t0:t0 + n]) for kc in range(8)], [wqk, hT])
                    kb.mm(pb, pb.t[:, 0:n], [(wqk.t[:, kc, which * 256 + 128:which * 256 + 256], hT.t[:, kc, t0:t0 + n]) for kc in range(8)], [wqk, hT])
                    for part in range(2):
                        ca, cb = (cos, sin) if part == 0 else (sin, cos)
                        kb.op(DVE, lambda: V.scalar_tensor_tensor(out=t1.t[:, 0:n], in0=pa.t[:, 0:n], scalar=sc, in1=ca, op0=ALU.mult, op1=ALU.mult),
                              reads=[pa, rtile], writes=[t1])
                        kb.op(DVE, lambda: V.scalar_tensor_tensor(out=t2.t[:, 0:n], in0=pb.t[:, 0:n], scalar=sc, in1=cb, op0=ALU.mult, op1=ALU.mult),
                              reads=[pb, rtile], writes=[t2])
                        kb.op(DVE, lambda: V.tensor_tensor(out=qkt.t[:, 2 * which + part, 0:n], in0=t1.t[:, 0:n], in1=t2.t[:, 0:n],
                                                           op=(ALU.subtract if part == 0 else ALU.add)), reads=[t1, t2], writes=[qkt])

            qkproj(0)
            for ti, (t0, n) in enumerate(TT):
                is_s = (ti == 4)
                qkt = QK[ti % 2]
                if ti + 1 < len(TT):
                    qkproj(ti + 1)
                ogt = OGT[ti % 2]
                if not is_s:
                    def stageA(cc, ti_=None):
                        ti_ = ti if ti_ is None else ti_
                        qkt_ = QK[ti_ % 2]
                        gi = ti_ * 4 + cc
                        first = (ti_ == 0 and cc == 0)
                        cs = cc * 128
                        tok0 = TT[ti_][0] + cs
                        vb, gt, atm, qd, kd = VB[gi % 2], GT[gi % 2], ATM[gi % 2], QD[gi % 2], KD[gi % 2]
                        pv, pg = PS[0], PS[1]
                        kb.mm(pv, pv.t[:], [(hT.t[:, kc, tok0:tok0 + 128], Wv.t[:, kc, :]) for kc in range(8)], [hT, Wv])
                        kb.mm(pg, pg.t[:], [(hT.t[:, kc, tok0:tok0 + 128], Wg.t[:, kc, :]) for kc in range(8)], [hT, Wg])
                        kb.op(ACT, lambda: S.copy(out=vb.t[:], in_=pv.t[:]), reads=[pv], writes=[vb])
                        kb.op(ACT, lambda: S.activation(out=gt.t[:], in_=pg.t[:], func=AF.Silu), reads=[pg], writes=[gt])
                        pa = PS[2]
                        kb.mm(pa, pa.t[:, 0:128], [(qkt_.t[:, 2 + hf, cs:cs + 128], qkt_.t[:, hf, cs:cs + 128]) for hf in range(2)], [qkt_])
                        kb.op(DVE, lambda: V.tensor_tensor(out=atm.t[:], in0=pa.t[:, 0:128], in1=dmh, op=ALU.mult), reads=[pa, CST], writes=[atm])
                        if not first:
                            kb.op(DVE, lambda: V.tensor_tensor(out=qd.t[:], in0=qkt_.t[:, 0:2, cs:cs + 128],
                                                               in1=qdh.unsqueeze(1).to_broadcast([128, 2, 128]), op=ALU.mult),
                                  reads=[qkt_, CST], writes=[qd])
                        pab = pa.t[:].bitcast(BF16)
                        kb.transposes(pa, [(pab[:, 256 + hf * 128:256 + (hf + 1) * 128], qkt_.t[:, 2 + hf, cs:cs + 128]) for hf in range(2)], [qkt_], identb)
                        kb.op(DVE, lambda: V.tensor_scalar(out=kd.t[:], in0=pab[:, 256:512], scalar1=C("kd")[:, h:h + 1], scalar2=None,
                                                           op0=ALU.mult), reads=[pa, CST], writes=[kd])

                    def stageB(cc):
                        gi = ti * 4 + cc
                        first = (ti == 0 and cc == 0)
                        cs = cc * 128
                        vb, gt, atm, qd, og, kd = VB[gi % 2], GT[gi % 2], ATM[gi % 2], QD[gi % 2], OG[gi % 2], KD[gi % 2]
                        po = PS[3]
                        pairs = [(atm.t[:], vb.t[:])]
                        rds = [atm, vb]
                        if not first:
                            pairs += [(qd.t[:, hf, :], Sb.t[:, hf, :]) for hf in range(2)]
                            rds += [qd, Sb]
                        kb.mm(po, po.t[:], pairs, rds)
                        gn_gate(po, 128, gt, og)
                        pt = PS[4]
                        ptb = pt.t[:].bitcast(BF16)
                        kb.transposes(pt, [(ptb[:, ec * 128:(ec + 1) * 128], og.t[:, ec * 128:(ec + 1) * 128]) for ec in range(4)], [og], identb)
                        kb.op(ACT, lambda: S.copy(out=ogt.t[:, :, cs:cs + 128], in_=ptb[:, 0:512].rearrange("p (e t) -> p e t", e=4)),
                              reads=[pt], writes=[ogt])
                        for hf in range(2):
                            ps_ = PS[5 + hf]
                            kb.mm(ps_, ps_.t[:], [(kd.t[:, hf * 128:(hf + 1) * 128], vb.t[:])], [kd, vb])
                            if first:
                                kb.op(DVE, lambda: V.tensor_copy(out=Sf.t[:, hf, :], in_=ps_.t[:]), reads=[ps_], writes=[Sf])
                            else:
                                kb.op(DVE, lambda: V.scalar_tensor_tensor(out=Sf.t[:, hf, :], in0=Sf.t[:, hf, :], scalar=cdec, in1=ps_.t[:],
                                                                          op0=ALU.mult, op1=ALU.add), reads=[Sf, ps_], writes=[Sf])
                        kb.op(ACT, lambda: S.copy(out=Sb.t[:], in_=Sf.t[:]), reads=[Sf], writes=[Sb])

                    if ti == 0:
                        stageA(0)
                    for cc in range(4):
                        if cc + 1 < 4:
                            stageA(cc + 1)
                        elif ti + 1 < 4:
                            stageA(0, ti + 1)
                        stageB(cc)
                    if ti == 3:
                        kb.dma(SP, r_p[h].rearrange("(hf p) e -> p hf e", p=128), Sf.t[:], reads=[Sf])
                else:
                    vb, gt, atm, og, kd = VB[0], GT[0], ATM[0], OG[0], KD[0]
                    pv, pg = PS[0], PS[1]
                    kb.mm(pv, pv.t[0:NS, :], [(hT.t[:, kc, t0:t0 + NS], Wv.t[:, kc, :]) for kc in range(8)], [hT, Wv])
                    kb.mm(pg, pg.t[0:NS, :], [(hT.t[:, kc, t0:t0 + NS], Wg.t[:, kc, :]) for kc in range(8)], [hT, Wg])
                    kb.op(ACT, lambda: S.copy(out=vb.t[0:NS, :], in_=pv.t[0:NS, :]), reads=[pv], writes=[vb])
                    kb.op(ACT, lambda: S.activation(out=gt.t[0:NS, :], in_=pg.t[0:NS, :], func=AF.Silu), reads=[pg], writes=[gt])
                    pa = PS[2]
                    kb.mm(pa, pa.t[0:NS, 0:NS], [(qkt.t[:, 2 + hf, 0:NS], qkt.t[:, hf, 0:NS]) for hf in range(2)], [qkt])
                    kb.op(DVE, lambda: V.tensor_tensor(out=atm.t[0:NS, 0:NS], in0=pa.t[0:NS, 0:NS], in1=C("dmS")[0:NS, h * 64:(h + 1) * 64],
                                                       op=ALU.mult), reads=[pa, CST], writes=[atm])
                    qds = QD[0]
                    kb.op(DVE, lambda: V.tensor_tensor(out=qds.t[:, :, 0:NS], in0=qkt.t[:, 0:2, 0:NS],
                                                       in1=C("qdS")[:, h * 64:(h + 1) * 64].unsqueeze(1).to_broadcast([128, 2, NS]), op=ALU.mult),
                          reads=[qkt, CST], writes=[qds])
                    pab = pa.t[:].bitcast(BF16)
                    kb.transposes(pa, [(pab[0:NS, 256 + hf * 128:256 + (hf + 1) * 128], qkt.t[:, 2 + hf, 0:NS]) for hf in range(2)], [qkt], identb)
                    kb.op(DVE, lambda: V.tensor_scalar(out=kd.t[0:NS, :], in0=pab[0:NS, 256:512], scalar1=C("kdS")[0:NS, h:h + 1], scalar2=None,
                                                       op0=ALU.mult), reads=[pa, CST], writes=[kd])
                    po = PS[3]
                    kb.mm(po, po.t[0:NS, :], [(atm.t[0:NS, 0:NS], vb.t[0:NS, :])], [atm, vb], start=True, stop=False)
                    al_src = [RT[1], QK[1], OGT[1]]
                    al = []
                    for o_ in al_src:
                        ap_ = o_.t[:] if o_ is RT[1] else o_.t[:].rearrange("p a b -> p (a b)").bitcast(F32).rearrange("p (h e) -> p h e", h=2)
                        fz = dict(o_.rd)
                        if o_.lw and fz.get(o_.lw[0], (None, 0))[1] < o_.lw[2]:
                            fz[o_.lw[0]] = (o_.lw[1], o_.lw[2])
                        al.append(Buf(ap_, o_.name + "_al", fz))
                    stbufs = SST + al
                    def st_load(bb):
                        kb.dma(SP, stbufs[bb % 5].t[:], st_in[bb, h].rearrange("(hf p) e -> p hf e", p=128), writes=[stbufs[bb % 5]])
                    for bb in range(4):
                        st_load(bb)
                    for b in range(NSEQ):
                        stb, s0b, qp, kdb = stbufs[b % 5], S0B[b % 2], QP[b % 2], KDB[b % 2]
                        kb.op(ACT, lambda: S.copy(out=s0b.t[:], in_=stb.t[:]), reads=[stb], writes=[s0b])
                        kb.op(DVE, lambda: V.tensor_tensor(out=qp.t[:], in0=qds.t[:, :, 0:NS],
                                                           in1=C("bmc")[:, b * 64:(b + 1) * 64].unsqueeze(1).to_broadcast([128, 2, NS]), op=ALU.mult),
                              reads=[qds, CST], writes=[qp])
                        kb.mm(po, po.t[0:NS, :], [(qp.t[:, hf, :], s0b.t[:, hf, :]) for hf in range(2)], [qp, s0b], start=False, stop=(b == NSEQ - 1))
                        kb.op(DVE, lambda: V.tensor_scalar(out=kdb.t[0:NS, :], in0=kd.t[0:NS, :], scalar1=C("bm")[0:NS, b:b + 1], scalar2=None,
                                                           op0=ALU.mult), reads=[kd, CST], writes=[kdb])
                        for hf in range(2):
                            ps_ = PS[5 + hf]
                            kb.mm(ps_, ps_.t[:], [(kdb.t[0:NS, hf * 128:(hf + 1) * 128], vb.t[0:NS, :])], [kdb, vb])
                            kb.op(DVE, lambda: V.scalar_tensor_tensor(out=stb.t[:, hf, :], in0=stb.t[:, hf, :], scalar=cdec4, in1=ps_.t[:],
                                                                      op0=ALU.mult, op1=ALU.add), reads=[stb, ps_], writes=[stb])
                        kb.dma(SP, r_s[b, h].rearrange("(hf p) e -> p hf e", p=128), stb.t[:], reads=[stb])
                        if b + 4 < NSEQ:
                            st_load(b + 4)
                    for o_, a_ in zip(al_src, al):
                        for tk in ([a_.lw] if a_.lw else []):
                            if o_.rd.get(tk[0], (None, 0))[1] < tk[2]:
                                o_.rd[tk[0]] = (tk[1], tk[2])
                        for k_, v_ in a_.rd.items():
                            if o_.rd.get(k_, (None, 0))[1] < v_[1]:
                                o_.rd[k_] = v_
                        if a_.dsem:
                            kb.dpool.extend(a_.dsem.values())
                    gn_gate(po, NS, gt, og)
                    pt = PS[4]
                    ptb = pt.t[:].bitcast(BF16)
                    kb.transposes(pt, [(ptb[:, ec * 128:ec * 128 + NS], og.t[0:NS, ec * 128:(ec + 1) * 128]) for ec in range(4)], [og], identb)
                    kb.op(ACT, lambda: S.copy(out=ogt.t[:, :, 0:NS], in_=ptb[:, 0:512].rearrange("p (e t) -> p e t", e=4)[:, :, 0:NS]),
                          reads=[pt], writes=[ogt])
                for oc in range(8):
                    pb = PS[7]
                    kb.mm(pb, pb.t[:, 0:n], [(Wo.t[:, ec, oc * 128:(oc + 1) * 128], ogt.t[:, ec, 0:n]) for ec in range(4)], [Wo, ogt])
                    kb.op(DVE, lambda: V.tensor_tensor(out=xT.t[:, oc, t0:t0 + n], in0=xT.t[:, oc, t0:t0 + n], in1=pb.t[:, 0:n], op=ALU.add),
                          reads=[xT, pb], writes=[xT])


_CACHE = {}
LEVEL = 6
DEBUG = False
DBG_OUT = {}


def kernel(x_prompt, x_sample, cache_k, cache_v, cache_kidx, state_ret, page_table, rel_bias, ln_mix, ln_mlp,
           att_w_in, att_q_gain, att_k_gain, att_w_out, ret_w_in, ret_w_out, mlp_w_in, mlp_w_out):
    if "nc" not in _CACHE:
        _CACHE["nc"] = build_program(LEVEL)
    nc, (cst_np, cstr_np) = _CACHE["nc"]
    rot = _build_rot()
    f = lambda a: np.ascontiguousarray(np.asarray(a, dtype=np.float32))
    gl = np.concatenate([np.asarray(v, np.float32).reshape(8, 128).T for v in (ln_mix[0], ln_mlp[0], ln_mix[1], ln_mlp[1])], axis=1)
    shared = {
        "cache_k": f(cache_k).reshape(NPHYS * 128, 256), "cache_v": f(cache_v).reshape(NPHYS * 128, 256),
        "cache_i": f(cache_kidx).reshape(NPHYS * 128, 64),
        "relb": f(rel_bias).reshape(1, 256), "gains": np.ascontiguousarray(gl),
        "qkg": np.ascontiguousarray(np.stack([f(att_q_gain)[0], f(att_k_gain)[0]], axis=1)),
        "kg_row": f(att_k_gain).reshape(1, 128),
        "w_att_in": f(att_w_in)[0], "w_att_out": f(att_w_out)[0], "w_ret_in": f(ret_w_in)[0], "w_ret_out": f(ret_w_out)[0],
        "w_mlp_in": f(mlp_w_in), "w_mlp_out": f(mlp_w_out), "cst": cst_np, "cstr": cstr_np, "rot": rot,
    }
    xp = f(x_prompt)
    xs = f(x_sample)
    stt = f(state_ret)
    pt = np.ascontiguousarray(np.asarray(page_table, dtype=np.int32))
    in_maps = []
    for i in range(NCORES):
        m = dict(shared)
        m["x_p"] = xp[i]
        m["x_s"] = xs[16 * i:16 * i + 16].reshape(NS, D)
        m["st_in"] = stt[0, 16 * i:16 * i + 16]
        m["ptab"] = pt[16 * i:16 * i + 16].reshape(1, 256)
        in_maps.append(m)
    res = run_bass_kernel_spmd(nc, in_maps, core_ids=list(range(NCORES)))
    R = res.results
    if DEBUG:
        for k_ in ('dbgI', 'dbgA', 'dbgL', 'dbgN', 'dbgD', 'dbgW', 'dbgG', 'dbgB', 'dbgT', 'dbgM'):
            DBG_OUT[k_] = np.asarray(R[0][k_])
    cat = lambda k: np.stack([np.asarray(r[k]) for r in R])
    y_p = cat("y_p").astype(np.float32)
    y_s = cat("y_s").reshape(128, 4, D).astype(np.float32)
    k_p = cat("k_p").reshape(1, 8, SEQ, 2, 128).astype(np.float32)
    v_p = cat("v_p").reshape(1, 8, SEQ, 2, 128).astype(np.float32)
    i_p = cat("i_p").reshape(1, 8, SEQ, 64).astype(np.float32)
    r_p = cat("r_p").reshape(1, 8, 4, 256, 512).astype(np.float32)
    k_s = cat("k_s").reshape(1, 128, 4, 2, 128).astype(np.float32)
    v_s = cat("v_s").reshape(1, 128, 4, 2, 128).astype(np.float32)
    i_s = cat("i_s").reshape(1, 128, 4, 64).astype(np.float32)
    r_s = cat("r_s").reshape(1, 128, 4, 256, 512).astype(np.float32)
    return (y_p, y_s, k_p, v_p, i_p, r_p, k_s, v_s, i_s, r_s)
```

```python
import math
from contextlib import ExitStack
import numpy as np
import concourse.bass as bass
import concourse.mybir as mybir
from concourse.bass_utils import run_bass_kernel_spmd

F32 = mybir.dt.float32
BF16 = mybir.dt.bfloat16
I32 = mybir.dt.int32
ALU = mybir.AluOpType
AF = mybir.ActivationFunctionType
AX = mybir.AxisListType

NCORES = 8
D = 1024
SEQ = 2048
NS = 64
NSEQ = 16
T = SEQ + NS
NPHYS = 2560
EPS = 1e-6
NEG = -30000.0
BIGNEG = -1.0e30
NBIS = 24
TT = [(0, 512), (512, 512), (1024, 512), (1536, 512), (2048, 64)]
ATT_IN = 1860
GAM = [1.0 - 2.0 ** (-5.0 - h) for h in range(4)]


def _t5_bucket(dist):
    n = np.maximum(dist, 0)
    nf = np.maximum(n, 1).astype(np.float32)
    large = 16 + (np.log(nf / np.float32(16)) / np.float32(math.log(128 / 16)) * np.float32(16)).astype(np.int32)
    large = np.minimum(large, 31)
    return np.where(n < 16, n, large)


def _build_consts():
    c = {}
    p = np.arange(128)
    c["ident"] = np.eye(128, dtype=np.float32)
    t = p[:, None]
    s = p[None, :]
    c["causneg"] = np.where(s <= t, 0.0, BIGNEG).astype(np.float32)
    delta = np.arange(256)[None, :] - p[:, None]
    c["bkt_p"] = np.where(delta >= 0, _t5_bucket(delta), 31).astype(np.float32)
    pos = (128 * (p // 8) + 16 * (p % 8))[:, None, None] + np.arange(16)[None, :, None]
    dl = 2048 + np.arange(4)[None, None, :] - pos
    c["bkt_s"] = _t5_bucket(dl).astype(np.float32).reshape(128, 64)
    bp = np.arange(64) // 4
    tp = np.arange(64) % 4
    valid = (bp[:, None, None] == np.arange(16)[None, :, None]) & (tp[:, None, None] <= np.arange(4)[None, None, :])
    c["newvalid"] = np.where(valid, 0.0, BIGNEG).astype(np.float32).reshape(64, 64)
    c["newvalid"] = np.concatenate([c["newvalid"], np.zeros((64, 64), np.float32)], 0)
    bn = np.clip(np.arange(4)[None, None, :] - tp[:, None, None], 0, 31) * np.ones((1, 16, 1))
    c["bkt_n"] = np.concatenate([bn.reshape(64, 64).astype(np.float32), np.full((64, 64), 31, np.float32)], 0)
    i = np.arange(128, dtype=np.float64)
    dm = np.zeros((128, 4, 128), np.float64)
    qd = np.zeros((128, 4, 128), np.float64)
    kd = np.zeros((128, 4), np.float64)
    dmS = np.zeros((128, 4, 64), np.float64)
    qdS = np.zeros((128, 4, 64), np.float64)
    kdS = np.zeros((128, 4), np.float64)
    for h in range(4):
        lg = np.log1p(-np.exp2(-5.0 - h))
        diff = i[None, :] - i[:, None]
        dm[:, h, :] = np.where(diff >= 0, np.exp(lg * np.maximum(diff, 0)), 0.0)
        qd[:, h, :] = np.exp(lg * (i + 1.0))[None, :]
        kd[:, h] = np.exp(lg * (127.0 - i))
        ii = np.arange(64)
        dS = (ii % 4)[None, :] - (ii % 4)[:, None]
        same = (ii // 4)[None, :] == (ii // 4)[:, None]
        dmS[:64, h, :] = np.where(same & (dS >= 0), np.exp(lg * np.maximum(dS, 0)), 0.0)
        qdS[:, h, :] = np.exp(lg * ((ii % 4) + 1.0))[None, :]
        kdS[:64, h] = np.exp(lg * (3.0 - (ii % 4)))
    c["dm"] = dm.reshape(128, 512).astype(np.float32)
    c["qd"] = qd.reshape(128, 512).astype(np.float32)
    c["kd"] = kd.astype(np.float32)
    c["dmS"] = dmS.reshape(128, 256).astype(np.float32)
    c["qdS"] = qdS.reshape(128, 256).astype(np.float32)
    c["kdS"] = kdS.astype(np.float32)
    bm = np.zeros((128, 16), np.float32)
    bm[np.arange(64), np.arange(64) // 4] = 1.0
    c["bm"] = bm
    bmc = np.zeros((128, 16, 64), np.float32)
    for b in range(16):
        bmc[:, b, 4 * b:4 * b + 4] = 1.0
    c["bmc"] = bmc.reshape(128, 1024)
    c["iota_p"] = np.arange(128, dtype=np.float32).reshape(128, 1)
    c["pofs"] = (16 * (np.arange(128) % 8)).astype(np.float32).reshape(128, 1)
    sel = np.zeros((128, 16), np.float32)
    sel[np.arange(128), np.arange(128) // 8] = 1.0
    c["pagesel"] = sel
    dd = 255 - np.arange(383)
    bk_d = np.where(dd >= 0, _t5_bucket(dd), 31)
    gr = np.zeros((128, 383), np.float32)
    gr[bk_d, np.arange(383)] = 1.0
    c["gr"] = gr
    c["bkt_all"] = np.concatenate([c.pop("bkt_p"), c.pop("bkt_s"), c.pop("bkt_n")], axis=1)
    c["pow2"] = np.tile((2.0 ** -(np.arange(NBIS) + 1.0)).astype(np.float32)[None, :], (128, 1))
    rkeys = ("dm", "qd", "kd", "dmS", "qdS", "kdS", "bm", "bmc", "gr")
    outs = []
    for keys in ([k for k in c if k not in rkeys], list(rkeys)):
        offs = {}
        cols = []
        o = 0
        for k in keys:
            v = c[k]
            offs[k] = (o, v.shape[1])
            cols.append(v)
            o += v.shape[1]
        outs.append((np.ascontiguousarray(np.concatenate(cols, axis=1)), offs))
    return outs


def _build_rot():
    half = 128
    theta = (1.0 / (10000.0 ** np.linspace(0.0, 1.0, half, dtype=np.float32))).astype(np.float32)
    pos = np.concatenate([np.arange(SEQ), np.tile(2048 + np.arange(4), NSEQ)]).astype(np.float32)
    ang = (theta[:, None] * pos[None, :]).astype(np.float32)
    return np.ascontiguousarray(np.stack([np.cos(ang), np.sin(ang)], axis=1).astype(np.float32))


class Buf:
    def __init__(self, t, name, fence):
        self.t = t
        self.name = name
        self.lw = None
        self.rd = dict(fence)
        self.dsem = None
        self.dcnt = 0
        self.excl = False

    def __getitem__(self, k):
        return self.t[k]


class Eng:
    def __init__(self, h, sem, key, is_pe=False):
        self.h = h
        self.sem = sem
        self.key = key
        self.cnt = 0
        self.seen = {}
        self.is_pe = is_pe


class KB:
    def __init__(self, nc):
        self.nc = nc
        self.es = ExitStack()
        self.fence = {}
        self.dma_bufs = []
        self.nsem = 0
        self.uid = 0
        self.dpool = []
        self.dsems = []
        mk = lambda n: self.es.enter_context(nc.semaphore(n))
        self.PE = Eng(nc.tensor, mk("s_pe"), "pe", True)
        self.ACT = Eng(nc.scalar, mk("s_act"), "act")
        self.DVE = Eng(nc.vector, mk("s_dve"), "dve")
        self.POOL = Eng(nc.gpsimd, mk("s_pool"), "pool")
        self.SP = Eng(nc.sync, mk("s_sp"), "sp")

    def sb(self, st, name, shape, dt):
        self.uid += 1
        t = st.enter_context(self.nc.sbuf_tensor("%s_%d" % (name, self.uid), list(shape), dt))
        b = Buf(t, name, self.fence)
        st.callback(self._free, b)
        return b

    def _free(self, b):
        for tk in ([b.lw] if b.lw else []) + list(b.rd.items()):
            if isinstance(tk, tuple) and len(tk) == 2 and isinstance(tk[1], tuple):
                key, (sem, val) = tk
            else:
                key, sem, val = tk
            if self.fence.get(key, (None, 0))[1] < val:
                self.fence[key] = (sem, val)
        if b.dsem is not None:
            self.dpool.extend(b.dsem.values())
            b.dsem = None

    def psum(self, name):
        t = self.es.enter_context(self.nc.psum_tensor(name, [128, 512], F32))
        b = Buf(t, name, {})
        b.excl = True
        return b

    def _deps(self, eng, reads, writes):
        deps = {}

        def add(key, sem, val, war):
            if key == eng.key and eng.is_pe:
                return
            if deps.get(key, (None, 0))[1] < val:
                deps[key] = (sem, val)

        for b in reads:
            if b.lw:
                add(b.lw[0], b.lw[1], b.lw[2], False)
            if b.excl:
                for key, (sem, val) in b.rd.items():
                    if key != eng.key:
                        add(key, sem, val, True)
        for b in writes:
            if b.lw:
                add(b.lw[0], b.lw[1], b.lw[2], False)
            for key, (sem, val) in b.rd.items():
                add(key, sem, val, True)
        for key, (sem, val) in deps.items():
            if eng.seen.get(key, 0) < val:
                eng.h.wait_ge(sem, val)
                eng.seen[key] = val

    def _commit(self, tk, reads, writes):
        key, sem, val = tk
        for b in reads:
            if b.rd.get(key, (None, 0))[1] < val:
                b.rd[key] = (sem, val)
        for b in writes:
            b.lw = tk
            b.rd = {}

    def op(self, eng, fn, reads=(), writes=()):
        self._deps(eng, reads, writes)
        inst = fn()
        eng.cnt += 1
        inst.then_inc(eng.sem, 1)
        self._commit((eng.key, eng.sem, eng.cnt), reads, writes)

    def mm(self, out_buf, out_ap, pairs, reads, start=True, stop=True):
        eng = self.PE
        self._deps(eng, reads, [out_buf])
        n = len(pairs)
        inst = None
        for i, (l, r) in enumerate(pairs):
            inst = self.nc.tensor.matmul(out_ap, lhsT=l, rhs=r, start=(start and i == 0), stop=(stop and i == n - 1))
        eng.cnt += 1
        inst.then_inc(eng.sem, 1)
        self._commit((eng.key, eng.sem, eng.cnt), reads, [out_buf])

    def transposes(self, out_buf, items, reads, ident):
        eng = self.PE
        self._deps(eng, list(reads) + [ident], [out_buf])
        inst = None
        for (o, i) in items:
            inst = self.nc.tensor.transpose(o, i, ident.t[0:i.shape[0], 0:i.shape[0]])
        eng.cnt += 1
        inst.then_inc(eng.sem, 1)
        self._commit((eng.key, eng.sem, eng.cnt), list(reads) + [ident], [out_buf])

    def dma(self, q, out_ap, in_ap, reads=(), writes=(), indirect=None):
        self._deps(q, reads, writes)
        b = (list(writes) + list(reads))[0]
        if b.dsem is None:
            b.dsem = {}
        if q.key not in b.dsem:
            pool = [d for d in self.dpool if d[3] == q.key]
            if pool:
                d0 = pool[-1]
                b.dsem[q.key] = d0
                self.dpool.remove(d0)
                if q.seen.get(d0[1], 0) < d0[2]:
                    q.h.wait_ge(d0[0], d0[2])
                    q.seen[d0[1]] = d0[2]
            else:
                self.nsem += 1
                ds = [self.es.enter_context(self.nc.semaphore("dsem%d" % self.nsem)), "dsem%d" % self.nsem, 0, q.key]
                self.dsems.append(ds)
                b.dsem[q.key] = ds
        ds = b.dsem[q.key]
        ds[2] += 16
        if indirect is not None:
            inst = q.h.indirect_dma_start(out=out_ap, out_offset=None, in_=in_ap, in_offset=indirect)
        else:
            inst = q.h.dma_start(out=out_ap, in_=in_ap)
        inst.then_inc(ds[0], 16)
        self._commit((ds[1], ds[0], ds[2]), reads, writes)

    def finish(self):
        for ds in self.dsems:
            if self.SP.seen.get(ds[1], 0) < ds[2]:
                self.nc.sync.wait_ge(ds[0], ds[2])
                self.SP.seen[ds[1]] = ds[2]
        for e in (self.PE, self.ACT, self.DVE, self.POOL):
            if e.cnt > 0:
                self.nc.sync.wait_ge(e.sem, e.cnt)


def build_program(level=99):
    nc = bass.Bass("TRN2", target_bir_lowering=False)
    (cst_np, CO), (cstr_np, COR) = _build_consts()
    NCST = cst_np.shape[1]
    dr = lambda n, s, dt=F32, kind="ExternalInput": nc.dram_tensor(n, list(s), dt, kind=kind).ap()
    x_p = dr("x_p", [SEQ, D])
    x_s = dr("x_s", [NS, D])
    cache_k = dr("cache_k", [NPHYS * 128, 256])
    cache_v = dr("cache_v", [NPHYS * 128, 256])
    cache_i = dr("cache_i", [NPHYS * 128, 64])
    st_in = dr("st_in", [NSEQ, 4, 256, 512])
    ptab = dr("ptab", [1, NSEQ * 16], I32)
    relb = dr("relb", [1, 256])
    gains = dr("gains", [128, 32])
    qkg = dr("qkg", [128, 2])
    kg_row = dr("kg_row", [1, 128])
    w_att_in = dr("w_att_in", [D, ATT_IN])
    w_att_out = dr("w_att_out", [D, D])
    w_ret_in = dr("w_ret_in", [D, 6144])
    w_ret_out = dr("w_ret_out", [2048, D])
    w_mlp_in = dr("w_mlp_in", [2, D, 4096])
    w_mlp_out = dr("w_mlp_out", [2, 4096, D])
    cst = dr("cst", [128, NCST])
    cstr = dr("cstr", [128, cstr_np.shape[1]])
    rot = dr("rot", [128, 2, T])
    OUT = "ExternalOutput"
    y_p = dr("y_p", [SEQ, D], kind=OUT)
    y_s = dr("y_s", [NS, D], kind=OUT)
    k_p = dr("k_p", [SEQ, 256], kind=OUT)
    v_p = dr("v_p", [SEQ, 256], kind=OUT)
    i_p = dr("i_p", [SEQ, 64], kind=OUT)
    r_p = dr("r_p", [4, 256, 512], kind=OUT)
    k_s = dr("k_s", [NS, 256], kind=OUT)
    v_s = dr("v_s", [NS, 256], kind=OUT)
    i_s = dr("i_s", [NS, 64], kind=OUT)
    r_s = dr("r_s", [NSEQ, 4, 256, 512], kind=OUT)

    DBG = {}
    if DEBUG:
        DBG["I"] = dr("dbgI", [128, 32], I32, kind=OUT)
        DBG["A"] = dr("dbgA", [128, 1024], kind=OUT)
        DBG["L"] = dr("dbgL", [128, 64], kind=OUT)
        DBG["N"] = dr("dbgN", [128, 64], kind=OUT)
        DBG["D"] = dr("dbgD", [128, 32], kind=OUT)
        DBG["W"] = dr("dbgW", [128, 256], kind=OUT)
        DBG["G"] = dr("dbgG", [128, 1024], kind=OUT)
        DBG["B"] = dr("dbgB", [128, 2048], BF16, kind=OUT)
        DBG["T"] = dr("dbgT", [128, 2048], BF16, kind=OUT)
        DBG["M"] = dr("dbgM", [128, 256], kind=OUT)
    kb = KB(nc)
    kb.DBG = DBG
    PE, ACT, DVE, POOL, SP = kb.PE, kb.ACT, kb.DVE, kb.POOL, kb.SP
    V = nc.vector
    S = nc.scalar
    G = nc.gpsimd
    PS = [kb.psum("ps%d" % i) for i in range(8)]
    top = kb.es

    def C(name, rows=128):
        o, w = CO[name]
        return CST.t[0:rows, o:o + w]

    CST = kb.sb(top, "cst", [128, NCST], F32)
    kb.dma(SP, CST.t[:], cst, writes=[CST])
    xT = kb.sb(top, "xT", [128, 8, T], F32)
    identb = kb.sb(top, "identb", [128, 128], BF16)
    onesb = kb.sb(top, "onesb", [128, 128], BF16)
    ones_d = kb.sb(top, "ones_d", [128, 128], BF16)
    ones_h = kb.sb(top, "ones_h", [128, 128], BF16)
    gn = kb.sb(top, "gn", [128, 32], F32)
    qk_g = kb.sb(top, "qk_g", [128, 2], F32)
    kgbc = kb.sb(top, "kgbc", [128, 128], F32)
    kb.dma(SP, gn.t[:], gains, writes=[gn])
    kb.dma(SP, qk_g.t[:], qkg, writes=[qk_g])
    kb.dma(SP, kgbc.t[:], kg_row.partition_broadcast(128), writes=[kgbc])
    kb.op(DVE, lambda: V.tensor_copy(out=identb.t[:], in_=C("ident")), reads=[CST], writes=[identb])
    kb.op(DVE, lambda: V.memset(onesb.t[:], 1.0), writes=[onesb])
    kb.op(DVE, lambda: V.memset(ones_d.t[:], 1.0 / 1024), writes=[ones_d])
    kb.op(DVE, lambda: V.memset(ones_h.t[:], 1.0 / 128), writes=[ones_h])
    kb.op(DVE, lambda: V.tensor_scalar(out=qk_g.t[:, 0:1], in0=qk_g.t[:, 0:1], scalar1=128.0 ** -0.5, scalar2=None,
                                       op0=ALU.mult), reads=[qk_g], writes=[qk_g])

    class IdentF:
        pass
    identf = Buf(None, "identf", {})
    identf.t = CST.t[:, CO["ident"][0]:CO["ident"][0] + 128]
    identf_dep = CST

    rr = [0]

    def evac_engine():
        rr[0] ^= 1
        return ACT if rr[0] else DVE

    def copy(eng, out_ap, in_ap, reads, writes):
        if eng is ACT:
            kb.op(ACT, lambda: S.copy(out=out_ap, in_=in_ap), reads=reads, writes=writes)
        else:
            kb.op(DVE, lambda: V.tensor_copy(out=out_ap, in_=in_ap), reads=reads, writes=writes)

    with ExitStack() as st:
        xs = [kb.sb(st, "xs%d" % i, [128, D], F32) for i in range(2)]
        for c in range(17):
            n = 128 if c < 16 else NS
            src = x_p[c * 128:(c + 1) * 128, :] if c < 16 else x_s[:, :]
            xb = xs[c % 2]
            kb.dma(SP, xb.t[0:n, :], src, writes=[xb])
            for g in range(2):
                pb = PS[(2 * c + g) % 8]
                items = [(pb.t[:, j * 128:j * 128 + n], xb.t[0:n, (4 * g + j) * 128:(4 * g + j + 1) * 128]) for j in range(4)]
                eng = kb.PE
                kb._deps(eng, [xb, CST], [pb])
                inst = None
                for (o, i_) in items:
                    inst = nc.tensor.transpose(o, i_, identf.t[0:n, 0:n])
                eng.cnt += 1
                inst.then_inc(eng.sem, 1)
                kb._commit((eng.key, eng.sem, eng.cnt), [xb, CST], [pb])
                e = evac_engine()
                copy(e, xT.t[:, 4 * g:4 * g + 4, c * 128:c * 128 + n],
                     pb.t[:].rearrange("p (j t) -> p j t", j=4)[:, :, 0:n], [pb], [xT])

    def rmsnorm_tile(hbuf, h_ap, t0, n, gcol, sqb, rsb):
        kb.op(ACT, lambda: S.activation(out=sqb.t[:, :, 0:n], in_=xT.t[:, :, t0:t0 + n], func=AF.Square),
              reads=[xT], writes=[sqb])
        pb = PS[7]
        kb.mm(pb, pb.t[:, 0:n], [(ones_d.t[:], sqb.t[:, kc, 0:n]) for kc in range(8)], [ones_d, sqb])
        kb.op(ACT, lambda: S.activation(out=rsb.t[:, 0:n], in_=pb.t[:, 0:n], func=AF.Sqrt, bias=EPSB.t[:, 0:1], scale=1.0),
              reads=[pb, EPSB], writes=[rsb])
        kb.op(DVE, lambda: V.reciprocal(out=rsb.t[:, 0:n], in_=rsb.t[:, 0:n]), reads=[rsb], writes=[rsb])
        for kc in range(8):
            kb.op(DVE, lambda kc=kc: V.scalar_tensor_tensor(out=h_ap[:, kc, :], in0=xT.t[:, kc, t0:t0 + n],
                                                           scalar=gn.t[:, gcol + kc:gcol + kc + 1], in1=rsb.t[:, 0:n],
                                                           op0=ALU.mult, op1=ALU.mult),
                  reads=[xT, gn, rsb], writes=[hbuf])

    EPSB = kb.sb(top, "epsb", [128, 1], F32)
    kb.op(DVE, lambda: V.memset(EPSB.t[:], EPS), writes=[EPSB])

    def load_w(q, wbuf, w_ap_dst, src_ap):
        kb.dma(q, w_ap_dst, src_ap, writes=[wbuf])

    def add_resid(oc, t0, n, pb):
        kb.op(DVE, lambda: V.tensor_tensor(out=xT.t[:, oc, t0:t0 + n], in0=xT.t[:, oc, t0:t0 + n], in1=pb.t[:, 0:n],
                                           op=ALU.add), reads=[xT, pb], writes=[xT])

    with ExitStack() as L0:
        Win = kb.sb(L0, "Win", [128, 8, ATT_IN + 128], BF16)
        Wo = kb.sb(L0, "Wo", [128, 8, D], BF16)
        wsrc = w_att_in.rearrange("(kc p) n -> p kc n", p=128)
        for (a, b_) in [(0, 512), (512, 1024), (1536, ATT_IN), (1024, 1536)]:
            kb.dma(POOL, Win.t[:, :, a:b_], wsrc[:, :, a:b_], writes=[Win])
        kb.dma(POOL, Win.t[:, :, ATT_IN:ATT_IN + 64], wsrc[:, :, 1792:1856], writes=[Win])
        kb.dma(POOL, Win.t[:, :, ATT_IN + 64:ATT_IN + 128], wsrc[:, :, 1792:1856], writes=[Win])
        kb.dma(POOL, Wo.t[:], w_att_out.rearrange("(kc p) n -> p kc n", p=128), writes=[Wo])

        with ExitStack() as LK:
            kT = kb.sb(LK, "kT", [128, 2, T], BF16)
            Vall = kb.sb(LK, "Vall", [128, 17, 256], BF16)
            kiT = kb.sb(LK, "kiT", [128, T], BF16)
            WIa = kb.sb(LK, "WIa", [128, 17, 4], F32)
            WIs = kb.sb(LK, "WIs", [128, 17, 4], F32)
            biasN = kb.sb(LK, "biasN", [128, 2, 8, 128], BF16)
            biasS = kb.sb(LK, "biasS", [128, 16, 8, 4], F32)
            biasNn = kb.sb(LK, "biasNn", [128, 8, 64], F32)
            with ExitStack() as st:
                rb = kb.sb(st, "rb", [128, 256], F32)
                rbd = kb.sb(st, "rbd", [128, 256], F32)
                oh2 = kb.sb(st, "oh2", [128, 128], F32)
                tmp2 = kb.sb(st, "tmp2", [128, 8, 128], F32)
                bacc2 = kb.sb(st, "bacc2", [128, 8, 128], F32)
                rb32 = kb.sb(st, "rb32", [32, 8], F32)
                r31 = kb.sb(st, "r31", [32, 8], F32)
                rbdb = kb.sb(st, "rbdb", [32, 8], BF16)
                grb = kb.sb(st, "grb", [32, 383], BF16)
                kb.dma(SP, rb.t[:], relb.partition_broadcast(128), writes=[rb])
                kb.dma(SP, rb32.t[:], relb.rearrange("o (k h) -> (o k) h", h=8), writes=[rb32])
                kb.dma(SP, r31.t[:], relb[:, 248:256].partition_broadcast(32), writes=[r31])
                kb.op(DVE, lambda: V.tensor_tensor(out=rbdb.t[:], in0=rb32.t[:], in1=r31.t[:], op=ALU.subtract), reads=[rb32, r31], writes=[rbdb])
                grf = kb.sb(st, "grf", [32, 383], F32)
                kb.dma(SP, grf.t[:], cstr[0:32, COR["gr"][0]:COR["gr"][0] + 383], writes=[grf])
                kb.op(DVE, lambda: V.tensor_copy(out=grb.t[:], in_=grf.t[:]), reads=[grf], writes=[grb])
                for bank in range(4):
                    pbk = PS[bank]
                    items = []
                    for tl in range(64):
                        tp_ = bank * 64 + tl
                        items.append((pbk.t[:, tl * 8:tl * 8 + 8], grb.t[0:32, 255 - tp_:255 - tp_ + 128], rbdb.t[0:32, :]))
                    mm_multi(kb, nc, pbk, items, [grb, rbdb])
                    bb_, th = bank // 2, (bank % 2) * 64
                    copy_any(kb, nc, bank % 2, biasN.t[:, bb_, :, th:th + 64], pbk.t[:, 0:512].rearrange("p (t h) -> p h t", h=8), [pbk], [biasN])
                kb.op(POOL, lambda: G.tensor_tensor(out=rbd.t[:].rearrange("p (k h) -> p k h", h=8),
                                                    in0=rb.t[:].rearrange("p (k h) -> p k h", h=8),
                                                    in1=rb.t[:, 248:256].unsqueeze(1).to_broadcast([128, 32, 8]),
                                                    op=ALU.subtract), reads=[rb], writes=[rbd])
                kb.op(POOL, lambda: G.memset(bacc2.t[:], 0.0), writes=[bacc2])
                bk = C("bkt_all")
                for k in range(31):
                    kb.op(POOL, lambda k=k: G.tensor_single_scalar(out=oh2.t[:], in_=bk[:, 256:384], scalar=float(k), op=ALU.is_equal),
                          reads=[CST], writes=[oh2])
                    kb.op(POOL, lambda k=k: G.tensor_tensor(out=tmp2.t[:], in0=oh2.t[:].unsqueeze(1).to_broadcast([128, 8, 128]),
                                                            in1=rbd.t[:, k * 8:k * 8 + 8].unsqueeze(2).to_broadcast([128, 8, 128]), op=ALU.mult),
                          reads=[oh2, rbd], writes=[tmp2])
                    kb.op(POOL, lambda: G.tensor_tensor(out=bacc2.t[:], in0=bacc2.t[:], in1=tmp2.t[:], op=ALU.add),
                          reads=[bacc2, tmp2], writes=[bacc2])
                kb.op(POOL, lambda: G.tensor_copy(out=biasS.t[:], in_=bacc2.t[:, :, 0:64].rearrange("p h (c t) -> p c h t", t=4)),
                      reads=[bacc2], writes=[biasS])
                kb.op(POOL, lambda: G.tensor_copy(out=biasNn.t[:], in_=bacc2.t[:, :, 64:128]), reads=[bacc2], writes=[biasNn])

            for ti, (t0, n) in enumerate(TT):
                if level < 1:
                    break
                is_s = (ti == 4)
                with ExitStack() as TS:
                    qT = kb.sb(TS, "qT", [128, 8, 512], BF16)
                    qiT = kb.sb(TS, "qiT", [128, 2, 512], BF16)
                    WB = kb.sb(TS, "WB", [128, 16, 16], F32)
                    onT = kb.sb(TS, "onT", [128, 8, 512], BF16)
                    HS = ExitStack()
                    hT = kb.sb(HS, "hT", [128, 8, 512], BF16)
                    with ExitStack() as st:
                        sqb = kb.sb(st, "sqb", [128, 8, 512], BF16)
                        rsb = kb.sb(st, "rsb", [128, 512], F32)
                        rmsnorm_tile(hT, hT.t[:, :, 0:n], t0, n, 0, sqb, rsb)
                    with ExitStack() as st:
                        SQ = [kb.sb(st, "sq", [128, 512], BF16) for _ in range(2)]
                        QR = [kb.sb(st, "qraw", [128, 512], F32) for _ in range(2)]
                        RS = [kb.sb(st, "rs2", [128, 512], F32) for _ in range(2)]

                        def qs1(h):
                            pa = PS[h % 2]
                            sq, qraw = SQ[h % 2], QR[h % 2]
                            kb.mm(pa, pa.t[:, 0:n], [(Win.t[:, kc, h * 128:(h + 1) * 128], hT.t[:, kc, 0:n]) for kc in range(8)],
                                  [Win, hT])
                            kb.op(ACT, lambda: S.activation(out=sq.t[:, 0:n], in_=pa.t[:, 0:n], func=AF.Square),
                                  reads=[pa], writes=[sq])
                            kb.op(DVE, lambda: V.tensor_copy(out=qraw.t[:, 0:n], in_=pa.t[:, 0:n]), reads=[pa], writes=[qraw])

                        def qs2(h):
                            pb2 = PS[2 + h % 2]
                            sq, qraw, rs2 = SQ[h % 2], QR[h % 2], RS[h % 2]
                            kb.mm(pb2, pb2.t[:, 0:n], [(ones_h.t[:], sq.t[:, 0:n])], [ones_h, sq])
                            kb.op(ACT, lambda: S.activation(out=rs2.t[:, 0:n], in_=pb2.t[:, 0:n], func=AF.Sqrt,
                                                            bias=EPSB.t[:, 0:1], scale=1.0), reads=[pb2, EPSB], writes=[rs2])
                            kb.op(DVE, lambda: V.reciprocal(out=rs2.t[:, 0:n], in_=rs2.t[:, 0:n]), reads=[rs2], writes=[rs2])
                            kb.op(DVE, lambda: V.scalar_tensor_tensor(out=qT.t[:, h, 0:n], in0=qraw.t[:, 0:n],
                                                                      scalar=qk_g.t[:, 0:1], in1=rs2.t[:, 0:n],
                                                                      op0=ALU.mult, op1=ALU.mult),
                                  reads=[qraw, qk_g, rs2], writes=[qT])

                        qs1(0)
                        for h in range(8):
                            if h + 1 < 8:
                                qs1(h + 1)
                            qs2(h)
                        for j in range(3):
                            pa = PS[4 + j % 2]
                            c0 = 1536 + j * 128 if j < 2 else ATT_IN
                            kb.mm(pa, pa.t[:, 0:n], [(Win.t[:, kc, c0:c0 + 128], hT.t[:, kc, 0:n]) for kc in range(8)], [Win, hT])
                            if j < 2:
                                copy(evac_engine(), qiT.t[:, j, 0:n], pa.t[:, 0:n], [pa], [qiT])
                            else:
                                copy(evac_engine(), kiT.t[:, t0:t0 + n], pa.t[:, 0:n], [pa], [kiT])
                    if is_s and level >= 6:
                        with ExitStack() as st:
                            Wrep = kb.sb(st, "Wrep", [128, 8, 4, 128], BF16)
                            kb.op(DVE, lambda: V.tensor_copy(out=Wrep.t[:], in_=Win.t[:, :, 1856:1860].unsqueeze(3).to_broadcast([128, 8, 4, 128])),
                                  reads=[Win], writes=[Wrep])
                            pw = PS[6]
                            for h in range(4):
                                xs_ = (h % 2) * 2 + h // 2
                                kb.mm(pw, pw.t[:, xs_ * 64:(xs_ + 1) * 64], [(Wrep.t[:, kc, h, :], hT.t[:, kc, 0:NS]) for kc in range(8)], [Wrep, hT])
                            kb.op(DVE, lambda: V.tensor_copy(out=WB.t[:].rearrange("p b (x t) -> p b x t", x=4), in_=pw.t[:, 0:256].rearrange("p (x b t) -> p b x t", x=4, b=16)), reads=[pw], writes=[WB])
                    with ExitStack() as st:
                        ko = [kb.sb(st, "ko%d" % i, [128, 256], F32) for i in range(2)]
                        vo = [kb.sb(st, "vo%d" % i, [128, 256], F32) for i in range(2)]
                        io = [kb.sb(st, "io%d" % i, [128, 64], F32) for i in range(2)]
                        KBF = [kb.sb(st, "kbf", [128, 256], BF16) for _ in range(2)]
                        SSQ = [kb.sb(st, "ssq", [128, 2], F32) for _ in range(2)]
                        junk = kb.sb(st, "junk", [128, 128], F32)
                        nchunk = 4 if not is_s else 1
                        def ck1(cc):
                            kbf, ssq = KBF[cc % 2], SSQ[cc % 2]
                            cn = 128 if not is_s else NS
                            ci = ti * 4 + cc
                            cs = cc * 128
                            pa = PS[cc % 2]
                            pb2 = PS[2 + cc % 2]
                            kb.mm(pa, pa.t[0:cn, :], [(hT.t[:, kc, cs:cs + cn], Win.t[:, kc, 1024:1536]) for kc in range(8)], [Win, hT])
                            kb.mm(pb2, pb2.t[0:cn, 0:68], [(hT.t[:, kc, cs:cs + cn], Win.t[:, kc, 1792:1860]) for kc in range(8)], [Win, hT])
                            kob, vob, iob = ko[cc % 2], vo[cc % 2], io[cc % 2]
                            for g in range(2):
                                kb.op(ACT, lambda g=g: S.activation(out=junk.t[0:cn, :], in_=pa.t[0:cn, g * 128:(g + 1) * 128],
                                                                     func=AF.Square, accum_out=ssq.t[0:cn, g:g + 1]),
                                      reads=[pa], writes=[junk, ssq])
                            kb.op(ACT, lambda: S.activation(out=ssq.t[0:cn, :], in_=ssq.t[0:cn, :], func=AF.Sqrt,
                                                            bias=EPSB.t[0:cn, 0:1], scale=1.0 / 128), reads=[ssq, EPSB], writes=[ssq])
                            kb.op(DVE, lambda: V.reciprocal(out=ssq.t[0:cn, :], in_=ssq.t[0:cn, :]), reads=[ssq], writes=[ssq])
                            for g in range(2):
                                kb.op(DVE, lambda g=g: V.scalar_tensor_tensor(
                                    out=kob.t[0:cn, g * 128:(g + 1) * 128], in0=pa.t[0:cn, g * 128:(g + 1) * 128],
                                    scalar=ssq.t[0:cn, g:g + 1], in1=kgbc.t[0:cn, :], op0=ALU.mult, op1=ALU.mult),
                                    reads=[pa, ssq, kgbc], writes=[kob])
                            kb.op(ACT, lambda: S.copy(out=vob.t[0:cn, :], in_=pa.t[0:cn, 256:512]), reads=[pa], writes=[vob])
                            kb.op(ACT, lambda: S.copy(out=iob.t[0:cn, :], in_=pb2.t[0:cn, 0:64]), reads=[pb2], writes=[iob])
                            kb.op(ACT, lambda: S.activation(out=WIa.t[0:cn, ci, :], in_=pb2.t[0:cn, 64:68], func=AF.Abs),
                                  reads=[pb2], writes=[WIa])
                            kb.op(DVE, lambda: V.tensor_scalar(out=WIs.t[0:cn, ci, :], in0=pb2.t[0:cn, 64:68], scalar1=0.0,
                                                               scalar2=2.0, op0=ALU.is_ge, op1=ALU.mult), reads=[pb2], writes=[WIs])
                            kb.op(DVE, lambda: V.tensor_scalar(out=WIs.t[0:cn, ci, :], in0=WIs.t[0:cn, ci, :], scalar1=-1.0,
                                                               scalar2=None, op0=ALU.add), reads=[WIs], writes=[WIs])
                            kb.op(DVE, lambda: V.tensor_copy(out=kbf.t[0:cn, :], in_=kob.t[0:cn, :]), reads=[kob], writes=[kbf])
                            kb.op(ACT, lambda: S.copy(out=Vall.t[0:cn, ci, :], in_=vob.t[0:cn, :]), reads=[vob], writes=[Vall])
                            if not is_s:
                                r0 = ci * 128
                                kb.dma(SP, k_p[r0:r0 + 128, :], kob.t[:], reads=[kob])
                                kb.dma(SP, v_p[r0:r0 + 128, :], vob.t[:], reads=[vob])
                                kb.dma(SP, i_p[r0:r0 + 128, :], iob.t[:], reads=[iob])
                            else:
                                kb.dma(SP, k_s[:, :], kob.t[0:NS, :], reads=[kob])
                                kb.dma(SP, v_s[:, :], vob.t[0:NS, :], reads=[vob])
                                kb.dma(SP, i_s[:, :], iob.t[0:NS, :], reads=[iob])

                        def ck2(cc):
                            kbf = KBF[cc % 2]
                            cn = 128 if not is_s else NS
                            cs = cc * 128
                            pt = PS[4 + cc % 2]
                            ptb = pt.t[:].bitcast(BF16)
                            kb.transposes(pt, [(ptb[:, g * 128:g * 128 + cn], kbf.t[0:cn, g * 128:(g + 1) * 128]) for g in range(2)],
                                          [kbf], identb)
                            copy(evac_engine(), kT.t[:, :, t0 + cs:t0 + cs + cn],
                                 ptb[:, 0:256].rearrange("p (g t) -> p g t", g=2)[:, :, 0:cn], [pt], [kT])

                        ck1(0)
                        for cc in range(nchunk):
                            if cc + 1 < nchunk:
                                ck1(cc + 1)
                            ck2(cc)

                    HS.close()
                    if level < 2:
                        continue
                    if not is_s:
                        prompt_attention(nc, kb, PS, C, CST, ti, t0, qT, qiT, kT, Vall, kiT, WIa, WIs, biasN, identb, onesb, onT)
                    elif level < 6:
                        kb.op(DVE, lambda: V.memset(onT.t[:], 0.0), writes=[onT])
                    else:
                        sample_attention(nc, kb, PS, C, CST, qT, qiT, kT, Vall, kiT, WB, identb, onesb, onT,
                                         cache_k, cache_v, cache_i, ptab, relb, Win, biasS, biasNn)
                    for oc in range(8):
                        pb = PS[6 + oc % 2]
                        kb.mm(pb, pb.t[:, 0:n], [(Wo.t[:, kc, oc * 128:(oc + 1) * 128], onT.t[:, kc, 0:n]) for kc in range(8)], [Wo, onT])
                        add_resid(oc, t0, n, pb)

    if level >= 3:
        mlp(nc, kb, PS, xT, gn, EPSB, ones_d, 0, w_mlp_in, w_mlp_out, rmsnorm_tile)
    if level >= 4:
        retention(nc, kb, PS, (cstr, COR, cstr_np.shape[1]), None, xT, gn, identb, w_ret_in, w_ret_out, rot, st_in, r_p, r_s, rmsnorm_tile, level, EPSB)
    if level >= 5:
        mlp(nc, kb, PS, xT, gn, EPSB, ones_d, 1, w_mlp_in, w_mlp_out, rmsnorm_tile)

    with ExitStack() as st:
        ys = [kb.sb(st, "ys%d" % i, [128, D], F32) for i in range(2)]
        for c in range(17):
            n = 128 if c < 16 else NS
            yb = ys[c % 2]
            for g in range(2):
                pb = PS[(2 * c + g) % 8]
                eng = kb.PE
                kb._deps(eng, [xT, CST], [pb])
                inst = None
                for j in range(4):
                    inst = nc.tensor.transpose(pb.t[0:n, j * 128:(j + 1) * 128], xT.t[:, 4 * g + j, c * 128:c * 128 + n], identf.t[:, :])
                eng.cnt += 1
                inst.then_inc(eng.sem, 1)
                kb._commit((eng.key, eng.sem, eng.cnt), [xT, CST], [pb])
                copy(evac_engine(), yb.t[0:n, g * 512:(g + 1) * 512], pb.t[0:n, :], [pb], [yb])
            dst = y_p[c * 128:(c + 1) * 128, :] if c < 16 else y_s[:, :]
            kb.dma(SP, dst, yb.t[0:n, :], reads=[yb])
    kb.finish()
    kb.es.close()
    return nc, (cst_np, cstr_np)


def prompt_attention(nc, kb, PS, C, CST, ti, t0, qT, qiT, kT, Vall, kiT, WIa, WIs, biasN, identb, onesb, onT):
    V = nc.vector
    S = nc.scalar
    PE, ACT, DVE = kb.PE, kb.ACT, kb.DVE
    with ExitStack() as st:
        ACC = [kb.sb(st, "acc%d" % i, [128, 2048], F32) for i in range(2)]
        rt = [kb.sb(st, "rt%d" % i, [128, 512], F32) for i in range(2)]
        MASKB = [kb.sb(st, "maskb%d" % i, [128, 2048], BF16) for i in range(2)]
        MASKT = [kb.sb(st, "maskT%d" % i, [128, 16, 128], BF16) for i in range(2)]
        nearb = [kb.sb(st, "nearb%d" % i, [128, 4, 128], BF16) for i in range(2)]
        pT = [kb.sb(st, "pT%d" % i, [128, 512], BF16) for i in range(2)]
        rden = rt[0]
        LO = kb.sb(st, "b_lo", [128, 2], F32)
        MX = kb.sb(st, "b_mx", [128, 2], F32)
        MID = kb.sb(st, "b_mid", [128, 2], F32)
        CNT = kb.sb(st, "b_cnt", [128, 2], F32)
        GM = kb.sb(st, "b_gm", [128, 2], F32)
        TAU = kb.sb(st, "b_tau", [128, 2], F32)
        WH = kb.sb(st, "b_wh", [128, 2, NBIS], F32)
        HALF = kb.sb(st, "b_half", [128, 2], F32)
        WH2 = kb.sb(st, "b_wh2", [128, NBIS], F32)
        THR = kb.sb(st, "b_thr", [128, 1], F32)
        SA = kb.sb(st, "b_sa", [128, 1], F32)
        GA = kb.sb(st, "b_ga", [128, 1], F32)
        MIDA = [kb.sb(st, "b_mida%d" % i, [128, 1], F32) for i in range(2)]
        ONE = kb.sb(st, "b_one", [128, 2], F32)
        kb.op(DVE, lambda: V.memset(HALF.t[:], 0.5), writes=[HALF])
        kb.op(DVE, lambda: V.memset(ONE.t[:], 1.0), writes=[ONE])
        for pair in range(2):
            blocks = [ti * 4 + 2 * pair, ti * 4 + 2 * pair + 1]
            for k, b in enumerate(blocks):
                acc = ACC[k]
                q0 = (b % 4) * 128
                Sb = 128 * (b + 1)
                nch = (Sb + 511) // 512
                for sc in range(nch):
                    s0 = sc * 512
                    N = min(512, Sb - s0)
                    for h in range(4):
                        par, pr = h % 2, h // 2
                        pa = PS[(sc * 4 + h) % 2]
                        kb.mm(pa, pa.t[:, 0:N], [(qiT.t[par * 64:(par + 1) * 64, pr, q0:q0 + 128],
                                                  kiT.t[par * 64:(par + 1) * 64, s0:s0 + N])], [qiT, kiT])
                        r = rt[h % 2]
                        kb.op(ACT, lambda: S.activation(out=r.t[:, 0:N], in_=pa.t[:, 0:N], func=AF.Relu,
                                                        scale=WIa.t[:, b, h:h + 1]), reads=[pa, WIa], writes=[r])
                        if h == 0:
                            kb.op(DVE, lambda: V.tensor_scalar(out=acc.t[:, s0:s0 + N], in0=r.t[:, 0:N], scalar1=WIs.t[:, b, 0:1],
                                                               scalar2=None, op0=ALU.mult), reads=[r, WIs], writes=[acc])
                        else:
                            kb.op(DVE, lambda h=h: V.scalar_tensor_tensor(out=acc.t[:, s0:s0 + N], in0=r.t[:, 0:N],
                                                                         scalar=WIs.t[:, b, h:h + 1], in1=acc.t[:, s0:s0 + N],
                                                                         op0=ALU.mult, op1=ALU.add), reads=[r, WIs, acc], writes=[acc])
                kb.op(DVE, lambda: V.tensor_tensor(out=acc.t[:, Sb - 128:Sb], in0=acc.t[:, Sb - 128:Sb], in1=C("causneg"), op=ALU.add),
                      reads=[acc, CST], writes=[acc])
            if blocks[0] >= 2:
                for k, b in enumerate(blocks):
                    Sb = 128 * (b + 1)
                    kb.op(DVE, lambda: V.tensor_reduce(out=LO.t[:, k:k + 1], in_=ACC[k].t[:, 0:Sb - 128], axis=AX.X, op=ALU.min),
                          reads=[ACC[k]], writes=[LO])
                    kb.op(DVE, lambda: V.tensor_reduce(out=MX.t[:, k:k + 1], in_=ACC[k].t[:, 0:Sb], axis=AX.X, op=ALU.max),
                          reads=[ACC[k]], writes=[MX])
                kb.op(DVE, lambda: V.scalar_tensor_tensor(out=MX.t[:], in0=MX.t[:], scalar=1.0, in1=LO.t[:], op0=ALU.add, op1=ALU.subtract),
                      reads=[MX, LO], writes=[MX])
                for k in range(2):
                    kb.op(DVE, lambda: V.tensor_scalar(out=WH.t[:, k, :], in0=C("pow2"), scalar1=MX.t[:, k:k + 1], scalar2=None, op0=ALU.mult),
                          reads=[CST, MX], writes=[WH])
                kb.op(DVE, lambda: V.tensor_tensor(out=MID.t[:], in0=LO.t[:], in1=WH.t[:, :, 0], op=ALU.add), reads=[LO, WH], writes=[MID])
                Sb0 = 128 * (blocks[0] + 1)
                Sb1 = 128 * (blocks[1] + 1)
                kb.op(DVE, lambda: V.tensor_scalar(out=WH2.t[:], in0=WH.t[:, 1, :], scalar1=0.5, scalar2=None, op0=ALU.mult), reads=[WH], writes=[WH2])
                kb.op(DVE, lambda: V.memset(THR.t[:], float(Sb1) - 510.5), writes=[THR])
                kb.op(DVE, lambda: V.tensor_copy(out=MIDA[0].t[:], in_=MID.t[:, 1:2]), reads=[MID], writes=[MIDA[0]])
                for it in range(NBIS):
                    last = (it == NBIS - 1)
                    kb.op(DVE, lambda: V.tensor_scalar(out=MASKB[0].t[:, 0:Sb0], in0=ACC[0].t[:, 0:Sb0], scalar1=MID.t[:, 0:1], scalar2=0.0,
                                                       op0=ALU.is_ge, op1=ALU.add, accum_out=CNT.t[:, 0:1]),
                          reads=[ACC[0], MID], writes=[MASKB[0], CNT])
                    kb.op(DVE, lambda: V.scalar_tensor_tensor(out=GM.t[:, 0:1], in0=CNT.t[:, 0:1], scalar=255.5, in1=(ONE if last else HALF).t[:, 0:1],
                                                              op0=ALU.is_ge, op1=ALU.subtract), reads=[CNT, ONE, HALF], writes=[GM])
                    dst = TAU if last else MID
                    kb.op(DVE, lambda: V.scalar_tensor_tensor(out=dst.t[:, 0:1], in0=GM.t[:, 0:1], scalar=WH.t[:, 0, it:it + 1],
                                                              in1=MID.t[:, 0:1], op0=ALU.mult, op1=ALU.add),
                          reads=[GM, WH, MID], writes=[dst])
                    ma, mb_ = MIDA[it % 2], MIDA[(it + 1) % 2]
                    kb.op(ACT, lambda: S.activation(out=MASKB[1].t[:, 0:Sb1], in_=ACC[1].t[:, 0:Sb1], func=AF.Sign, scale=-1.0,
                                                    bias=ma.t[:, 0:1], accum_out=SA.t[:, 0:1]), reads=[ACC[1], ma], writes=[MASKB[1], SA])
                    kb.op(ACT, lambda: S.activation(out=GA.t[:, 0:1], in_=SA.t[:, 0:1], func=AF.Sign, scale=-1.0, bias=THR.t[:, 0:1]),
                          reads=[SA, THR], writes=[GA])
                    kb.op(ACT, lambda: S.activation(out=mb_.t[:, 0:1], in_=GA.t[:, 0:1], func=AF.Identity, scale=WH2.t[:, it:it + 1],
                                                    bias=ma.t[:, 0:1]), reads=[GA, WH2, ma], writes=[mb_])
                kb.op(DVE, lambda: V.tensor_tensor(out=TAU.t[:, 1:2], in0=MIDA[NBIS % 2].t[:, 0:1], in1=WH2.t[:, NBIS - 1:NBIS], op=ALU.subtract),
                      reads=[MIDA[NBIS % 2], WH2], writes=[TAU])
            else:
                kb.op(DVE, lambda: V.memset(TAU.t[:], -1.0e29), writes=[TAU])
            for k, b in enumerate(blocks):
                Sb = 128 * (b + 1)
                maskb, maskT = MASKB[k], MASKT[k]
                kb.op(DVE, lambda: V.tensor_scalar(out=maskb.t[:, 0:Sb], in0=ACC[k].t[:, 0:Sb], scalar1=TAU.t[:, k:k + 1], scalar2=NEG,
                                                   op0=ALU.is_lt, op1=ALU.mult), reads=[ACC[k], TAU], writes=[maskb])
                for j0 in range(0, b + 1, 8):
                    j1 = min(b + 1, j0 + 8)
                    pt = PS[2 + (j0 // 8) % 2]
                    ptb = pt.t[:].bitcast(BF16)
                    kb.transposes(pt, [(ptb[:, (j - j0) * 128:(j - j0 + 1) * 128], maskb.t[:, j * 128:(j + 1) * 128]) for j in range(j0, j1)],
                                  [maskb], identb)
                    src = ptb[:, 0:(j1 - j0) * 128].rearrange("p (j t) -> p j t", t=128)
                    if (j0 // 8) % 2 == 0:
                        kb.op(ACT, lambda: S.copy(out=maskT.t[:, j0:j1, :], in_=src), reads=[pt], writes=[maskT])
                    else:
                        kb.op(DVE, lambda: V.tensor_copy(out=maskT.t[:, j0:j1, :], in_=src), reads=[pt], writes=[maskT])
            for k, b in enumerate(blocks):
                maskT = MASKT[k]
                q0 = (b % 4) * 128
                for g in range(2):
                    po = PS[4 + g]
                    pd = PS[6 + g]
                    def logits(j):
                        pl = PS[j % 2]
                        near = (j >= b - 1)
                        if near:
                            nb_ = nearb[j % 2]
                            kb.op(DVE, lambda: V.tensor_tensor(out=nb_.t[:], in0=biasN.t[:, b - j, 4 * g:4 * g + 4, :],
                                                               in1=maskT.t[:, j, :].unsqueeze(1).to_broadcast([128, 4, 128]), op=ALU.add),
                                  reads=[biasN, maskT], writes=[nb_])
                            rhs2 = nb_.t[:]
                            rd2 = [nb_]
                        else:
                            rhs2 = maskT.t[:, j, :].unsqueeze(1).to_broadcast([128, 4, 128])
                            rd2 = [maskT]
                        kb.mm(pl, pl.t[:].rearrange("p (r t) -> p r t", r=4),
                              [(kT.t[:, g, j * 128:(j + 1) * 128], qT.t[:, 4 * g:4 * g + 4, q0:q0 + 128]), (identb.t[:], rhs2)],
                              [kT, qT, identb] + rd2)

                    logits(0)
                    for j in range(b + 1):
                        if j + 1 <= b:
                            logits(j + 1)
                        pl = PS[j % 2]
                        pp = pT[j % 2]
                        kb.op(ACT, lambda: S.activation(out=pp.t[:], in_=pl.t[:], func=AF.Exp), reads=[pl], writes=[pp])
                        kb.mm(po, po.t[:], [(Vall.t[:, j, g * 128:(g + 1) * 128], pp.t[:])], [Vall, pp], start=(j == 0), stop=(j == b))
                        kb.mm(pd, pd.t[:], [(onesb.t[:], pp.t[:])], [onesb, pp], start=(j == 0), stop=(j == b))
                    kb.op(DVE, lambda: V.reciprocal(out=rden.t[:], in_=pd.t[:]), reads=[pd], writes=[rden])
                    kb.op(DVE, lambda: V.tensor_tensor(out=onT.t[:, 4 * g:4 * g + 4, q0:q0 + 128],
                                                       in0=po.t[:].rearrange("p (r t) -> p r t", r=4),
                                                       in1=rden.t[:].rearrange("p (r t) -> p r t", r=4), op=ALU.mult),
                          reads=[po, rden], writes=[onT])


def mm_multi(kb, nc, out_buf, items, reads):
    eng = kb.PE
    kb._deps(eng, reads, [out_buf])
    inst = None
    for (o, l, r) in items:
        inst = nc.tensor.matmul(o, lhsT=l, rhs=r, start=True, stop=True)
    eng.cnt += 1
    inst.then_inc(eng.sem, 1)
    kb._commit((eng.key, eng.sem, eng.cnt), reads, [out_buf])


def sample_attention(nc, kb, PS, C, CST, qT, qiT, kT, Vall, kiT, WB, identb, onesb, onT, cache_k, cache_v, cache_i, ptab, relb, Win, biasS, biasNn):
    V = nc.vector
    S = nc.scalar
    ACT, DVE, POOL, SP = kb.ACT, kb.DVE, kb.POOL, kb.SP
    IOA = bass.IndirectOffsetOnAxis
    with ExitStack() as st:
        IDX = kb.sb(st, "IDX", [128, 32], I32)
        ISa = kb.sb(st, "ISa", [128, 16, 16, 4], F32)
        ISn = kb.sb(st, "ISn", [128, 16, 4], F32)
        LO = kb.sb(st, "LO", [128, 64], F32)
        with ExitStack() as s2:
            PT = kb.sb(s2, "PT", [128, 256], I32)
            PTf = kb.sb(s2, "PTf", [128, 16, 16], F32)
            PG = kb.sb(s2, "PG", [128, 32], F32)
            kb.dma(SP, PT.t[:], ptab.partition_broadcast(128), writes=[PT])
            kb.op(DVE, lambda: V.tensor_copy(out=PTf.t[:], in_=PT.t[:].rearrange("p (b j) -> p b j", j=16)), reads=[PT], writes=[PTf])
            kb.op(DVE, lambda: V.tensor_tensor(out=PTf.t[:], in0=PTf.t[:], in1=C("pagesel").unsqueeze(1).to_broadcast([128, 16, 16]),
                                               op=ALU.mult), reads=[PTf, CST], writes=[PTf])
            kb.op(DVE, lambda: V.tensor_reduce(out=PG.t[:, 0:16], in_=PTf.t[:], axis=AX.X, op=ALU.add), reads=[PTf], writes=[PG])
            kb.op(DVE, lambda: V.tensor_scalar(out=PG.t[:, 0:16], in0=PG.t[:, 0:16], scalar1=128.0, scalar2=C("pofs")[:, 0:1],
                                               op0=ALU.mult, op1=ALU.add), reads=[PG, CST], writes=[PG])
            kb.op(DVE, lambda: V.tensor_scalar(out=PG.t[:, 16:32], in0=PG.t[:, 0:16], scalar1=8.0, scalar2=None, op0=ALU.add),
                  reads=[PG], writes=[PG])
            kb.op(DVE, lambda: V.tensor_copy(out=IDX.t[:], in_=PG.t[:]), reads=[PG], writes=[IDX])

        with ExitStack() as s2:
            IG = [kb.sb(s2, "IG", [128, 16, 64], F32) for _ in range(2)]
            IB2 = kb.sb(s2, "IB2", [128, 16, 128], BF16)
            ITK = kb.sb(s2, "ITK", [128, 16, 128], BF16)
            tmp = kb.sb(s2, "tmpI", [128, 16, 4, 4], F32)
            for b in range(NSEQ):
                ig = IG[b % 2]
                kb.dma(POOL, ig.t[:].rearrange("p c d -> p (c d)"), cache_i, reads=[IDX], writes=[ig], indirect=IOA(ap=IDX.t[:, b:b + 1], axis=0))
                kb.op(ACT, lambda: S.copy(out=IB2.t[:, :, 0:64], in_=ig.t[:]), reads=[ig], writes=[IB2])
                kb.op(DVE, lambda: V.tensor_copy(out=IB2.t[:, :, 64:128], in_=ig.t[:]), reads=[ig], writes=[IB2])
                for half in range(2):
                    pt = PS[half]
                    ptb = pt.t[:].bitcast(BF16)
                    kb.transposes(pt, [(ptb[:, j * 128:(j + 1) * 128], IB2.t[:, half * 8 + j, :]) for j in range(8)], [IB2], identb)
                    copy_any(kb, nc, half, ITK.t[:, half * 8:half * 8 + 8, :], ptb[:, :].rearrange("p (j t) -> p j t", t=128), [pt], [ITK])
                pIs = [PS[2], PS[7]]
                wbb = WB.t[:, b, :]
                for par in range(2):
                    pI = pIs[par]
                    items = []
                    for c in range(16):
                        items.append((pI.t[:, c * 8:c * 8 + 8], ITK.t[par * 64:(par + 1) * 64, c, :],
                                      qiT.t[par * 64:(par + 1) * 64, :, 4 * b:4 * b + 4]))
                    items.append((pI.t[0:NS, 128:136], kiT.t[par * 64:(par + 1) * 64, SEQ:SEQ + NS],
                                  qiT.t[par * 64:(par + 1) * 64, :, 4 * b:4 * b + 4]))
                    mm_multi(kb, nc, pI, items, [ITK, qiT, kiT])
                for par in range(2):
                    pI = pIs[par]
                    kb.op(DVE, lambda: V.scalar_tensor_tensor(out=tmp.t[:, :, 2 * par:2 * par + 2, :].rearrange("p c h t -> p c (h t)"),
                                                              in0=pI.t[:, 0:128].rearrange("p (c x) -> p c x", c=16), scalar=0.0,
                                                              in1=wbb[:, 8 * par:8 * par + 8].unsqueeze(1).to_broadcast([128, 16, 8]),
                                                              op0=ALU.max, op1=ALU.mult), reads=[pI, WB], writes=[tmp])
                kb.op(DVE, lambda: V.tensor_reduce(out=ISa.t[:, b, :, :], in_=tmp.t[:].rearrange("p c h t -> p c t h"), axis=AX.X, op=ALU.add),
                      reads=[tmp], writes=[ISa])
                if kb.DBG and b == 14:
                    kb.dma(SP, kb.DBG["G"], ig.t[:].rearrange("p c d -> p (c d)"), reads=[ig])
                    kb.dma(SP, kb.DBG["B"], IB2.t[:].rearrange("p c d -> p (c d)"), reads=[IB2])
                    kb.dma(SP, kb.DBG["T"], ITK.t[:].rearrange("p c d -> p (c d)"), reads=[ITK])
                    kb.dma(SP, kb.DBG["M"], tmp.t[:].rearrange("p c h t -> p (c h t)"), reads=[tmp])
                for par in range(2):
                    pI = pIs[par]
                    kb.op(DVE, lambda: V.scalar_tensor_tensor(out=tmp.t[0:NS, 0, 2 * par:2 * par + 2, :].rearrange("p h t -> p (h t)"),
                                                              in0=pI.t[0:NS, 128:136], scalar=0.0, in1=wbb[0:NS, 8 * par:8 * par + 8],
                                                              op0=ALU.max, op1=ALU.mult), reads=[pI, WB], writes=[tmp])
                kb.op(DVE, lambda: V.tensor_reduce(out=ISn.t[0:NS, b, :], in_=tmp.t[0:NS, 0, :, :].rearrange("p h t -> p t h"), axis=AX.X, op=ALU.add),
                      reads=[tmp], writes=[ISn])
            kb.op(DVE, lambda: V.tensor_tensor(out=ISn.t[0:NS, :, :], in0=ISn.t[0:NS, :, :],
                                               in1=C("newvalid")[0:NS, :].rearrange("p (b t) -> p b t", t=4), op=ALU.add),
                  reads=[ISn, CST], writes=[ISn])

        with ExitStack() as s2:
            Wd = kb.sb(s2, "Wd", [128, 64], F32)
            WH = kb.sb(s2, "WH", [128, 64], F32)
            MID = kb.sb(s2, "MID", [128, 64], F32)
            GE = kb.sb(s2, "GE", [128, 64], F32)
            CMP = kb.sb(s2, "CMP", [128, 16, 16, 4], BF16)
            CNP = kb.sb(s2, "CNP", [128, 64], F32)
            CMN = kb.sb(s2, "CMN", [128, 64], F32)
            MX = kb.sb(s2, "MX", [128, 128], F32)
            DG = kb.sb(s2, "DG", [128, 128], F32)
            onesf = kb.sb(s2, "onesf", [128, 128], F32)
            kb.op(DVE, lambda: V.memset(onesf.t[:], 1.0), writes=[onesf])
            kb.op(DVE, lambda: V.tensor_reduce(out=MX.t[:, 0:64].rearrange("p (b t) -> p b t", t=4), in_=ISa.t[:].rearrange("p b c t -> p b t c"),
                                               axis=AX.X, op=ALU.max), reads=[ISa], writes=[MX])
            kb.op(DVE, lambda: V.tensor_reduce(out=MX.t[:, 64:128].rearrange("p (b t) -> p b t", t=4), in_=ISa.t[:].rearrange("p b c t -> p b t c"),
                                               axis=AX.X, op=ALU.min), reads=[ISa], writes=[MX])
            kb.op(DVE, lambda: V.tensor_tensor(out=MX.t[0:NS, 0:64], in0=MX.t[0:NS, 0:64], in1=ISn.t[0:NS, :, :].rearrange("p b t -> p (b t)"),
                                               op=ALU.max), reads=[MX, ISn], writes=[MX])
            pm = PS[3]
            eng = kb.PE
            kb._deps(eng, [MX, CST], [pm])
            ins_ = nc.tensor.transpose(pm.t[:, 0:128], MX.t[:, :], CST.t[:, 0:128])
            eng.cnt += 1
            ins_.then_inc(eng.sem, 1)
            kb._commit((eng.key, eng.sem, eng.cnt), [MX, CST], [pm])
            kb.op(DVE, lambda: V.tensor_reduce(out=GE.t[0:64, 0:1], in_=pm.t[0:64, 0:128], axis=AX.X, op=ALU.max), reads=[pm], writes=[GE])
            kb.op(DVE, lambda: V.tensor_reduce(out=GE.t[64:128, 0:1], in_=pm.t[64:128, 0:128], axis=AX.X, op=ALU.min), reads=[pm], writes=[GE])
            kb.op(DVE, lambda: V.tensor_scalar(out=DG.t[:], in0=CST.t[:, 0:128], scalar1=GE.t[:, 0:1], scalar2=None, op0=ALU.mult),
                  reads=[CST, GE], writes=[DG])
            pq = PS[4]
            kb.mm(pq, pq.t[:, 0:128], [(onesf.t[:], DG.t[:])], [onesf, DG])
            kb.op(DVE, lambda: V.tensor_copy(out=LO.t[:], in_=pq.t[:, 64:128]), reads=[pq], writes=[LO])
            kb.op(DVE, lambda: V.scalar_tensor_tensor(out=Wd.t[:], in0=pq.t[:, 0:64], scalar=1.0, in1=LO.t[:], op0=ALU.add, op1=ALU.subtract),
                  reads=[pq, LO], writes=[Wd])
            pc = PS[5]
            for it in range(NBIS):
                kb.op(DVE, lambda: V.tensor_scalar(out=WH.t[:], in0=Wd.t[:], scalar1=0.5, scalar2=None, op0=ALU.mult), reads=[Wd], writes=[WH])
                kb.op(DVE, lambda: V.tensor_tensor(out=MID.t[:], in0=LO.t[:], in1=WH.t[:], op=ALU.add), reads=[LO, WH], writes=[MID])
                kb.op(DVE, lambda: V.tensor_tensor(out=CMP.t[:], in0=ISa.t[:],
                                                   in1=MID.t[:].rearrange("p (b t) -> p b t", t=4).unsqueeze(2).to_broadcast([128, 16, 16, 4]),
                                                   op=ALU.is_ge), reads=[ISa, MID], writes=[CMP])
                kb.op(DVE, lambda: V.tensor_reduce(out=CNP.t[:].rearrange("p (b t) -> p b t", t=4), in_=CMP.t[:].rearrange("p b c t -> p b t c"),
                                                   axis=AX.X, op=ALU.add), reads=[CMP], writes=[CNP])
                kb.op(DVE, lambda: V.tensor_tensor(out=CMN.t[0:NS, :], in0=ISn.t[0:NS, :, :].rearrange("p b t -> p (b t)"), in1=MID.t[0:NS, :],
                                                   op=ALU.is_ge), reads=[ISn, MID], writes=[CMN])
                kb.mm(pc, pc.t[:, 0:64], [(onesf.t[:], CNP.t[:]), (onesf.t[0:NS, :], CMN.t[0:NS, :])], [onesf, CNP, CMN])
                kb.op(DVE, lambda: V.tensor_scalar(out=GE.t[:, 0:64], in0=pc.t[:, 0:64], scalar1=255.5, scalar2=None, op0=ALU.is_ge), reads=[pc], writes=[GE])
                kb.op(DVE, lambda: V.tensor_tensor(out=GE.t[:, 0:64], in0=GE.t[:, 0:64], in1=WH.t[:], op=ALU.mult), reads=[GE, WH], writes=[GE])
                kb.op(DVE, lambda: V.tensor_tensor(out=LO.t[:], in0=LO.t[:], in1=GE.t[:, 0:64], op=ALU.add), reads=[LO, GE], writes=[LO])
                kb.op(DVE, lambda: V.tensor_copy(out=Wd.t[:], in_=WH.t[:]), reads=[WH], writes=[Wd])

        with ExitStack() as s2:
            MB = kb.sb(s2, "MB", [128, 16, 16, 4], F32)
            MBN = kb.sb(s2, "MBN", [128, 16, 4], F32)
            kb.op(DVE, lambda: V.tensor_tensor(out=MB.t[:], in0=ISa.t[:],
                                               in1=LO.t[:].rearrange("p (b t) -> p b t", t=4).unsqueeze(2).to_broadcast([128, 16, 16, 4]),
                                               op=ALU.is_lt), reads=[ISa, LO], writes=[MB])
            kb.op(DVE, lambda: V.tensor_scalar(out=MB.t[:], in0=MB.t[:], scalar1=NEG, scalar2=None, op0=ALU.mult), reads=[MB], writes=[MB])
            kb.op(DVE, lambda: V.tensor_tensor(out=MBN.t[0:NS], in0=ISn.t[0:NS], in1=LO.t[0:NS, :].rearrange("p (b t) -> p b t", t=4), op=ALU.is_lt),
                  reads=[ISn, LO], writes=[MBN])
            kb.op(DVE, lambda: V.tensor_scalar(out=MBN.t[0:NS], in0=MBN.t[0:NS], scalar1=NEG, scalar2=None, op0=ALU.mult), reads=[MBN], writes=[MBN])
            KGS = [kb.sb(s2, "KG", [128, 8, 256], F32) for _ in range(2)]
            kgi = [0]
            wfl = Win.t[:].rearrange("p a b -> p (a b)")
            wf = {}
            if Win.lw:
                wf[Win.lw[0]] = (Win.lw[1], Win.lw[2])
            for k_, v_ in Win.rd.items():
                if wf.get(k_, (None, 0))[1] < v_[1]:
                    wf[k_] = v_
            KB_ = Buf(wfl[:, 0:4096].rearrange("p (c d) -> p c d", d=256), "KBb", wf)
            VB_ = Buf(wfl[:, 4096:8192].rearrange("p (c d) -> p c d", d=256), "VBb", wf)
            KT = Buf(wfl[:, 8192:12288].rearrange("p (c g t) -> p c g t", g=2, t=128), "KTs", wf)
            LG = kb.sb(s2, "LG", [128, 16, 8, 4], F32)
            MBI = kb.sb(s2, "MBI", [128, 16, 8, 4], F32)
            LGN = kb.sb(s2, "LGN", [128, 8, 4], F32)
            PT_ = kb.sb(s2, "PTs", [128, 16, 8, 4], BF16)
            PTN = kb.sb(s2, "PTN", [128, 8, 4], BF16)
            DEN = kb.sb(s2, "DEN", [128, 8, 4], F32)
            for b in range(NSEQ):
                for (src, dstb, e) in ((cache_k, KB_, 0), (cache_v, VB_, 1)):
                    for half in range(2):
                        KG = KGS[kgi[0] % 2]
                        kgi[0] += 1
                        kb.dma(POOL, KG.t[:].rearrange("p c d -> p (c d)"), src, reads=[IDX], writes=[KG], indirect=IOA(ap=IDX.t[:, half * 16 + b:half * 16 + b + 1], axis=0))
                        copy_any(kb, nc, (half + e) % 2, dstb.t[:, half * 8:half * 8 + 8, :], KG.t[:], [KG], [dstb])
                for q4 in range(4):
                    pt = PS[q4 % 2]
                    ptb = pt.t[:].bitcast(BF16)
                    kb.transposes(pt, [(ptb[:, (2 * j + g) * 128:(2 * j + g + 1) * 128], KB_.t[:, q4 * 4 + j, g * 128:(g + 1) * 128])
                                       for j in range(4) for g in range(2)], [KB_], identb)
                    copy_any(kb, nc, q4 % 2, KT.t[:, q4 * 4:q4 * 4 + 4, :, :], ptb[:, :].rearrange("p (j g t) -> p j g t", j=4, g=2), [pt], [KT])
                pL = PS[2]
                pLv = pL.t[:].rearrange("p (c h t) -> p c h t", c=16, h=8)
                items = []
                for c in range(16):
                    for g in range(2):
                        items.append((pL.t[:, (c * 8 + 4 * g) * 4:(c * 8 + 4 * g) * 4 + 16], KT.t[:, c, g, :], qT.t[:, 4 * g:4 * g + 4, 4 * b:4 * b + 4]))
                mm_multi(kb, nc, pL, items, [KT, qT])
                pN = PS[3]
                pNv = pN.t[0:NS, 0:32].rearrange("p (h t) -> p h t", h=8)
                mm_multi(kb, nc, pN, [(pN.t[0:NS, g * 16:(g + 1) * 16], kT.t[:, g, SEQ:SEQ + NS], qT.t[:, 4 * g:4 * g + 4, 4 * b:4 * b + 4]) for g in range(2)],
                         [kT, qT])
                kb.op(DVE, lambda: V.tensor_tensor(out=MBI.t[:], in0=biasS.t[:], in1=MB.t[:, b, :, :].unsqueeze(2).to_broadcast([128, 16, 8, 4]),
                                                   op=ALU.add), reads=[biasS, MB], writes=[MBI])
                kb.op(DVE, lambda: V.tensor_tensor(out=LG.t[:], in0=pLv, in1=MBI.t[:], op=ALU.add), reads=[pL, MBI], writes=[LG])
                kb.op(ACT, lambda: S.activation(out=PT_.t[:], in_=LG.t[:], func=AF.Exp), reads=[LG], writes=[PT_])
                kb.op(DVE, lambda: V.tensor_tensor(out=LGN.t[0:NS], in0=biasNn.t[0:NS, :, 4 * b:4 * b + 4],
                                                   in1=MBN.t[0:NS, b, :].unsqueeze(1).to_broadcast([NS, 8, 4]), op=ALU.add),
                      reads=[biasNn, MBN], writes=[LGN])
                kb.op(DVE, lambda: V.tensor_tensor(out=LGN.t[0:NS], in0=pNv, in1=LGN.t[0:NS], op=ALU.add), reads=[pN, LGN], writes=[LGN])
                kb.op(ACT, lambda: S.activation(out=PTN.t[0:NS], in_=LGN.t[0:NS], func=AF.Exp), reads=[LGN], writes=[PTN])
                pD = PS[4]
                kb.mm(pD, pD.t[:], [(onesb.t[:], PT_.t[:].rearrange("p c h t -> p (c h t)"))], [onesb, PT_])
                pDn = PS[5]
                kb.mm(pDn, pDn.t[:, 0:32], [(onesb.t[0:NS, :], PTN.t[0:NS].rearrange("p h t -> p (h t)"))], [onesb, PTN])
                kb.op(DVE, lambda: V.tensor_reduce(out=DEN.t[:], in_=pD.t[:].rearrange("p (c h t) -> p h t c", c=16, h=8), axis=AX.X, op=ALU.add),
                      reads=[pD], writes=[DEN])
                kb.op(DVE, lambda: V.tensor_tensor(out=DEN.t[:], in0=DEN.t[:], in1=pDn.t[:, 0:32].rearrange("p (h t) -> p h t", h=8), op=ALU.add),
                      reads=[DEN, pDn], writes=[DEN])
                kb.op(DVE, lambda: V.reciprocal(out=DEN.t[:], in_=DEN.t[:]), reads=[DEN], writes=[DEN])
                pO = PS[6]
                pOv = pO.t[:, 0:32].rearrange("p (h t) -> p h t", h=8)
                for g in range(2):
                    prs = [(VB_.t[:, c, g * 128:(g + 1) * 128], PT_.t[:, c, 4 * g:4 * g + 4, :]) for c in range(16)]
                    prs.append((Vall.t[0:NS, 16, g * 128:(g + 1) * 128], PTN.t[0:NS, 4 * g:4 * g + 4, :]))
                    kb.mm(pO, pO.t[:, g * 16:(g + 1) * 16], prs, [VB_, PT_, Vall, PTN])
                kb.op(DVE, lambda: V.tensor_tensor(out=onT.t[:, :, 4 * b:4 * b + 4], in0=pOv, in1=DEN.t[:], op=ALU.mult),
                      reads=[pO, DEN], writes=[onT])
            if kb.DBG:
                kb.dma(SP, kb.DBG["I"], IDX.t[:], reads=[IDX])
                kb.dma(SP, kb.DBG["A"], ISa.t[:].rearrange("p b c t -> p (b c t)"), reads=[ISa])
                kb.dma(SP, kb.DBG["L"], LO.t[:], reads=[LO])
                kb.dma(SP, kb.DBG["N"][0:NS, :], ISn.t[0:NS].rearrange("p b t -> p (b t)"), reads=[ISn])
                kb.dma(SP, kb.DBG["D"], DEN.t[:].rearrange("p h t -> p (h t)"), reads=[DEN])
                kb.dma(SP, kb.DBG["W"], WB.t[:].rearrange("p b x -> p (b x)"), reads=[WB])


def copy_any(kb, nc, which, out_ap, in_ap, reads, writes):
    if which == 0:
        kb.op(kb.ACT, lambda: nc.scalar.copy(out=out_ap, in_=in_ap), reads=reads, writes=writes)
    else:
        kb.op(kb.DVE, lambda: nc.vector.tensor_copy(out=out_ap, in_=in_ap), reads=reads, writes=writes)


def mlp(nc, kb, PS, xT, gn, EPSB, ones_d, l, w_in, w_out, rmsnorm_tile):
    V = nc.vector
    S = nc.scalar
    ACT, DVE, POOL = kb.ACT, kb.DVE, kb.POOL
    with ExitStack() as st:
        hT = kb.sb(st, "hTm", [128, 8, T], BF16)
        W1 = [kb.sb(st, "W1", [128, 8, 512], BF16) for _ in range(3)]
        W2 = [kb.sb(st, "W2", [128, 4, 1024], BF16) for _ in range(3)]
        U = [kb.sb(st, "U", [128, 4, T], BF16) for _ in range(2)]
        R = [kb.sb(st, "R", [128, 512], BF16) for _ in range(2)]
        w1src = w_in[l].rearrange("(kc p) n -> p kc n", p=128)
        cnt = [0, 0]

        def load(fg):
            kb.dma(POOL, W1[fg % 3].t[:], w1src[:, :, fg * 512:(fg + 1) * 512], writes=[W1[fg % 3]])
            kb.dma(POOL, W2[fg % 3].t[:], w_out[l][fg * 512:(fg + 1) * 512, :].rearrange("(c p) n -> p c n", p=128),
                   writes=[W2[fg % 3]])

        load(0)
        load(1)
        with ExitStack() as s2:
            sqb = kb.sb(s2, "sqbm", [128, 8, 512], BF16)
            rsb = kb.sb(s2, "rsbm", [128, 512], F32)
            for (t0, n) in TT:
                rmsnorm_tile(hT, hT.t[:, :, t0:t0 + n], t0, n, 8 + 16 * l, sqb, rsb)

        def phase1(fg):
            w1, ub = W1[fg % 3], U[fg % 2]
            for ffc in range(4):
                for (t0, n) in TT:
                    pb = PS[cnt[0] % 4]
                    rb = R[cnt[0] % 2]
                    cnt[0] += 1
                    kb.mm(pb, pb.t[:, 0:n], [(w1.t[:, kc, ffc * 128:(ffc + 1) * 128], hT.t[:, kc, t0:t0 + n]) for kc in range(8)],
                          [w1, hT])
                    kb.op(ACT, lambda: S.activation(out=rb.t[:, 0:n], in_=pb.t[:, 0:n], func=AF.Relu), reads=[pb], writes=[rb])
                    kb.op(DVE, lambda: V.tensor_tensor(out=ub.t[:, ffc, t0:t0 + n], in0=rb.t[:, 0:n], in1=rb.t[:, 0:n], op=ALU.mult),
                          reads=[rb], writes=[ub])

        def phase2(fg):
            w2, ub = W2[fg % 3], U[fg % 2]
            for oc in range(8):
                for (t0, n) in TT:
                    pb = PS[4 + cnt[1] % 4]
                    cnt[1] += 1
                    kb.mm(pb, pb.t[:, 0:n], [(w2.t[:, ffc, oc * 128:(oc + 1) * 128], ub.t[:, ffc, t0:t0 + n]) for ffc in range(4)],
                          [w2, ub])
                    kb.op(DVE, lambda: V.tensor_tensor(out=xT.t[:, oc, t0:t0 + n], in0=xT.t[:, oc, t0:t0 + n], in1=pb.t[:, 0:n],
                                                       op=ALU.add), reads=[xT, pb], writes=[xT])

        phase1(0)
        for fg in range(8):
            if fg + 2 < 8:
                load(fg + 2)
            if fg + 1 < 8:
                phase1(fg + 1)
            phase2(fg)


def retention(nc, kb, PS, C, CST, xT, gn, identb, w_in, w_out, rot, st_in, r_p, r_s, rmsnorm_tile, level, EPSB_):
    V = nc.vector
    S = nc.scalar
    ACT, DVE, POOL, SP = kb.ACT, kb.DVE, kb.POOL, kb.SP
    with ExitStack() as st:
        cstr_ap, COR, ncr = C
        CST = kb.sb(st, "cstr", [128, ncr], F32)
        kb.dma(SP, CST.t[:], cstr_ap, writes=[CST])

        def C(name):
            o, w = COR[name]
            return CST.t[:, o:o + w]
        hT = kb.sb(st, "hTr", [128, 8, T], BF16)
        with ExitStack() as s2:
            sqb = kb.sb(s2, "sqbr", [128, 8, 512], BF16)
            rsb = kb.sb(s2, "rsbr", [128, 512], F32)
            for (t0, n) in TT:
                rmsnorm_tile(hT, hT.t[:, :, t0:t0 + n], t0, n, 16, sqb, rsb)
        Sf = kb.sb(st, "Sf", [128, 2, 512], F32)
        Sb = kb.sb(st, "Sb", [128, 2, 512], BF16)
        Wqk = [kb.sb(st, "Wqk", [128, 8, 512], BF16) for _ in range(1)]
        Wv = kb.sb(st, "Wv", [128, 8, 512], BF16)
        Wg = kb.sb(st, "Wg", [128, 8, 512], BF16)
        Wo = kb.sb(st, "Wor", [128, 4, 1024], BF16)
        RT = [kb.sb(st, "rt", [128, 2, 512], F32) for _ in range(2)]
        QK = [kb.sb(st, "qk", [128, 4, 512], BF16) for _ in range(2)]
        OGT = [kb.sb(st, "ogT", [128, 4, 512], BF16) for _ in range(2)]
        t1 = kb.sb(st, "t1", [128, 512], F32)
        t2 = kb.sb(st, "t2", [128, 512], F32)
        VB = [kb.sb(st, "vb", [128, 512], BF16) for _ in range(2)]
        GT = [kb.sb(st, "gt", [128, 512], BF16) for _ in range(2)]
        ATM = [kb.sb(st, "atm", [128, 128], BF16) for _ in range(2)]
        QD = [kb.sb(st, "qd", [128, 2, 128], BF16) for _ in range(2)]
        OG = [kb.sb(st, "og", [128, 512], BF16) for _ in range(2)]
        KD = [kb.sb(st, "kd", [128, 256], BF16) for _ in range(2)]
        SS = kb.sb(st, "ss", [128, 2], F32)
        junk = kb.sb(st, "junkr", [128, 512], BF16)
        SST = [kb.sb(st, "sst", [128, 2, 512], F32) for _ in range(2)]
        S0B = [kb.sb(st, "s0b", [128, 2, 512], BF16) for _ in range(2)]
        QP = [kb.sb(st, "qp", [128, 2, 64], BF16) for _ in range(2)]
        KDB = [kb.sb(st, "kdb", [128, 256], BF16) for _ in range(2)]
        wsrc = w_in.rearrange("(kc p) n -> p kc n", p=128)
        gidx = [0]
        for h in range(4):
            wqk = Wqk[0]
            kb.dma(POOL, wqk.t[:, :, 0:256], wsrc[:, :, h * 256:(h + 1) * 256], writes=[wqk])
            kb.dma(POOL, wqk.t[:, :, 256:512], wsrc[:, :, 1024 + h * 256:1024 + (h + 1) * 256], writes=[wqk])
            kb.dma(POOL, Wv.t[:], wsrc[:, :, 2048 + h * 512:2048 + (h + 1) * 512], writes=[Wv])
            kb.dma(POOL, Wg.t[:], wsrc[:, :, 4096 + h * 512:4096 + (h + 1) * 512], writes=[Wg])
            kb.dma(POOL, Wo.t[:], w_out[h * 512:(h + 1) * 512, :].rearrange("(c p) n -> p c n", p=128), writes=[Wo])
            dmh = C("dm")[:, h * 128:(h + 1) * 128]
            qdh = C("qd")[:, h * 128:(h + 1) * 128]
            cdec = float(np.exp(np.log1p(-np.exp2(-5.0 - h)) * 128.0))
            cdec4 = float(np.exp(np.log1p(-np.exp2(-5.0 - h)) * 4.0))

            def gn_gate(po, rows, gt, og):
                kb.op(ACT, lambda: S.activation(out=junk.t[0:rows, :], in_=po.t[0:rows, :], func=AF.Square,
                                                accum_out=SS.t[0:rows, 0:1]), reads=[po], writes=[junk, SS])
                kb.op(ACT, lambda: S.activation(out=SS.t[0:rows, 0:1], in_=SS.t[0:rows, 0:1], func=AF.Sqrt,
                                                bias=EPSB_.t[0:rows, 0:1], scale=1.0 / 512), reads=[SS, EPSB_], writes=[SS])
                kb.op(DVE, lambda: V.reciprocal(out=SS.t[0:rows, 0:1], in_=SS.t[0:rows, 0:1]), reads=[SS], writes=[SS])
                kb.op(DVE, lambda: V.scalar_tensor_tensor(out=og.t[0:rows, :], in0=po.t[0:rows, :], scalar=SS.t[0:rows, 0:1],
                                                          in1=gt.t[0:rows, :], op0=ALU.mult, op1=ALU.mult),
                      reads=[po, SS, gt], writes=[og])

            def qkproj(ti):
                t0, n = TT[ti]
                rtile = RT[ti % 2]
                kb.dma(SP, rtile.t[:, :, 0:n], rot[:, :, t0:t0 + n], writes=[rtile])
                qkt = QK[ti % 2]
                cos = rtile.t[:, 0, 0:n]
                sin = rtile.t[:, 1, 0:n]
                for which in range(2):
                    sc = 1.0 if which == 0 else 1.0 / 16
                    pa, pb = PS[0], PS[1]
                    kb.mm(pa, pa.t[:, 0:n], [(wqk.t[:, kc, which * 256:which * 256 + 128], hT.t[:, kc, t0:t0 + n]) for kc in range(8)], [wqk, hT])
                    kb.mm(pb, pb.t[:, 0:n], [(wqk.t[:, kc, which * 256 + 128:which * 256 + 256], hT.t[:, kc, t0:t0 + n]) for kc in range(8)], [wqk, hT])
                    for part in range(2):
                        ca, cb = (cos, sin) if part == 0 else (sin, cos)
                        kb.op(DVE, lambda: V.scalar_tensor_tensor(out=t1.t[:, 0:n], in0=pa.t[:, 0:n], scalar=sc, in1=ca, op0=ALU.mult, op1=ALU.mult),
                              reads=[pa, rtile], writes=[t1])
                        kb.op(DVE, lambda: V.scalar_tensor_tensor(out=t2.t[:, 0:n], in0=pb.t[:, 0:n], scalar=sc, in1=cb, op0=ALU.mult, op1=ALU.mult),
                              reads=[pb, rtile], writes=[t2])
                        kb.op(DVE, lambda: V.tensor_tensor(out=qkt.t[:, 2 * which + part, 0:n], in0=t1.t[:, 0:n], in1=t2.t[:, 0:n],
                                                           op=(ALU.subtract if part == 0 else ALU.add)), reads=[t1, t2], writes=[qkt])

            qkproj(0)
            for ti, (t0, n) in enumerate(TT):
                is_s = (ti == 4)
                qkt = QK[ti % 2]
                if ti + 1 < len(TT):
                    qkproj(ti + 1)
                ogt = OGT[ti % 2]
                if not is_s:
                    def stageA(cc):
                        gi = ti * 4 + cc
                        first = (ti == 0 and cc == 0)
                        cs = cc * 128
                        tok0 = t0 + cs
                        vb, gt, atm, qd, kd = VB[gi % 2], GT[gi % 2], ATM[gi % 2], QD[gi % 2], KD[gi % 2]
                        pv, pg = PS[0], PS[1]
                        kb.mm(pv, pv.t[:], [(hT.t[:, kc, tok0:tok0 + 128], Wv.t[:, kc, :]) for kc in range(8)], [hT, Wv])
                        kb.mm(pg, pg.t[:], [(hT.t[:, kc, tok0:tok0 + 128], Wg.t[:, kc, :]) for kc in range(8)], [hT, Wg])
                        kb.op(ACT, lambda: S.copy(out=vb.t[:], in_=pv.t[:]), reads=[pv], writes=[vb])
                        kb.op(ACT, lambda: S.activation(out=gt.t[:], in_=pg.t[:], func=AF.Silu), reads=[pg], writes=[gt])
                        pa = PS[2]
                        kb.mm(pa, pa.t[:, 0:128], [(qkt.t[:, 2 + hf, cs:cs + 128], qkt.t[:, hf, cs:cs + 128]) for hf in range(2)], [qkt])
                        kb.op(DVE, lambda: V.tensor_tensor(out=atm.t[:], in0=pa.t[:, 0:128], in1=dmh, op=ALU.mult), reads=[pa, CST], writes=[atm])
                        if not first:
                            kb.op(DVE, lambda: V.tensor_tensor(out=qd.t[:], in0=qkt.t[:, 0:2, cs:cs + 128],
                                                               in1=qdh.unsqueeze(1).to_broadcast([128, 2, 128]), op=ALU.mult),
                                  reads=[qkt, CST], writes=[qd])
                        pab = pa.t[:].bitcast(BF16)
                        kb.transposes(pa, [(pab[:, 256 + hf * 128:256 + (hf + 1) * 128], qkt.t[:, 2 + hf, cs:cs + 128]) for hf in range(2)], [qkt], identb)
                        kb.op(DVE, lambda: V.tensor_scalar(out=kd.t[:], in0=pab[:, 256:512], scalar1=C("kd")[:, h:h + 1], scalar2=None,
                                                           op0=ALU.mult), reads=[pa, CST], writes=[kd])

                    def stageB(cc):
                        gi = ti * 4 + cc
                        first = (ti == 0 and cc == 0)
                        cs = cc * 128
                        vb, gt, atm, qd, og, kd = VB[gi % 2], GT[gi % 2], ATM[gi % 2], QD[gi % 2], OG[gi % 2], KD[gi % 2]
                        po = PS[3]
                        pairs = [(atm.t[:], vb.t[:])]
                        rds = [atm, vb]
                        if not first:
                            pairs += [(qd.t[:, hf, :], Sb.t[:, hf, :]) for hf in range(2)]
                            rds += [qd, Sb]
                        kb.mm(po, po.t[:], pairs, rds)
                        gn_gate(po, 128, gt, og)
                        pt = PS[4]
                        ptb = pt.t[:].bitcast(BF16)
                        kb.transposes(pt, [(ptb[:, ec * 128:(ec + 1) * 128], og.t[:, ec * 128:(ec + 1) * 128]) for ec in range(4)], [og], identb)
                        kb.op(ACT, lambda: S.copy(out=ogt.t[:, :, cs:cs + 128], in_=ptb[:, 0:512].rearrange("p (e t) -> p e t", e=4)),
                              reads=[pt], writes=[ogt])
                        for hf in range(2):
                            ps_ = PS[5 + hf]
                            kb.mm(ps_, ps_.t[:], [(kd.t[:, hf * 128:(hf + 1) * 128], vb.t[:])], [kd, vb])
                            if first:
                                kb.op(DVE, lambda: V.tensor_copy(out=Sf.t[:, hf, :], in_=ps_.t[:]), reads=[ps_], writes=[Sf])
                            else:
                                kb.op(DVE, lambda: V.scalar_tensor_tensor(out=Sf.t[:, hf, :], in0=Sf.t[:, hf, :], scalar=cdec, in1=ps_.t[:],
                                                                          op0=ALU.mult, op1=ALU.add), reads=[Sf, ps_], writes=[Sf])
                        kb.op(ACT, lambda: S.copy(out=Sb.t[:], in_=Sf.t[:]), reads=[Sf], writes=[Sb])

                    stageA(0)
                    for cc in range(4):
                        if cc + 1 < 4:
                            stageA(cc + 1)
                        stageB(cc)
                    if ti == 3:
                        kb.dma(SP, r_p[h].rearrange("(hf p) e -> p hf e", p=128), Sf.t[:], reads=[Sf])
                else:
                    vb, gt, atm, og, kd = VB[0], GT[0], ATM[0], OG[0], KD[0]
                    pv, pg = PS[0], PS[1]
                    kb.mm(pv, pv.t[0:NS, :], [(hT.t[:, kc, t0:t0 + NS], Wv.t[:, kc, :]) for kc in range(8)], [hT, Wv])
                    kb.mm(pg, pg.t[0:NS, :], [(hT.t[:, kc, t0:t0 + NS], Wg.t[:, kc, :]) for kc in range(8)], [hT, Wg])
                    kb.op(ACT, lambda: S.copy(out=vb.t[0:NS, :], in_=pv.t[0:NS, :]), reads=[pv], writes=[vb])
                    kb.op(ACT, lambda: S.activation(out=gt.t[0:NS, :], in_=pg.t[0:NS, :], func=AF.Silu), reads=[pg], writes=[gt])
                    pa = PS[2]
                    kb.mm(pa, pa.t[0:NS, 0:NS], [(qkt.t[:, 2 + hf, 0:NS], qkt.t[:, hf, 0:NS]) for hf in range(2)], [qkt])
                    kb.op(DVE, lambda: V.tensor_tensor(out=atm.t[0:NS, 0:NS], in0=pa.t[0:NS, 0:NS], in1=C("dmS")[0:NS, h * 64:(h + 1) * 64],
                                                       op=ALU.mult), reads=[pa, CST], writes=[atm])
                    qds = QD[0]
                    kb.op(DVE, lambda: V.tensor_tensor(out=qds.t[:, :, 0:NS], in0=qkt.t[:, 0:2, 0:NS],
                                                       in1=C("qdS")[:, h * 64:(h + 1) * 64].unsqueeze(1).to_broadcast([128, 2, NS]), op=ALU.mult),
                          reads=[qkt, CST], writes=[qds])
                    pab = pa.t[:].bitcast(BF16)
                    kb.transposes(pa, [(pab[0:NS, 256 + hf * 128:256 + (hf + 1) * 128], qkt.t[:, 2 + hf, 0:NS]) for hf in range(2)], [qkt], identb)
                    kb.op(DVE, lambda: V.tensor_scalar(out=kd.t[0:NS, :], in0=pab[0:NS, 256:512], scalar1=C("kdS")[0:NS, h:h + 1], scalar2=None,
                                                       op0=ALU.mult), reads=[pa, CST], writes=[kd])
                    po = PS[3]
                    kb.mm(po, po.t[0:NS, :], [(atm.t[0:NS, 0:NS], vb.t[0:NS, :])], [atm, vb], start=True, stop=False)
                    al_src = [RT[1], QK[1], OGT[1]]
                    al = []
                    for o_ in al_src:
                        ap_ = o_.t[:] if o_ is RT[1] else o_.t[:].rearrange("p a b -> p (a b)").bitcast(F32).rearrange("p (h e) -> p h e", h=2)
                        fz = dict(o_.rd)
                        if o_.lw and fz.get(o_.lw[0], (None, 0))[1] < o_.lw[2]:
                            fz[o_.lw[0]] = (o_.lw[1], o_.lw[2])
                        al.append(Buf(ap_, o_.name + "_al", fz))
                    stbufs = SST + al
                    def st_load(bb):
                        kb.dma(SP, stbufs[bb % 5].t[:], st_in[bb, h].rearrange("(hf p) e -> p hf e", p=128), writes=[stbufs[bb % 5]])
                    for bb in range(4):
                        st_load(bb)
                    for b in range(NSEQ):
                        stb, s0b, qp, kdb = stbufs[b % 5], S0B[b % 2], QP[b % 2], KDB[b % 2]
                        kb.op(ACT, lambda: S.copy(out=s0b.t[:], in_=stb.t[:]), reads=[stb], writes=[s0b])
                        kb.op(DVE, lambda: V.tensor_tensor(out=qp.t[:], in0=qds.t[:, :, 0:NS],
                                                           in1=C("bmc")[:, b * 64:(b + 1) * 64].unsqueeze(1).to_broadcast([128, 2, NS]), op=ALU.mult),
                              reads=[qds, CST], writes=[qp])
                        kb.mm(po, po.t[0:NS, :], [(qp.t[:, hf, :], s0b.t[:, hf, :]) for hf in range(2)], [qp, s0b], start=False, stop=(b == NSEQ - 1))
                        kb.op(DVE, lambda: V.tensor_scalar(out=kdb.t[0:NS, :], in0=kd.t[0:NS, :], scalar1=C("bm")[0:NS, b:b + 1], scalar2=None,
                                                           op0=ALU.mult), reads=[kd, CST], writes=[kdb])
                        for hf in range(2):
                            ps_ = PS[5 + hf]
                            kb.mm(ps_, ps_.t[:], [(kdb.t[0:NS, hf * 128:(hf + 1) * 128], vb.t[0:NS, :])], [kdb, vb])
                            kb.op(DVE, lambda: V.scalar_tensor_tensor(out=stb.t[:, hf, :], in0=stb.t[:, hf, :], scalar=cdec4, in1=ps_.t[:],
                                                                      op0=ALU.mult, op1=ALU.add), reads=[stb, ps_], writes=[stb])
                        kb.dma(SP, r_s[b, h].rearrange("(hf p) e -> p hf e", p=128), stb.t[:], reads=[stb])
                        if b + 4 < NSEQ:
                            st_load(b + 4)
                    for o_, a_ in zip(al_src, al):
                        for tk in ([a_.lw] if a_.lw else []):
                            if o_.rd.get(tk[0], (None, 0))[1] < tk[2]:
                                o_.rd[tk[0]] = (tk[1], tk[2])
                        for k_, v_ in a_.rd.items():
                            if o_.rd.get(k_, (None, 0))[1] < v_[1]:
                                o_.rd[k_] = v_
                        if a_.dsem:
                            kb.dpool.extend(a_.dsem.values())
                    gn_gate(po, NS, gt, og)
                    pt = PS[4]
                    ptb = pt.t[:].bitcast(BF16)
                    kb.transposes(pt, [(ptb[:, ec * 128:ec * 128 + NS], og.t[0:NS, ec * 128:(ec + 1) * 128]) for ec in range(4)], [og], identb)
                    kb.op(ACT, lambda: S.copy(out=ogt.t[:, :, 0:NS], in_=ptb[:, 0:512].rearrange("p (e t) -> p e t", e=4)[:, :, 0:NS]),
                          reads=[pt], writes=[ogt])
                for oc in range(8):
                    pb = PS[7]
                    kb.mm(pb, pb.t[:, 0:n], [(Wo.t[:, ec, oc * 128:(oc + 1) * 128], ogt.t[:, ec, 0:n]) for ec in range(4)], [Wo, ogt])
                    kb.op(DVE, lambda: V.tensor_tensor(out=xT.t[:, oc, t0:t0 + n], in0=xT.t[:, oc, t0:t0 + n], in1=pb.t[:, 0:n], op=ALU.add),
                          reads=[xT, pb], writes=[xT])


_CACHE = {}
LEVEL = 6
DEBUG = False
DBG_OUT = {}


def kernel(x_prompt, x_sample, cache_k, cache_v, cache_kidx, state_ret, page_table, rel_bias, ln_mix, ln_mlp,
           att_w_in, att_q_gain, att_k_gain, att_w_out, ret_w_in, ret_w_out, mlp_w_in, mlp_w_out):
    if "nc" not in _CACHE:
        _CACHE["nc"] = build_program(LEVEL)
    nc, (cst_np, cstr_np) = _CACHE["nc"]
    rot = _build_rot()
    f = lambda a: np.ascontiguousarray(np.asarray(a, dtype=np.float32))
    gl = np.concatenate([np.asarray(v, np.float32).reshape(8, 128).T for v in (ln_mix[0], ln_mlp[0], ln_mix[1], ln_mlp[1])], axis=1)
    shared = {
        "cache_k": f(cache_k).reshape(NPHYS * 128, 256), "cache_v": f(cache_v).reshape(NPHYS * 128, 256),
        "cache_i": f(cache_kidx).reshape(NPHYS * 128, 64),
        "relb": f(rel_bias).reshape(1, 256), "gains": np.ascontiguousarray(gl),
        "qkg": np.ascontiguousarray(np.stack([f(att_q_gain)[0], f(att_k_gain)[0]], axis=1)),
        "kg_row": f(att_k_gain).reshape(1, 128),
        "w_att_in": f(att_w_in)[0], "w_att_out": f(att_w_out)[0], "w_ret_in": f(ret_w_in)[0], "w_ret_out": f(ret_w_out)[0],
        "w_mlp_in": f(mlp_w_in), "w_mlp_out": f(mlp_w_out), "cst": cst_np, "cstr": cstr_np, "rot": rot,
    }
    xp = f(x_prompt)
    xs = f(x_sample)
    stt = f(state_ret)
    pt = np.ascontiguousarray(np.asarray(page_table, dtype=np.int32))
    in_maps = []
    for i in range(NCORES):
        m = dict(shared)
        m["x_p"] = xp[i]
        m["x_s"] = xs[16 * i:16 * i + 16].reshape(NS, D)
        m["st_in"] = stt[0, 16 * i:16 * i + 16]
        m["ptab"] = pt[16 * i:16 * i + 16].reshape(1, 256)
        in_maps.append(m)
    res = run_bass_kernel_spmd(nc, in_maps, core_ids=list(range(NCORES)))
    R = res.results
    if DEBUG:
        for k_ in ('dbgI', 'dbgA', 'dbgL', 'dbgN', 'dbgD', 'dbgW', 'dbgG', 'dbgB', 'dbgT', 'dbgM'):
            DBG_OUT[k_] = np.asarray(R[0][k_])
    cat = lambda k: np.stack([np.asarray(r[k]) for r in R])
    y_p = cat("y_p").astype(np.float32)
    y_s = cat("y_s").reshape(128, 4, D).astype(np.float32)
    k_p = cat("k_p").reshape(1, 8, SEQ, 2, 128).astype(np.float32)
    v_p = cat("v_p").reshape(1, 8, SEQ, 2, 128).astype(np.float32)
    i_p = cat("i_p").reshape(1, 8, SEQ, 64).astype(np.float32)
    r_p = cat("r_p").reshape(1, 8, 4, 256, 512).astype(np.float32)
    k_s = cat("k_s").reshape(1, 128, 4, 2, 128).astype(np.float32)
    v_s = cat("v_s").reshape(1, 128, 4, 2, 128).astype(np.float32)
    i_s = cat("i_s").reshape(1, 128, 4, 64).astype(np.float32)
    r_s = cat("r_s").reshape(1, 128, 4, 256, 512).astype(np.float32)
    return (y_p, y_s, k_p, v_p, i_p, r_p, k_s, v_s, i_s, r_s)
```
